# Optimizing a Trainium2 kernel written in Bass

```python
import math
import jax, jax.numpy as jnp
from jax import lax
import numpy as np

D_MODEL = 1024
BATCH = 4
SEQ = 8192
DEPTH = 2

HEAD_DIM = 64
A_HEADS = D_MODEL // (2 * HEAD_DIM)
B_HEADS = D_MODEL // (4 * HEAD_DIM)
C_HEADS = D_MODEL // (2 * HEAD_DIM)
D_HEADS = D_MODEL // (2 * HEAD_DIM)
D_KV_HEADS = D_HEADS // 4
A_W = A_HEADS * HEAD_DIM
B_W = B_HEADS * 2 * HEAD_DIM
C_W = C_HEADS * HEAD_DIM
D_W = D_HEADS * HEAD_DIM
D_KV_W = D_KV_HEADS * HEAD_DIM
EVEN_WIDTH = A_W + B_W
ODD_WIDTH = C_W + D_W
EVEN_SPLITS = (A_W, A_W, A_W, A_W, B_W, B_W, B_W, B_W)
ODD_SPLITS = (C_W, C_W, C_W, C_W, D_W, D_KV_W, D_KV_W, D_KV_W, D_KV_W, D_KV_W, D_KV_W, D_W, 3 * D_HEADS)
EVEN_COLS = 4 * A_W + 4 * B_W
ODD_COLS = 4 * C_W + 2 * D_W + 6 * D_KV_W + 3 * D_HEADS
N_EVEN = (DEPTH + 1) // 2
N_ODD = DEPTH // 2
DILATED_PATTERNS = ((128, 1), (512, 4), (2048, 16))
BAND_BLK = 128
DENSE_BLK = 128
MOBA_BLK = 256
MOBA_TOPK = 3
MOBA_CQ = 32
CMP_LEN = 32
CMP_STRIDE = 16
CMP_HIDDEN = 2 * HEAD_DIM
SLC_BLK = 64
SLC_TOPK = 16
NSA_WINDOW = 512
NSA_CQ = 64
RMS_EPS = 1e-6

kernel_name = 'hybrid_dilated_diff_moba_nsa'


def rmsnorm(x, g):
    xf = x.astype(jnp.float32)
    y = xf * lax.rsqrt(jnp.mean(xf * xf, axis=-1, keepdims=True) + RMS_EPS)
    return (y * g.astype(jnp.float32)).astype(x.dtype)


def alibi_slopes(n):
    return jnp.exp2(-8.0 * jnp.arange(1, n + 1, dtype=jnp.float32) / n)


def split_cols(h, sizes):
    out, off = [], 0
    for s in sizes:
        out.append(h[..., off:off + s])
        off += s
    return out


def softmax_parts(s, mask):
    s = jnp.where(mask, s, -jnp.inf)
    m = jnp.max(s, axis=-1, keepdims=True)
    m = jnp.where(jnp.isfinite(m), m, 0.0)
    p = jnp.exp(s - m)
    return p, m, jnp.sum(p, axis=-1, keepdims=True)


def take_blocks(blocks, idx):
    return jax.vmap(jax.vmap(lambda t, i: t[i]))(blocks, idx)


def banded_attention(q, k, v, max_dist, slopes, dist_scale=1):
    N, L, Hq, dh = q.shape
    Hkv = k.shape[2]
    G = Hq // Hkv
    nb = L // BAND_BLK
    nprev = -(-max_dist // BAND_BLK)
    C = (nprev + 1) * BAND_BLK

    def band(t):
        tb = jnp.pad(t.reshape(N, nb, BAND_BLK, Hkv, dh), ((0, 0), (nprev, 0), (0, 0), (0, 0), (0, 0)))
        return jnp.concatenate([tb[:, i:i + nb] for i in range(nprev + 1)], axis=2)

    kb, vb = band(k), band(v)
    qb = q.reshape(N, nb, BAND_BLK, Hkv, G, dh)
    s = jnp.einsum('nbqhgd,nbchd->nbhgqc', qb, kb, preferred_element_type=jnp.float32) * dh ** -0.5
    dist = jnp.arange(BAND_BLK)[:, None] + nprev * BAND_BLK - jnp.arange(C)[None, :]
    kglob = jnp.arange(nb)[:, None] * BAND_BLK - nprev * BAND_BLK + jnp.arange(C)[None, :]
    mask = ((dist >= 0) & (dist <= max_dist))[None, None, None, None] & (kglob >= 0)[None, :, None, None, None, :]
    bias = -slopes.astype(jnp.float32).reshape(Hkv, G, 1, 1) * (dist * dist_scale).astype(jnp.float32)
    p, m, l = softmax_parts(s + bias, mask)
    o = jnp.einsum('nbhgqc,nbchd->nbhgqd', p.astype(v.dtype), vb, preferred_element_type=jnp.float32) / l
    o = o.transpose(0, 1, 4, 2, 3, 5).reshape(N, L, Hq, dh)
    stat = lambda t: t[..., 0].transpose(0, 1, 4, 2, 3).reshape(N, L, Hq)
    return o, stat(m), stat(l)


def dilated_attention(q, k, v, slopes):
    B, S, H, dh = q.shape
    outs, maxes, dens = [], [], []
    for window, dil in DILATED_PATTERNS:
        span = dil * BAND_BLK
        Sp = -(-S // span) * span
        Lp = Sp // dil

        def to_phase(t):
            t = jnp.pad(t, ((0, 0), (0, Sp - S), (0, 0), (0, 0)))
            return jnp.swapaxes(t.reshape(B, Lp, dil, H, dh), 1, 2).reshape(B * dil, Lp, H, dh)

        def from_phase(t):
            rest = t.shape[2:]
            t = jnp.swapaxes(t.reshape(B, dil, Lp, *rest), 1, 2).reshape(B, Sp, *rest)
            return t[:, :S]

        o, m, l = banded_attention(to_phase(q), to_phase(k), to_phase(v), window // dil, slopes, dil)
        outs.append(from_phase(o))
        maxes.append(from_phase(m))
        dens.append(from_phase(l))
    m_all = jnp.stack(maxes)
    w = jnp.stack(dens) * jnp.exp(m_all - jnp.max(m_all, axis=0, keepdims=True))
    w = w / jnp.sum(w, axis=0, keepdims=True)
    return jnp.sum(w[..., None] * jnp.stack(outs), axis=0)


def diff_attention(q, k, v, lam, slopes):
    B, S, H, _, dh = q.shape
    nq = S // DENSE_BLK
    qb = jnp.moveaxis(q.reshape(B, nq, DENSE_BLK, H, 2, dh), 1, 0)
    kpos = jnp.arange(S)
    sl = slopes[None, :, None, None, None]

    def block(args):
        qi, i = args
        s = jnp.einsum('bqhcd,bkhcd->bhcqk', qi, k, preferred_element_type=jnp.float32) * dh ** -0.5
        dist = (i * DENSE_BLK + jnp.arange(DENSE_BLK))[:, None] - kpos[None, :]
        s = jnp.where(dist >= 0, s - sl * dist.astype(jnp.float32), -jnp.inf)
        p = jax.nn.softmax(s, axis=-1)
        a = p[:, :, 0] - lam * p[:, :, 1]
        return jnp.einsum('bhqk,bkhe->bqhe', a.astype(v.dtype), v, preferred_element_type=jnp.float32)

    o = lax.map(block, (qb, jnp.arange(nq)))
    return jnp.moveaxis(o, 0, 1).reshape(B, S, H, 2 * dh)


def moba_attention(q, k, v, slopes):
    B, S, H, dh = q.shape
    Sp = -(-S // MOBA_BLK) * MOBA_BLK
    pad = ((0, 0), (0, Sp - S), (0, 0), (0, 0))
    q, k, v = jnp.pad(q, pad), jnp.pad(k, pad), jnp.pad(v, pad)
    nblk = Sp // MOBA_BLK
    kbh = k.reshape(B, nblk, MOBA_BLK, H, dh).transpose(0, 3, 1, 2, 4)
    vbh = v.reshape(B, nblk, MOBA_BLK, H, dh).transpose(0, 3, 1, 2, 4)
    kmean = jnp.mean(kbh.astype(jnp.float32), axis=3)
    gate = jnp.einsum('bshd,bhnd->bhsn', q.astype(jnp.float32), kmean)
    past = jnp.arange(nblk)[None, :] < (jnp.arange(Sp) // MOBA_BLK)[:, None]
    n_sel = min(MOBA_TOPK, nblk)
    _, idx = lax.top_k(jnp.where(past, gate, -jnp.inf), n_sel)
    nch = Sp // MOBA_CQ
    qs = jnp.moveaxis(q.reshape(B, nch, MOBA_CQ, H, dh), 1, 0)
    idxs = idx.reshape(B, H, nch, MOBA_CQ, n_sel).transpose(2, 0, 1, 3, 4)
    scale = dh ** -0.5

    def chunk(args):
        qc, ic, c = args
        qpos = c * MOBA_CQ + jnp.arange(MOBA_CQ)
        own = (c * MOBA_CQ) // MOBA_BLK
        k_own = lax.dynamic_index_in_dim(kbh, own, axis=2, keepdims=False)
        v_own = lax.dynamic_index_in_dim(vbh, own, axis=2, keepdims=False)
        k_sel = take_blocks(kbh, ic)
        v_sel = take_blocks(vbh, ic)
        kpos_sel = ic[..., None] * MOBA_BLK + jnp.arange(MOBA_BLK)
        s_sel = jnp.einsum('bqhd,bhqnkd->bhqnk', qc, k_sel, preferred_element_type=jnp.float32) * scale
        s_sel = s_sel - slopes[None, :, None, None, None] * (qpos[None, None, :, None, None] - kpos_sel).astype(jnp.float32)
        valid_sel = jnp.broadcast_to((ic < own)[..., None], s_sel.shape)
        kpos_own = own * MOBA_BLK + jnp.arange(MOBA_BLK)
        s_own = jnp.einsum('bqhd,bhkd->bhqk', qc, k_own, preferred_element_type=jnp.float32) * scale
        s_own = s_own - slopes[None, :, None, None] * (qpos[:, None] - kpos_own[None, :]).astype(jnp.float32)
        mask_own = jnp.broadcast_to(kpos_own[None, :] <= qpos[:, None], s_own.shape)
        nk = n_sel * MOBA_BLK
        s = jnp.concatenate([s_sel.reshape(B, H, MOBA_CQ, nk), s_own], axis=-1)
        mask = jnp.concatenate([valid_sel.reshape(B, H, MOBA_CQ, nk), mask_own], axis=-1)
        p = jax.nn.softmax(jnp.where(mask, s, -jnp.inf), axis=-1)
        p_sel = p[..., :nk].reshape(B, H, MOBA_CQ, n_sel, MOBA_BLK).astype(v.dtype)
        p_own = p[..., nk:].astype(v.dtype)
        return (jnp.einsum('bhqnk,bhqnkd->bqhd', p_sel, v_sel, preferred_element_type=jnp.float32)
                + jnp.einsum('bhqk,bhkd->bqhd', p_own, v_own, preferred_element_type=jnp.float32))

    o = lax.map(chunk, (qs, idxs, jnp.arange(nch)))
    return jnp.moveaxis(o, 0, 1).reshape(B, Sp, H, dh)[:, :S]


def compress_blocks(t, pe, w1, w2):
    B, S, Hkv, dh = t.shape
    n_cmp = (S - CMP_LEN) // CMP_STRIDE + 1
    r = CMP_LEN // CMP_STRIDE
    ch = t.reshape(B, S // CMP_STRIDE, CMP_STRIDE, Hkv, dh)
    blocks = jnp.concatenate([ch[:, i:i + n_cmp] for i in range(r)], axis=2) + pe[None, None, :, None, :]
    flat = blocks.transpose(0, 1, 3, 2, 4).reshape(B, n_cmp, Hkv, CMP_LEN * dh)
    return jax.nn.silu(flat @ w1) @ w2


def nsa_attention(q, k_cmp, v_cmp, k_slc, v_slc, k_win, v_win, gates, slopes):
    B, S, Hq, dh = q.shape
    Hkv = k_slc.shape[2]
    G = Hq // Hkv
    n_cmp = k_cmp.shape[1]
    n_slc = S // SLC_BLK
    n_top = min(SLC_TOPK, n_slc)
    scale = dh ** -0.5
    cstart = jnp.arange(n_cmp) * CMP_STRIDE
    cend = cstart + CMP_LEN - 1
    sstart = jnp.arange(n_slc) * SLC_BLK
    cmp_in_slc = ((cstart[:, None] <= sstart[None, :] + SLC_BLK - 1) & (cend[:, None] >= sstart[None, :])).astype(jnp.float32)
    ksb = k_slc.reshape(B, n_slc, SLC_BLK, Hkv, dh).transpose(0, 3, 1, 2, 4)
    vsb = v_slc.reshape(B, n_slc, SLC_BLK, Hkv, dh).transpose(0, 3, 1, 2, 4)
    sl = slopes.reshape(Hkv, G)
    nch = S // NSA_CQ
    qs = jnp.moveaxis(q.reshape(B, nch, NSA_CQ, Hkv, G, dh), 1, 0)
    jb = jnp.arange(n_slc)[None, :]

    def chunk(args):
        qc, c = args
        qpos = c * NSA_CQ + jnp.arange(NSA_CQ)
        s = jnp.einsum('bqhgd,bnhd->bhgqn', qc, k_cmp, preferred_element_type=jnp.float32) * scale
        s = s - sl[:, :, None, None] * (qpos[:, None] - cend[None, :]).astype(jnp.float32)
        p, _, l = softmax_parts(s, cend[None, :] <= qpos[:, None])
        p = p / jnp.where(l > 0, l, 1.0)
        o_cmp = jnp.einsum('bhgqn,bnhd->bqhgd', p.astype(v_cmp.dtype), v_cmp, preferred_element_type=jnp.float32)
        imp = jnp.einsum('bhgqn,nj->bhqj', p, cmp_in_slc)
        qb = (qpos // SLC_BLK)[:, None]
        forced = (jb == 0) | (jb == qb) | (jb == qb - 1)
        imp = jnp.where(forced, jnp.inf, jnp.where(jb > qb, -jnp.inf, imp))
        _, idx = lax.top_k(imp, n_top)
        k_sel = take_blocks(ksb, idx)
        v_sel = take_blocks(vsb, idx)
        dist = qpos[None, None, :, None, None] - (idx[..., None] * SLC_BLK + jnp.arange(SLC_BLK))
        s2 = jnp.einsum('bqhgd,bhqnkd->bhgqnk', qc, k_sel, preferred_element_type=jnp.float32) * scale
        s2 = s2 - sl[None, :, :, None, None, None] * dist[:, :, None].astype(jnp.float32)
        mask2 = jnp.broadcast_to((dist >= 0)[:, :, None], s2.shape)
        nk = n_top * SLC_BLK
        p2 = jax.nn.softmax(jnp.where(mask2, s2, -jnp.inf).reshape(B, Hkv, G, NSA_CQ, nk), axis=-1)
        p2 = p2.reshape(B, Hkv, G, NSA_CQ, n_top, SLC_BLK).astype(v_sel.dtype)
        o_slc = jnp.einsum('bhgqnk,bhqnkd->bqhgd', p2, v_sel, preferred_element_type=jnp.float32)
        return o_cmp, o_slc

    o_cmp, o_slc = lax.map(chunk, (qs, jnp.arange(nch)))
    o_cmp = jnp.moveaxis(o_cmp, 0, 1).reshape(B, S, Hq, dh)
    o_slc = jnp.moveaxis(o_slc, 0, 1).reshape(B, S, Hq, dh)
    o_win, _, _ = banded_attention(q, k_win, v_win, NSA_WINDOW - 1, slopes)
    g = gates.astype(jnp.float32)
    return g[..., 0:1] * o_cmp + g[..., 1:2] * o_slc + g[..., 2:3] * o_win


def even_layer(x, ln, w_in, qkn, lam, subln, w_out, layer):
    B, S, _ = x.shape
    h = rmsnorm(x, ln)
    aq, ak, av, az, bq, bk, bv, bz = split_cols(h @ w_in, EVEN_SPLITS)
    heads = lambda t, n: t.reshape(B, S, n, -1)
    oa = dilated_attention(rmsnorm(heads(aq, A_HEADS), qkn[0]), rmsnorm(heads(ak, A_HEADS), qkn[1]),
                           heads(av, A_HEADS), alibi_slopes(A_HEADS)).astype(x.dtype)
    bq = rmsnorm(bq.reshape(B, S, B_HEADS, 2, HEAD_DIM), qkn[2])
    bk = rmsnorm(bk.reshape(B, S, B_HEADS, 2, HEAD_DIM), qkn[3])
    lam_init = 0.8 - 0.6 * math.exp(-0.3 * layer)
    lf = lam.astype(jnp.float32)
    lam_f = jnp.exp(jnp.sum(lf[0] * lf[1])) - jnp.exp(jnp.sum(lf[2] * lf[3])) + lam_init
    ob = diff_attention(bq, bk, heads(bv, B_HEADS), lam_f, alibi_slopes(B_HEADS))
    ob = (rmsnorm(ob, subln) * (1.0 - lam_init)).astype(x.dtype)
    mixed = jnp.concatenate([oa.reshape(B, S, A_W) * jax.nn.silu(az),
                             ob.reshape(B, S, B_W) * jax.nn.silu(bz)], axis=-1)
    return x + mixed @ w_out


def odd_layer(x, ln, w_in, qkn, phi_pe, phi_w1, phi_w2, w_out):
    B, S, _ = x.shape
    h = rmsnorm(x, ln)
    cq, ck, cv, cz, dq, dkc, dvc, dks, dvs, dkw, dvw, dz, dg = split_cols(h @ w_in, ODD_SPLITS)
    heads = lambda t, n: t.reshape(B, S, n, HEAD_DIM)
    oc = moba_attention(rmsnorm(heads(cq, C_HEADS), qkn[0]), rmsnorm(heads(ck, C_HEADS), qkn[1]),
                        heads(cv, C_HEADS), alibi_slopes(C_HEADS)).astype(x.dtype)
    k_cmp = rmsnorm(compress_blocks(heads(dkc, D_KV_HEADS), phi_pe[0], phi_w1[0], phi_w2[0]), qkn[3])
    v_cmp = compress_blocks(heads(dvc, D_KV_HEADS), phi_pe[1], phi_w1[1], phi_w2[1])
    od = nsa_attention(rmsnorm(heads(dq, D_HEADS), qkn[2]), k_cmp, v_cmp,
                       rmsnorm(heads(dks, D_KV_HEADS), qkn[4]), heads(dvs, D_KV_HEADS),
                       rmsnorm(heads(dkw, D_KV_HEADS), qkn[5]), heads(dvw, D_KV_HEADS),
                       jax.nn.sigmoid(dg.reshape(B, S, D_HEADS, 3)), alibi_slopes(D_HEADS)).astype(x.dtype)
    mixed = jnp.concatenate([oc.reshape(B, S, C_W) * jax.nn.silu(cz),
                             od.reshape(B, S, D_W) * jax.nn.silu(dz)], axis=-1)
    return x + mixed @ w_out


def setup_inputs(seed: int = 0) -> dict:
    key = jax.random.key(seed)
    ks = jax.random.split(key, 14)
    nrm = lambda k, shape: jax.random.normal(k, shape, jnp.float32)
    w = lambda k, shape, fan_in: nrm(k, shape) * fan_in ** -0.5
    gain = lambda k, shape: 1.0 + 0.05 * nrm(k, shape)
    return {
        'x': nrm(ks[0], (BATCH, SEQ, D_MODEL)),
        'ln_e': gain(ks[1], (N_EVEN, D_MODEL)),
        'w_in_e': w(ks[2], (N_EVEN, D_MODEL, EVEN_COLS), D_MODEL),
        'qkn_e': gain(ks[3], (N_EVEN, 4, HEAD_DIM)),
        'lam_e': 0.1 * nrm(ks[4], (N_EVEN, 4, HEAD_DIM)),
        'subln_e': gain(ks[5], (N_EVEN, 2 * HEAD_DIM)),
        'w_out_e': w(ks[6], (N_EVEN, EVEN_WIDTH, D_MODEL), EVEN_WIDTH),
        'ln_o': gain(ks[7], (N_ODD, D_MODEL)),
        'w_in_o': w(ks[8], (N_ODD, D_MODEL, ODD_COLS), D_MODEL),
        'qkn_o': gain(ks[9], (N_ODD, 6, HEAD_DIM)),
        'phi_pe': 0.1 * nrm(ks[10], (N_ODD, 2, CMP_LEN, HEAD_DIM)),
        'phi_w1': w(ks[11], (N_ODD, 2, CMP_LEN * HEAD_DIM, CMP_HIDDEN), CMP_LEN * HEAD_DIM),
        'phi_w2': w(ks[12], (N_ODD, 2, CMP_HIDDEN, HEAD_DIM), CMP_HIDDEN),
        'w_out_o': w(ks[13], (N_ODD, ODD_WIDTH, D_MODEL), ODD_WIDTH),
    }


def reference(x, ln_e, w_in_e, qkn_e, lam_e, subln_e, w_out_e, ln_o, w_in_o, qkn_o,
              phi_pe, phi_w1, phi_w2, w_out_o):
    for layer in range(DEPTH):
        i = layer // 2
        if layer % 2 == 0:
            x = even_layer(x, ln_e[i], w_in_e[i], qkn_e[i], lam_e[i], subln_e[i], w_out_e[i], layer)
        else:
            x = odd_layer(x, ln_o[i], w_in_o[i], qkn_o[i], phi_pe[i], phi_w1[i], phi_w2[i], w_out_o[i])
    return x
```

```python
import contextlib
import math
import numpy as np
import ml_dtypes
import concourse.bass as bass
import concourse.mybir as mybir
from concourse.bass_utils import run_bass_kernel_spmd

F32 = mybir.dt.float32
BF16 = mybir.dt.bfloat16
ALU = mybir.AluOpType
AF = mybir.ActivationFunctionType
AX = mybir.AxisListType


import os as _os
SAME_ENGINE_SYNC = _os.environ.get('SAME_SYNC', '1') == '1'


class Op:
    __slots__ = ("eng", "fn", "deps", "signal", "sem", "val", "dma", "semkey", "gi")


class Sched:
    ENG = ["pe", "act", "dve", "pool", "sp"]

    def __init__(self, nc):
        self.nc = nc
        self.ops = {e: [] for e in self.ENG}
        self.lastw = {}
        self.readers = {}
        self.n = 0

    def add(self, eng, fn, reads=(), writes=(), dma=False, semkey=None):
        op = Op()
        op.eng, op.fn, op.dma = eng, fn, dma
        op.signal = dma
        op.sem = None
        op.val = 0
        op.gi = self.n
        self.n += 1
        if dma:
            op.semkey = semkey if semkey is not None else ("dma", writes[0])
        else:
            op.semkey = None
        excl = [k for k in reads if isinstance(k, tuple) and k[0] == "bank"]
        if excl:
            reads = [k for k in reads if k not in excl]
            writes = list(writes) + [k for k in excl if k not in writes]
        deps = set()
        for k in reads:
            w = self.lastw.get(k)
            if w is not None:
                deps.add(w)
        for k in writes:
            w = self.lastw.get(k)
            if w is not None:
                deps.add(w)
            for r in self.readers.get(k, ()):
                deps.add(r)
        deps.discard(op)
        if SAME_ENGINE_SYNC:
            op.deps = [d for d in deps if not (d.eng == "pe" and eng == "pe" and not d.dma)]
        else:
            op.deps = [d for d in deps if d.dma or dma or d.eng != eng]
        for d in op.deps:
            d.signal = True
        for k in reads:
            self.readers.setdefault(k, []).append(op)
        for k in writes:
            self.lastw[k] = op
            self.readers[k] = []
        self.ops[eng].append(op)
        return op

    def emit(self, final_wait_ops=()):
        nc = self.nc
        import contextlib
        with contextlib.ExitStack() as st:
            esem = {e: st.enter_context(nc.semaphore("s_" + e)) for e in self.ENG}
            dsem = {}
            for e in self.ENG:
                if self.ops[e]:
                    self.ops[e][-1].signal = True
            for e in self.ENG:
                cnt = 0
                for op in self.ops[e]:
                    if op.dma:
                        if op.semkey not in dsem:
                            dsem[op.semkey] = [st.enter_context(nc.semaphore("d%d" % len(dsem))), 0]
                        ent = dsem[op.semkey]
                        ent[1] += 16
                        op.sem, op.val = ent[0], ent[1]
                    elif op.signal:
                        cnt += 1
                        op.sem, op.val = esem[e], cnt
            self.nsem = len(dsem) + 5
            block = st.enter_context(nc.Block())
            engobj = {"pe": block.tensor, "act": block.scalar, "dve": block.vector,
                      "pool": block.gpsimd, "sp": block.sync}

            def run_engine(e, extra_wait):
                def body(eng):
                    waited = {}
                    for op in self.ops[e]:
                        need = {}
                        for d in op.deps:
                            k = id(d.sem)
                            if k not in need or need[k][1] < d.val:
                                need[k] = (d.sem, d.val)
                        for k, (sem, val) in need.items():
                            if waited.get(k, 0) < val:
                                eng.wait_ge(sem, val)
                                waited[k] = val
                        ins = op.fn(eng)
                        if op.dma:
                            ins.then_inc(op.sem, 16)
                        elif op.signal:
                            ins.then_inc(op.sem, 1)
                    if e == "sp":
                        for k, ent in dsem.items():
                            eng.wait_ge(ent[0], ent[1])
                        for e2 in self.ENG:
                            if e2 != "sp" and self.ops[e2]:
                                lo = self.ops[e2][-1]
                                if not lo.dma:
                                    eng.wait_ge(lo.sem, lo.val)
                return body

            for e in self.ENG:
                extra = final_wait_ops if e == "sp" else ()
                engobj[e](run_engine(e, extra))


def I(name, *a, **k):
    return lambda e: getattr(e, name)(*a, **k)


NEG = -30000.0
EPS = 1e-6
D = 1024


class Ctx:
    def __init__(self, nc, st):
        self.nc, self.st = nc, st
        self.S = Sched(nc)
        self.uid = 0

    def sb(self, name, shape, dt):
        return self.st.enter_context(self.nc.sbuf_tensor(name, shape, dt))

    def ps(self, name, shape, dt):
        return self.st.enter_context(self.nc.psum_tensor(name, shape, dt))

    def din(self, name, shape, dt):
        return self.nc.dram_tensor(name, list(shape), dt, kind="ExternalInput").ap()

    def dout(self, name, shape, dt):
        return self.nc.dram_tensor(name, list(shape), dt, kind="ExternalOutput").ap()


def alloc_common(c, qkw=256, bkw=256, zw=128):
    c.bank = [c.ps("bank%d" % i, [128, 512], F32) for i in range(8)]
    c.ident = c.sb("ident", [128, 128], BF16)
    S = c.S
    S.add("pool", I("memset", c.ident[:], 1.0), writes=["ident"])
    S.add("pool", I("affine_select", out=c.ident[:], in_=c.ident[:], pattern=[[-1, 128]],
                                            compare_op=ALU.is_equal, fill=0.0, base=0, channel_multiplier=1),
          reads=["ident"], writes=["ident"])
    c.epsb = c.sb("epsb", [128, 1], F32)
    S.add("pool", I("memset", c.epsb[:], EPS), writes=["epsb"])
    c.xt = [c.sb("xt%d" % i, [128, D], F32) for i in range(2)]
    c.xsq_ = [c.sb("xsq%d" % i, [128, D], BF16) for i in range(2)]
    c.xn_ = [c.sb("xn%d" % i, [128, D], BF16) for i in range(2)]
    c.hT = [c.sb("hT%d" % i, [128, D], BF16) for i in range(2)]
    c.st1 = [c.sb("st1_%d" % i, [128, 4], F32) for i in range(2)]
    c.qk32_ = [c.sb("qk32_%d" % i, [128, qkw], F32) for i in range(2)]
    c.qksq_ = [c.sb("qksq_%d" % i, [128, qkw], F32) for i in range(2)]
    c.qkb_ = [c.sb("qkb_%d" % i, [128, bkw], BF16) for i in range(2)]
    c.st8 = [c.sb("st8_%d" % i, [128, 4], F32) for i in range(2)]
    c.ze_ = [c.sb("ze_%d" % i, [128, zw], F32) for i in range(2)]


def load_weights(c, w_dram, lncol_dram, Wb, tag):
    S = c.S
    ncols = w_dram.shape[1]
    lncol = c.sb("lncol" + tag, [128, 8], F32)
    S.add("sp", I("dma_start", out=lncol[:], in_=lncol_dram), writes=["lncol" + tag], dma=True)
    n = 0
    for kc in range(8):
        for c0 in range(0, ncols, 1024):
            c1 = min(ncols, c0 + 1024)
            i = n % 2
            n += 1
            stg = c.xt[i]
            S.add("sp", I("dma_start", out=stg[:, 0:c1 - c0], in_=w_dram[kc * 128:(kc + 1) * 128, c0:c1]),
                  writes=["xt%d" % i], dma=True)
            S.add("dve", I("tensor_scalar", out=Wb[:, kc, c0:c1], in0=stg[:, 0:c1 - c0], scalar1=lncol[:, kc:kc + 1],
                           scalar2=None, op0=ALU.mult),
                  reads=["xt%d" % i, "lncol" + tag], writes=[("Wb", kc)])


def rstd_from_sumsq(c, S, st, n, ncol, key):
    S.add("dve", I("tensor_scalar", out=st[:, 0:ncol], in0=st[:, 0:ncol], scalar1=1.0 / n, scalar2=EPS,
                                           op0=ALU.mult, op1=ALU.add), reads=[key], writes=[key])
    S.add("act", I("activation", out=st[:, 0:ncol], in_=st[:, 0:ncol], func=AF.Ln), reads=[key], writes=[key])
    S.add("act", I("activation", out=st[:, 0:ncol], in_=st[:, 0:ncol], func=AF.Exp, scale=-0.5), reads=[key], writes=[key])


def emit_hT(c, x_dram, t, xreads=()):
    S = c.S
    i = t % 2
    xt, hT, st1 = c.xt[i], c.hT[i], c.st1[i]
    xsq, xn, xnk, xsk = c.xsq_[i], c.xn_[i], "xn%d" % i, "xsq%d" % i
    S.add("sp", I("dma_start", out=xt[:], in_=x_dram[t * 128:(t + 1) * 128, :]), reads=list(xreads), writes=["xt%d" % i], dma=True)
    S.add("act", I("activation", out=xsq[:], in_=xt[:], func=AF.Square, accum_out=st1[:, 0:1]),
          reads=["xt%d" % i], writes=[xsk, "st1_%d" % i])
    rstd_from_sumsq(c, S, st1, D, 1, "st1_%d" % i)
    S.add("dve", I("tensor_scalar", out=xn[:], in0=xt[:], scalar1=st1[:, 0:1], scalar2=None, op0=ALU.mult),
          reads=["xt%d" % i, "st1_%d" % i], writes=[xnk])
    pb = c.bank[i].bitcast(BF16)
    for kc in range(8):
        S.add("pe", I("transpose", out=pb[:, kc * 128:(kc + 1) * 128], in_=xn[:, kc * 128:(kc + 1) * 128],
                                                 identity=c.ident[:]),
              reads=[xnk, "ident"], writes=[("bank", i)])
    S.add("act", I("copy", out=hT[:], in_=pb[:, :]), reads=[("bank", i)], writes=["hT%d" % i])


def proj_tile(c, t, Wb, tag, Gqk, QT, KT, v_evac, zs, z_dram=None, kq="QT", kk="KT", gkey="Gqk"):
    S = c.S
    i = t % 2
    hT = c.hT[i]
    pb = c.bank[2 + i]
    qk32, qksq, qkb, ze = c.qk32_[i][:, 0:256], c.qksq_[i][:, 0:256], c.qkb_[i][:, 0:256], c.ze_[i][:, 0:128]
    k32, ksq, kkb, kze = "qk32_%d" % i, "qksq_%d" % i, "qkb_%d" % i, "ze_%d" % i
    for kc in range(8):
        S.add("pe", I("matmul", pb[:, :], lhsT=hT[:, kc * 128:(kc + 1) * 128], rhs=Wb[:, kc, :],
                                              start=(kc == 0), stop=(kc == 7)),
              reads=["hT%d" % i] + [("Wb", kc)], writes=[("bank", 2 + i)])
    bk = ("bank", 2 + i)
    st8 = c.st8[i]
    S.add("act", I("copy", out=qk32[:], in_=pb[:, 0:256]), reads=[bk], writes=[k32])
    S.add("dve", I("tensor_tensor", out=qksq[:], in0=qk32[:], in1=qk32[:], op=ALU.mult), reads=[k32], writes=[ksq])
    S.add("dve", I("tensor_reduce", out=st8[:, 0:4], in_=qksq[:].rearrange("p (g d) -> p g d", d=64), axis=AX.X, op=ALU.add),
          reads=[ksq], writes=["st8_%d" % i])
    rstd_from_sumsq(c, S, st8, 64, 4, "st8_%d" % i)
    for g in range(4):
        S.add("dve", I("scalar_tensor_tensor", out=qkb[:, g * 64:(g + 1) * 64], in0=qk32[:, g * 64:(g + 1) * 64],
                                                           scalar=st8[:, g:g + 1], in1=Gqk[:, g * 64:(g + 1) * 64],
                                                           op0=ALU.mult, op1=ALU.mult),
              reads=[k32, "st8_%d" % i, gkey], writes=[kkb])
    tb = c.bank[4 + i].bitcast(BF16)
    S.add("pe", I("transpose", out=tb[:, 0:128], in_=qkb[:, 0:128], identity=c.ident[:]), reads=[kkb, "ident"], writes=[("bank", 4 + i)])
    S.add("pe", I("transpose", out=tb[:, 128:256], in_=qkb[:, 128:256], identity=c.ident[:]), reads=[kkb, "ident"], writes=[("bank", 4 + i)])
    S.add("act", I("copy", out=QT[:, t * 128:(t + 1) * 128], in_=tb[:, 0:128]), reads=[("bank", 4 + i)], writes=[(kq, t)])
    S.add("dve", I("tensor_copy", out=KT[:, t * 128:(t + 1) * 128], in_=tb[:, 128:256]), reads=[("bank", 4 + i)], writes=[(kk, t)])
    v_evac(t, pb, bk)
    S.add("act", I("activation", out=ze[:], in_=pb[:, 384:512], func=AF.Exp, scale=-1.0), reads=[bk], writes=[kze])
    S.add("dve", I("tensor_scalar", out=ze[:], in0=ze[:], scalar1=1.0, scalar2=None, op0=ALU.add), reads=[kze], writes=[kze])
    S.add("dve", I("reciprocal", out=ze[:], in_=ze[:]), reads=[kze], writes=[kze])
    if z_dram is None:
        S.add("dve", I("tensor_tensor", out=zs[:, t, :], in0=ze[:], in1=pb[:, 384:512], op=ALU.mult), reads=[kze, bk], writes=[("zs", t)])
    else:
        zt = c.zt[i]
        S.add("dve", I("tensor_tensor", out=zt[:, 0:128], in0=ze[:], in1=pb[:, 384:512], op=ALU.mult), reads=[kze, bk], writes=["zt%d" % i])
        S.add("sp", I("dma_start", out=z_dram[t * 128:(t + 1) * 128, 0:128], in_=zt[:, 0:128]), reads=["zt%d" % i], writes=[("zsd", t)], dma=True,
              semkey=("dma", "zt%d" % i))


def attn_A(c, QT, KT, Vaug, zs, maskA, biasA, boff, qws, out_dram, col0, S_len):
    S = c.S
    NU = S_len // 512
    ss = SweepState()
    ocnt = 0
    for U in range(NU):
        mixt = c.mixt[U % 2]
        mk = "mixt%d" % (U % 2)
        for hl in range(2):
            r0 = hl * 64
            Jlo, Jhi = max(0, 4 * U - 16), 4 * U + 3
            oi = 6 + (ocnt % 2)
            ocnt += 1
            ob = c.bank[oi]
            qw = qws[hl]
            nsub = 512 // qw
            qreads = [("QT", 4 * U + j) for j in range(4)]

            def extra(J, U=U):
                return [(c.ident[:], maskA[:, J - (4 * U - 16), :], ["ident", "maskA"])]

            def biasf(J, i, U=U, hl=hl, nsub=nsub):
                bc = boff[hl] + (J - 4 * U + 16) * nsub + i
                return biasA[:, bc:bc + 1], "biasA"

            sweep(c, ss, QT[r0:r0 + 64, U * 512:(U + 1) * 512], qreads, list(range(Jlo, Jhi + 1)),
                  lambda J, r0=r0: KT[r0:r0 + 64, J * 128:(J + 1) * 128], lambda J: [("KT", J)], extra, biasf, qw,
                  lambda J, hl=hl: Vaug[:, J, hl, :], lambda J: [("V", J)], [(oi, j * 65) for j in range(4)], 65)
            rl = c.rl[ocnt % 2]
            rk = "rl%d" % (ocnt % 2)
            S.add("dve", I("reciprocal", out=rl[:, 0:4], in_=ob[:, 0:260].rearrange("p (j d) -> p j d", d=65)[:, :, 64]),
                  reads=[("bank", oi)], writes=[rk])
            for j in range(4):
                S.add("dve", I("scalar_tensor_tensor", out=mixt[:, j, r0:r0 + 64], in0=ob[:, j * 65:j * 65 + 64], scalar=rl[:, j:j + 1],
                               in1=zs[:, 4 * U + j, r0:r0 + 64], op0=ALU.mult, op1=ALU.mult),
                      reads=[("bank", oi), rk, ("zs", 4 * U + j)], writes=[mk])
        S.add("sp", I("dma_start", out=out_dram[U * 512:(U + 1) * 512, col0:col0 + 128].rearrange("(j p) c -> p j c", p=128), in_=mixt[:, :, :]),
              reads=[mk], writes=[("out", col0, U)], dma=True, semkey=("dma", mk))


def attn_B(c, QT, KT, Vb, zs, maskC, biasB, boff, neglam, subG, out_dram, col0, S_len):
    S = c.S
    NU = S_len // 512
    ss = SweepState()
    for U in range(NU):
        mixt = c.mixt[U % 2]
        mk = "mixt%d" % (U % 2)
        for cc in range(2):
            r0 = cc * 64
            Jhi = 4 * U + 3
            obs = [c.bank[3 + 2 * cc], c.bank[4 + 2 * cc]]
            oks = [("bank", 3 + 2 * cc), ("bank", 4 + 2 * cc)]
            qreads = [("QT", 4 * U + j) for j in range(4)]

            def extra(J, U=U):
                d = J - 4 * U
                if d >= 0:
                    return [(c.ident[:], maskC[:, d, :], ["ident", "maskC"])]
                return []

            def biasf(J, i, U=U):
                bc = boff + (J - 4 * U + 4 * (NU - 1))
                return biasB[:, bc:bc + 1], "biasB"

            sweep(c, ss, QT[r0:r0 + 64, U * 512:(U + 1) * 512], qreads, list(range(0, Jhi + 1)),
                  lambda J, r0=r0: KT[r0:r0 + 64, J * 128:(J + 1) * 128], lambda J: [("KT", J)], extra, biasf, 512,
                  lambda J: Vb[:, J, :], lambda J: [("V", J)], [(3 + 2 * cc + j // 2, (j % 2) * 129) for j in range(4)], 129,
                  skip=lambda J, j, U=U: (J - 4 * U) > j)
            oc = c.oc[cc]
            rl = c.rl[cc]
            for hb in range(2):
                S.add("dve", I("reciprocal", out=rl[:, 2 * hb:2 * hb + 2],
                                                                in_=obs[hb][:, 0:258].rearrange("p (j d) -> p j d", d=129)[:, :, 128]),
                      reads=[oks[hb]], writes=["rl%d" % cc])
            for j in range(4):
                ob = obs[j // 2]
                o0 = (j % 2) * 129
                S.add("dve", I("tensor_scalar", out=oc[:, j, :], in0=ob[:, o0:o0 + 128], scalar1=rl[:, j:j + 1],
                                                                                   scalar2=None, op0=ALU.mult),
                      reads=[oks[j // 2], "rl%d" % cc], writes=["oc%d" % cc])
        df = c.df
        S.add("dve", I("scalar_tensor_tensor", out=df[:, :, :], in0=c.oc[1][:, :, :], scalar=neglam[:, 0:1], in1=c.oc[0][:, :, :],
                                                      op0=ALU.mult, op1=ALU.add), reads=["oc0", "oc1", "neglam"], writes=["df"])
        S.add("pool", I("tensor_tensor", out=c.dsq[:, :, :], in0=df[:, :, :], in1=df[:, :, :], op=ALU.mult), reads=["df"], writes=["dsq"])
        st = c.st4
        S.add("dve", I("tensor_reduce", out=st[:, 0:4], in_=c.dsq[:, :, :], axis=AX.X, op=ALU.add), reads=["dsq"], writes=["st4"])
        rstd_from_sumsq(c, S, st, 128, 4, "st4")
        for j in range(4):
            S.add("dve", I("scalar_tensor_tensor", out=df[:, j, :], in0=df[:, j, :], scalar=st[:, j:j + 1], in1=subG[:, :],
                                                               op0=ALU.mult, op1=ALU.mult), reads=["df", "st4", "subG"], writes=["df"])
        S.add("dve", I("tensor_tensor", out=mixt[:, :, :], in0=df[:, :, :], in1=zs[:, 4 * U:4 * U + 4, :], op=ALU.mult),
              reads=["df"] + [("zs", 4 * U + j) for j in range(4)], writes=[mk])
        S.add("sp", I("dma_start",
            out=out_dram[U * 512:(U + 1) * 512, col0:col0 + 128].rearrange("(j p) c -> p j c", p=128), in_=mixt[:, :, :]),
            reads=[mk], writes=[("out", col0, U)], dma=True, semkey=("dma", mk))


def slopes(n):
    return [2.0 ** (-8.0 * (i + 1) / n) for i in range(n)]


QWS_A = [128, 256, 512, 512]


def consts_L0(half, S_len):
    NU = S_len // 512
    k = np.arange(128)[:, None]
    q = np.arange(512)[None, :]
    maskA = np.zeros((128, 20, 512), np.float32)
    for r in range(20):
        dl = q - k + 2048 - 128 * r
        cnt = ((dl >= 0) & (dl <= 128)).astype(np.float32) + ((dl >= 0) & (dl % 4 == 0) & (dl <= 512)) + ((dl >= 0) & (dl % 16 == 0) & (dl <= 2048))
        maskA[:, r, :] = np.where(cnt > 0, np.log(np.maximum(cnt, 1)), NEG)
    maskC = np.zeros((128, 4, 512), np.float32)
    for d in range(4):
        maskC[:, d, :] = np.where(128 * d + k <= q, 0.0, NEG)
    sA = slopes(8)[4 * half:4 * half + 4]
    cols = []
    boffA = []
    kk = np.arange(128, dtype=np.float64)
    for hl in range(4):
        qw = QWS_A[hl]
        nsub = 512 // qw
        boffA.append(len(cols))
        for dd in range(-16, 4):
            for i in range(nsub):
                cols.append(sA[hl] * (kk + 128 * dd - i * qw - qw / 2))
    biasA = np.stack(cols, axis=1).astype(np.float32)
    sB = slopes(4)[2 * half:2 * half + 2]
    cols = []
    for hb in range(2):
        for dd in range(-4 * (NU - 1), 4):
            cols.append(sB[hb] * (kk + 128 * dd - 256))
    biasB = np.stack(cols, axis=1).astype(np.float32)
    return dict(maskA=maskA.astype(ml_dtypes.bfloat16), maskC=maskC.astype(ml_dtypes.bfloat16), biasA=biasA, biasB=biasB), boffA


def build_L0(S_len, debug=False):
    NT = S_len // 128
    NU = S_len // 512
    nc = bass.Bass("TRN2", target_bir_lowering=False)
    st = contextlib.ExitStack()
    with st:
        c = Ctx(nc, st)
        S = c.S
        x = c.din("x", [S_len, D], F32)
        lncol = c.din("lncol", [128, 8], F32)
        wA = [c.din("wA%d" % i, [D, 512], F32) for i in range(2)]
        wB = [c.din("wB%d" % i, [D, 512], F32) for i in range(2)]
        gA = c.din("gA", [1, 256], F32)
        gB = c.din("gB", [1, 256], F32)
        lam = c.din("lam", [1, 256], F32)
        subln = c.din("subln", [1, 128], F32)
        maskA_d = c.din("maskA", [128, 20, 512], BF16)
        maskC_d = c.din("maskC", [128, 4, 512], BF16)
        nbA = sum(20 * (512 // q) for q in QWS_A)
        biasA_d = c.din("biasA", [128, nbA], F32)
        biasB_d = c.din("biasB", [128, 2 * 4 * NU], F32)
        out = c.dout("mixed", [S_len, 512], BF16)
        alloc_common(c)
        Wb = c.sb("Wb", [128, 8, 512], BF16)
        QT = c.sb("QT", [128, S_len], BF16)
        KT = c.sb("KT", [128, S_len], BF16)
        Vsb = c.sb("Vsb", [128, NT * 130], BF16)
        zs = c.sb("zs", [128, NT, 128], BF16)
        maskA = c.sb("maskA_s", [128, 20, 512], BF16)
        maskC = c.sb("maskC_s", [128, 4, 512], BF16)
        biasA = c.sb("biasA_s", [128, nbA], F32)
        biasB = c.sb("biasB_s", [128, 2 * 4 * NU], F32)
        GA = c.sb("GA", [128, 256], F32)
        GB = c.sb("GB", [128, 256], F32)
        lamt = c.sb("lamt", [128, 256], F32)
        lamp = c.sb("lamp", [128, 128], F32)
        lam2 = c.sb("lam2", [128, 2], F32)
        neglam = c.sb("neglam", [128, 1], F32)
        subG = c.sb("subG", [128, 128], F32)
        c.pt = [c.sb("pt%d" % i, [128, 512], BF16) for i in range(4)]
        c.mixt = [c.sb("mixt%d" % i, [128, 4, 128], BF16) for i in range(2)]
        c.rl = [c.sb("rl%d" % i, [128, 4], F32) for i in range(2)]
        c.oc = [c.sb("oc%d" % i, [128, 4, 128], F32) for i in range(2)]
        c.df = c.sb("df", [128, 4, 128], F32)
        c.dsq = c.sb("dsq", [128, 4, 128], F32)
        c.st4 = c.sb("st4", [128, 4], F32)
        S.add("sp", I("dma_start", out=maskA[:], in_=maskA_d), writes=["maskA"], dma=True)
        S.add("sp", I("dma_start", out=maskC[:], in_=maskC_d), writes=["maskC"], dma=True)
        S.add("sp", I("dma_start", out=biasA[:], in_=biasA_d), writes=["biasA"], dma=True)
        S.add("sp", I("dma_start", out=biasB[:], in_=biasB_d), writes=["biasB"], dma=True)
        S.add("sp", I("dma_start", out=GA[:], in_=gA.partition_broadcast(128)), writes=["GA"], dma=True)
        S.add("sp", I("dma_start", out=GB[:], in_=gB.partition_broadcast(128)), writes=["GB"], dma=True)
        S.add("sp", I("dma_start", out=lamt[:], in_=lam.partition_broadcast(128)), writes=["lamt"], dma=True)
        S.add("sp", I("dma_start", out=subG[:], in_=subln.partition_broadcast(128)), writes=["subG"], dma=True)
        S.add("dve", I("tensor_scalar", out=GA[:, 0:128], in0=GA[:, 0:128], scalar1=0.125, scalar2=None, op0=ALU.mult), reads=["GA"], writes=["GA"])
        S.add("dve", I("tensor_scalar", out=GB[:, 0:128], in0=GB[:, 0:128], scalar1=0.125, scalar2=None, op0=ALU.mult), reads=["GB"], writes=["GB"])
        lam_init = 0.8 - 0.6 * math.exp(-0.3 * 0)
        S.add("dve", I("tensor_scalar", out=subG[:], in0=subG[:], scalar1=1.0 - lam_init, scalar2=None, op0=ALU.mult), reads=["subG"], writes=["subG"])
        lv = lamt[:, :].rearrange("p (a b d) -> p a b d", a=2, b=2)
        S.add("dve", I("tensor_tensor", out=lamp[:, :].rearrange("p (a d) -> p a d", a=2), in0=lv[:, :, 0, :], in1=lv[:, :, 1, :], op=ALU.mult),
              reads=["lamt"], writes=["lamp"])
        S.add("dve", I("tensor_reduce", out=lam2[:, 0:2], in_=lamp[:, :].rearrange("p (a d) -> p a d", a=2), axis=AX.X, op=ALU.add),
              reads=["lamp"], writes=["lam2"])
        S.add("act", I("activation", out=lam2[:, 0:2], in_=lam2[:, 0:2], func=AF.Exp), reads=["lam2"], writes=["lam2"])
        S.add("dve", I("tensor_tensor", out=neglam[:, 0:1], in0=lam2[:, 1:2], in1=lam2[:, 0:1], op=ALU.subtract), reads=["lam2"], writes=["neglam"])
        S.add("dve", I("tensor_scalar", out=neglam[:, 0:1], in0=neglam[:, 0:1], scalar1=-lam_init, scalar2=None, op0=ALU.add), reads=["neglam"], writes=["neglam"])

        boffA = []
        o = 0
        for hl in range(4):
            boffA.append(o)
            o += 20 * (512 // QWS_A[hl])

        passes = [("A", 0), ("A", 1), ("B", 0), ("B", 1)]
        for kind, pi in passes:
            tag = "%s%d" % (kind, pi)
            load_weights(c, (wA if kind == "A" else wB)[pi], lncol, Wb, tag)
            G = GA if kind == "A" else GB
            Gk = "GA" if kind == "A" else "GB"
            if kind == "A":
                Vv = Vsb[:, 0:NT * 130].rearrange("p (t h d) -> p t h d", h=2, d=65)
                S.add("pool", I("memset", Vv[:, :, :, 64:65], 1.0), reads=[("V", t) for t in range(NT)], writes=[("V", t) for t in range(NT)])

                def v_evac(t, pb, bk, Vv=Vv):
                    S.add("act", I("copy", out=Vv[:, t, :, 0:64], in_=pb[:, 256:384].rearrange("p (h d) -> p h d", h=2)),
                          reads=[bk], writes=[("V", t)])
            else:
                Vv = Vsb[:, 0:NT * 129].rearrange("p (t d) -> p t d", d=129)
                S.add("pool", I("memset", Vv[:, :, 128:129], 1.0), reads=[("V", t) for t in range(NT)], writes=[("V", t) for t in range(NT)])

                def v_evac(t, pb, bk, Vv=Vv):
                    S.add("act", I("copy", out=Vv[:, t, 0:128], in_=pb[:, 256:384]), reads=[bk], writes=[("V", t)])
            emit_hT(c, x, 0)
            for t in range(NT):
                if t + 1 < NT:
                    emit_hT(c, x, t + 1)
                proj_tile(c, t, Wb, tag, G, QT, KT, v_evac, zs, gkey=Gk)
            if kind == "A":
                attn_A(c, QT, KT, Vv, zs, maskA, biasA, boffA[2 * pi:2 * pi + 2], QWS_A[2 * pi:2 * pi + 2], out, 128 * pi, S_len)
            else:
                attn_B(c, QT, KT, Vv, zs, maskC, biasB, pi * 4 * NU, neglam, subG, out, 256 + 128 * pi, S_len)
        S.emit()
    return nc


class SweepState:
    def __init__(self):
        self.cnt = 0


def sweep(c, ss, Qap, qreads, Js, Kap, kreads, extra, biasf, qw, Vap, vreads, subs, dv, skip=None, sbanks=(0, 1, 2)):
    S = c.S
    nsub = 512 // qw
    started = set()
    lastJ = {}
    for J in Js:
        for j in range(4):
            if not (skip and skip(J, j)):
                lastJ[j] = J
    slot = {}

    def qk(J):
        si = sbanks[ss.cnt % len(sbanks)]
        pi = ss.cnt % 4
        ss.cnt += 1
        slot[J] = (si, pi)
        sbk = c.bank[si]
        ex = extra(J)
        S.add("pe", I("matmul", sbk[:, :], lhsT=Kap(J), rhs=Qap, start=True, stop=(len(ex) == 0)),
              reads=list(kreads(J)) + list(qreads), writes=[("bank", si)])
        for n, (l_, r_, rd) in enumerate(ex):
            S.add("pe", I("matmul", sbk[:, :], lhsT=l_, rhs=r_, start=False, stop=(n == len(ex) - 1)), reads=list(rd), writes=[("bank", si)])

    def act(J):
        si, pi = slot[J]
        sbk, pt = c.bank[si], c.pt[pi]
        for i in range(nsub):
            bap, bkey = biasf(J, i)
            S.add("act", I("activation", out=pt[:, i * qw:(i + 1) * qw], in_=sbk[:, i * qw:(i + 1) * qw], func=AF.Exp, bias=bap),
                  reads=[("bank", si), bkey], writes=[("pt", pi)])

    def av(J):
        si, pi = slot[J]
        pt = c.pt[pi]
        for j in range(4):
            if skip and skip(J, j):
                continue
            bi, o0 = subs[j]
            S.add("pe", I("matmul", c.bank[bi][:, o0:o0 + dv], lhsT=pt[:, j * 128:(j + 1) * 128], rhs=Vap(J),
                          start=(bi not in started), stop=(J == lastJ[j]), skip_group_check=True),
                  reads=[("pt", pi)] + list(vreads(J)), writes=[("bank", bi)])
            started.add(bi)

    if Js:
        qk(Js[0])
    for n, J in enumerate(Js):
        if n + 1 < len(Js):
            qk(Js[n + 1])
        act(J)
        av(J)


QWS = [128, 256, 512, 512]


def bias_tables_L1(half, NU):
    sl = slopes(8)[4 * half:4 * half + 4]
    kk = np.arange(128, dtype=np.float64)
    cols, offs = [], []
    for h in range(4):
        qw = QWS[h]
        offs.append(len(cols))
        for dd in range(-4 * (NU - 1), 4):
            for i in range(512 // qw):
                cols.append(sl[h] * (kk + 128 * dd - i * qw - qw / 2))
    bias = np.stack(cols, 1).astype(np.float32)
    cols, offc = [], []
    for h in range(4):
        qw = QWS[h]
        offc.append(len(cols))
        for e in range(NU):
            for i in range(512 // qw):
                cols.append(sl[h] * (16 * kk + 31 - 512 * e - i * qw - qw / 2))
    biasc = np.stack(cols, 1).astype(np.float32)
    return bias, offs, biasc, offc


def consts_L1(half, S_len):
    NU = S_len // 512
    k = np.arange(128)[:, None]
    q = np.arange(512)[None, :]
    maskC = np.zeros((128, 4, 512), np.float32)
    for d in range(4):
        maskC[:, d, :] = np.where(128 * d + k <= q, 0.0, NEG)
    maskW = np.zeros((128, 8, 512), np.float32)
    for r in range(8):
        dl = q - k + 128 * (4 - r)
        maskW[:, r, :] = np.where((dl >= 0) & (dl <= 511), 0.0, NEG)
    maskK = np.zeros((128, 5, 512), np.float32)
    for e in range(5):
        maskK[:, e, :] = np.where(16 * k + 31 <= 512 * e + q, 0.0, NEG)
    bias, offs, biasc, offc = bias_tables_L1(half, NU)
    cidx = np.arange(S_len)[None, :]
    E32 = np.zeros((128, S_len), np.float32)
    E32[:32] = (cidx // 256 == np.arange(32)[:, None])
    E64 = (cidx // 64 == np.arange(128)[:, None]).astype(np.float32)
    PB = np.zeros((128, 64), np.float32)
    PB[:, 32:] = -1e30
    n = np.arange(512)[:, None]
    j = np.arange(128)[None, :]
    cis = ((16 * n <= 64 * j + 63) & (16 * n + 31 >= 64 * j) & (n < 511)).astype(np.float32)
    cis = cis.reshape(4, 128, 128).transpose(1, 0, 2)
    qh = (np.arange(128) // 64)[:, None]
    r = np.arange(255)[None, :] - 127
    T = np.where(r == qh, 1e9, np.where(r == qh - 1, 3e9, np.where(r > qh, -1e30, 0.0))).astype(np.float32)
    T = np.concatenate([T, np.zeros((128, 1), np.float32)], 1)
    return dict(maskC=maskC.astype(ml_dtypes.bfloat16), maskW=maskW.astype(ml_dtypes.bfloat16), maskK=maskK.astype(ml_dtypes.bfloat16),
                bias1=bias, biasc=biasc, E32=E32.astype(ml_dtypes.bfloat16), E64=E64.astype(ml_dtypes.bfloat16), PB=PB,
                cis=cis.astype(ml_dtypes.bfloat16), Ttab=T), offs, offc


def proj_tile_D(c, t, Wb, GD, QT0, QT1, KsT, KwT, kvcT, Vs, Vw, zs_dram, gsig):
    S = c.S
    i = t % 2
    hT = c.hT[i]
    X, Y, Z = c.bank[2 + i], c.bank[4 + i], c.bank[6]
    for bi, pb, c0, n in ((2 + i, X, 0, 512), (4 + i, Y, 512, 512), (6, Z, 1024, 12)):
        for kc in range(8):
            S.add("pe", I("matmul", pb[:, 0:n], lhsT=hT[:, kc * 128:(kc + 1) * 128], rhs=Wb[:, kc, c0:c0 + n], start=(kc == 0), stop=(kc == 7)),
                  reads=["hT%d" % i, ("Wb", kc)], writes=[("bank", bi)])
    bx, by, bz = ("bank", 2 + i), ("bank", 4 + i), ("bank", 6)
    st8 = c.st8d
    qk32d, qksqd, qkbd, zed = c.qk32_[i], c.qksq_[i], c.qkb_[i], c.ze_[i]
    k32, ksq, kkb, kze = "qk32_%d" % i, "qksq_%d" % i, "qkb_%d" % i, "ze_%d" % i
    S.add("act", I("copy", out=qk32d[:], in_=X[:, :]), reads=[bx], writes=[k32])
    S.add("pool", I("tensor_tensor", out=qksqd[:], in0=qk32d[:], in1=qk32d[:], op=ALU.mult), reads=[k32], writes=[ksq])
    S.add("dve", I("tensor_reduce", out=st8[:, 0:8], in_=qksqd[:].rearrange("p (g d) -> p g d", d=64), axis=AX.X, op=ALU.add),
          reads=[ksq], writes=["st8d"])
    rstd_from_sumsq(c, S, st8, 64, 8, "st8d")
    for g in range(8):
        S.add("dve", I("scalar_tensor_tensor", out=qkbd[:, g * 64:(g + 1) * 64], in0=qk32d[:, g * 64:(g + 1) * 64],
                       scalar=st8[:, g:g + 1], in1=GD[:, g * 64:(g + 1) * 64], op0=ALU.mult, op1=ALU.mult),
              reads=[k32, "st8d", "GD"], writes=[kkb])
    S.add("act", I("copy", out=qkbd[:, 512:640], in_=Y[:, 0:128]), reads=[by], writes=[kkb])
    tb = c.bank[7].bitcast(BF16)
    for n in range(5):
        S.add("pe", I("transpose", out=tb[:, n * 128:(n + 1) * 128], in_=qkbd[:, n * 128:(n + 1) * 128], identity=c.ident[:]),
              reads=[kkb, "ident"], writes=[("bank", 7)])
    sl = slice(t * 128, (t + 1) * 128)
    S.add("act", I("copy", out=QT0[:, sl], in_=tb[:, 0:128]), reads=[("bank", 7)], writes=[("B0", t)])
    S.add("dve", I("tensor_copy", out=QT1[:, sl], in_=tb[:, 128:256]), reads=[("bank", 7)], writes=[("B1", t)])
    S.add("act", I("copy", out=KsT[:, sl], in_=tb[:, 256:384]), reads=[("bank", 7)], writes=[("B2", t)])
    S.add("dve", I("tensor_copy", out=KwT[:, sl], in_=tb[:, 384:512]), reads=[("bank", 7)], writes=[("B3", t)])
    S.add("act", I("copy", out=kvcT[:, sl], in_=tb[:, 512:640]), reads=[("bank", 7)], writes=[("B4", t)])
    S.add("act", I("copy", out=Vs[:, t, 0:64], in_=Y[:, 128:192]), reads=[by], writes=[("V", t)])
    S.add("act", I("copy", out=Vw[:, t, 0:64], in_=Y[:, 192:256]), reads=[by], writes=[("V", t)])
    zt = c.zt[i]
    S.add("act", I("activation", out=zed[:], in_=Y[:, 256:512], func=AF.Exp, scale=-1.0), reads=[by], writes=[kze])
    S.add("dve", I("tensor_scalar", out=zed[:], in0=zed[:], scalar1=1.0, scalar2=None, op0=ALU.add), reads=[kze], writes=[kze])
    S.add("dve", I("reciprocal", out=zed[:], in_=zed[:]), reads=[kze], writes=[kze])
    S.add("dve", I("tensor_tensor", out=zt[:, 0:256], in0=zed[:], in1=Y[:, 256:512], op=ALU.mult), reads=[kze, by], writes=["zt%d" % i])
    S.add("sp", I("dma_start", out=zs_dram[t * 128:(t + 1) * 128, 0:256], in_=zt[:, 0:256]), reads=["zt%d" % i], writes=[("zsd", t)], dma=True,
          semkey=("dma", "zt%d" % i))
    S.add("act", I("activation", out=gsig[:, t, :], in_=Z[:, 0:12], func=AF.Exp, scale=-1.0), reads=[bz], writes=[("gsig", t)])
    S.add("dve", I("tensor_scalar", out=gsig[:, t, :], in0=gsig[:, t, :], scalar1=1.0, scalar2=None, op0=ALU.add), reads=[("gsig", t)], writes=[("gsig", t)])
    S.add("dve", I("reciprocal", out=gsig[:, t, :], in_=gsig[:, t, :]), reads=[("gsig", t)], writes=[("gsig", t)])


def attn_C(c, QT, KT, Vaug, zs_dram, E32, maskC, bias1, boffs, qws, kmT, PB, out_dram, col0, S_len):
    S = c.S
    NU = S_len // 512
    NT = S_len // 128
    ss = SweepState()
    ocnt = 0
    for U in range(NU):
        mixt = c.mixt[U % 2]
        mk = "mixt%d" % (U % 2)
        zt = c.zu[U % 2]
        zk = "zu%d" % (U % 2)
        S.add("sp", I("dma_start", out=zt[:, :, 0:128], in_=zs_dram[U * 512:(U + 1) * 512, 0:128].rearrange("(j p) c -> p j c", p=128)),
              reads=[("zsd", 4 * U + j) for j in range(4)], writes=[zk], dma=True)
        for hl in range(2):
            r0 = hl * 64
            qreads = [("B0", 4 * U + j) for j in range(4)]
            gb = c.bank[7]
            for j in range(4):
                S.add("pe", I("matmul", gb[:, j * 32:(j + 1) * 32], lhsT=QT[r0:r0 + 64, (4 * U + j) * 128:(4 * U + j + 1) * 128],
                              rhs=kmT[r0:r0 + 64, 0:32], start=True, stop=True), reads=qreads + ["kmT"], writes=[("bank", 7)])
            for j in range(4):
                own = (4 * U + j) // 2
                S.add("dve", I("tensor_tensor", out=c.gm[:, j, :], in0=gb[:, j * 32:(j + 1) * 32], in1=PB[:, 32 - own:64 - own], op=ALU.add),
                      reads=[("bank", 7), "PB"], writes=["gm"])
            for j in range(4):
                S.add("dve", I("max", out=c.mx[:, j, :], in_=c.gm[:, j, :]), reads=["gm"], writes=["mx"])
            for j in range(4):
                S.add("dve", I("tensor_scalar", out=c.mb[:, j, 0:32], in0=c.gm[:, j, :], scalar1=c.mx[:, j, 2:3], scalar2=NEG, op0=ALU.is_lt, op1=ALU.mult),
                      reads=["gm", "mx"], writes=["mb"])
            S.add("dve", I("memset", c.mb[:, 0:2, 2 * U:2 * U + 1], 0.0), reads=["mb"], writes=["mb"])
            S.add("dve", I("memset", c.mb[:, 2:4, 2 * U + 1:2 * U + 2], 0.0), reads=["mb"], writes=["mb"])
            tb = c.bank[7].bitcast(BF16)
            for j in range(4):
                S.add("pe", I("transpose", out=tb[0:32, 512 + j * 128:512 + (j + 1) * 128], in_=c.mb[:, j, 0:32], identity=c.ident[:]),
                      reads=["mb", "ident"], writes=[("bank", 7)])
            MBT = c.MBT[ocnt % 2]
            mbk = "MBT%d" % (ocnt % 2)
            S.add("act", I("copy", out=MBT[0:32, :], in_=tb[0:32, 512:1024]), reads=[("bank", 7)], writes=[mbk])
            oi = 3 + (ocnt % 2)
            ocnt += 1
            qw = qws[hl]
            nsub = 512 // qw

            def extra(J, MBT=MBT, mbk=mbk, U=U):
                ex = [(E32[:, J * 128:(J + 1) * 128], MBT[:, :], ["Ebuf", mbk])]
                d = J - 4 * U
                if d >= 0:
                    ex.append((c.ident[:], maskC[:, d, :], ["ident", "maskC"]))
                return ex

            def biasf(J, i, U=U, hl=hl, nsub=nsub):
                col = boffs[hl] + (J - 4 * U + 4 * (NU - 1)) * nsub + i
                return bias1[:, col:col + 1], "bias1"

            sweep(c, ss, QT[r0:r0 + 64, U * 512:(U + 1) * 512], qreads, list(range(0, 4 * U + 4)),
                  lambda J: KT[r0:r0 + 64, J * 128:(J + 1) * 128], lambda J: [("B1", J)], extra, biasf, qw,
                  lambda J: Vaug[:, J, hl, :], lambda J: [("V", J)], [(oi, j * 65) for j in range(4)], 65,
                  skip=lambda J, j, U=U: (J - 4 * U) > j)
            ob = c.bank[oi]
            rl = c.rl[ocnt % 2]
            rk = "rl%d" % (ocnt % 2)
            S.add("dve", I("reciprocal", out=rl[:, 0:4], in_=ob[:, 0:260].rearrange("p (j d) -> p j d", d=65)[:, :, 64]), reads=[("bank", oi)], writes=[rk])
            for j in range(4):
                S.add("dve", I("scalar_tensor_tensor", out=mixt[:, j, r0:r0 + 64], in0=ob[:, j * 65:j * 65 + 64], scalar=rl[:, j:j + 1],
                               in1=zt[:, j, r0:r0 + 64], op0=ALU.mult, op1=ALU.mult), reads=[("bank", oi), rk, zk], writes=[mk])
        S.add("sp", I("dma_start", out=out_dram[U * 512:(U + 1) * 512, col0:col0 + 128].rearrange("(j p) c -> p j c", p=128), in_=mixt[:, :, 0:128]),
              reads=[mk], writes=[("out", col0, U)], dma=True, semkey=("dma", mk))


def nsa_compress(c, kvcT, W1b, W2kk, W2v, peT, gk3col, BO, kcmpT, Vc, S_len):
    S = c.S
    NC = S_len // 16 - 1
    NT = S_len // 128
    allkv = [("B4", t) for t in range(NT)]
    wk = [("Wb", kc) for kc in range(8)]
    b1 = c.b1
    for s, bi in ((0, 3), (1, 4)):
        rows = slice(64 * s, 64 * s + 64)
        for i in range(32):
            S.add("pe", I("matmul", c.bank[bi][:, 0:1], lhsT=W1b[rows, i, :], rhs=peT[rows, i:i + 1], start=(i == 0), stop=(i == 31)),
                  reads=wk + ["peT"], writes=[("bank", bi)])
        S.add("act", I("copy", out=b1[:, s:s + 1], in_=c.bank[bi][:, 0:1]), reads=[("bank", bi)], writes=["b1"])
    S.add("dve", I("tensor_scalar", out=b1[:, 2:4], in0=b1[:, 0:2], scalar1=-1.0, scalar2=None, op0=ALU.mult), reads=["b1"], writes=["b1"])
    hs = c.hs
    for s, bi in ((0, 5), (1, 6)):
        rows = slice(64 * s, 64 * s + 64)
        hb = c.bank[bi]
        for i in range(32):
            S.add("pe", I("matmul", hb[:, 0:NC], lhsT=W1b[rows, i, :], rhs=kvcT[rows, i:i + 16 * (NC - 1) + 1:16], start=(i == 0), stop=(i == 31)),
                  reads=wk + allkv, writes=[("bank", bi)])
        S.add("act", I("activation", out=c.he[:, 0:NC], in_=hb[:, 0:NC], func=AF.Exp, scale=-1.0, bias=b1[:, 2 + s:3 + s]), reads=[("bank", bi), "b1"], writes=["he"])
        S.add("dve", I("tensor_scalar", out=c.he[:, 0:NC], in0=c.he[:, 0:NC], scalar1=1.0, scalar2=None, op0=ALU.add), reads=["he"], writes=["he"])
        S.add("dve", I("reciprocal", out=c.he[:, 0:NC], in_=c.he[:, 0:NC]), reads=["he"], writes=["he"])
        S.add("dve", I("memset", hs[s][:, :], 0.0), writes=["hs%d" % s])
        S.add("dve", I("scalar_tensor_tensor", out=hs[s][:, 0:NC], in0=hb[:, 0:NC], scalar=b1[:, s:s + 1], in1=c.he[:, 0:NC], op0=ALU.add, op1=ALU.mult),
              reads=[("bank", bi), "b1", "he", "hs%d" % s], writes=["hs%d" % s])
    kb = c.bank[3]
    S.add("pe", I("matmul", kb[:, 0:NC], lhsT=W2kk[:, :], rhs=hs[0][:, 0:NC], start=True, stop=True), reads=["W2", "hs0"], writes=[("bank", 3)])
    S.add("act", I("activation", out=c.ksq[:, 0:NC], in_=kb[:, 0:NC], func=AF.Square), reads=[("bank", 3)], writes=["ksq"])
    S.add("pe", I("matmul", c.bank[4][:, 0:NC], lhsT=BO[:, :], rhs=c.ksq[:, 0:NC], start=True, stop=True), reads=["BO", "ksq"], writes=[("bank", 4)])
    S.add("dve", I("tensor_scalar", out=c.he[:, 0:NC], in0=c.bank[4][:, 0:NC], scalar1=1.0 / 64, scalar2=EPS, op0=ALU.mult, op1=ALU.add),
          reads=[("bank", 4)], writes=["he"])
    S.add("act", I("activation", out=c.he[:, 0:NC], in_=c.he[:, 0:NC], func=AF.Ln), reads=["he"], writes=["he"])
    S.add("act", I("activation", out=c.he[:, 0:NC], in_=c.he[:, 0:NC], func=AF.Exp, scale=-0.5), reads=["he"], writes=["he"])
    S.add("dve", I("memset", kcmpT[:, :], 0.0), writes=["kcmpT"])
    S.add("dve", I("scalar_tensor_tensor", out=kcmpT[:, 0:NC], in0=kb[:, 0:NC], scalar=gk3col[:, 0:1], in1=c.he[:, 0:NC], op0=ALU.mult, op1=ALU.mult),
          reads=[("bank", 3), "gk3col", "he", "kcmpT"], writes=["kcmpT"])
    nch = (NC + 127) // 128
    for cj in range(nch):
        S.add("pe", I("matmul", c.bank[5][:, cj * 64:(cj + 1) * 64], lhsT=hs[1][:, cj * 128:(cj + 1) * 128], rhs=W2v[:, :], start=True, stop=True),
              reads=["hs1", "W2"], writes=[("bank", 5)])
    S.add("act", I("copy", out=Vc[:, 0:nch, 0:64], in_=c.bank[5][:, 0:nch * 64].rearrange("p (c d) -> p c d", d=64)), reads=[("bank", 5)], writes=["Vc"])


def attn_D(c, QTs, KsT, KwT, kcmpT, Vs, Vw, Vc, zs_dram, gsig, E64, maskC, maskW, maskK, bias1, boffs, biasc, boffc, Ttab, out_dram, S_len):
    S = c.S
    NU = S_len // 512
    ss = SweepState()
    NCH = (S_len // 16 - 1 + 127) // 128
    for U in range(NU):
        mixt = c.mixt[U % 2]
        mk = "mixt%d" % (U % 2)
        zt = c.zu[U % 2]
        zk = "zu%d" % (U % 2)
        S.add("sp", I("dma_start", out=zt[:, :, :], in_=zs_dram[U * 512:(U + 1) * 512, 0:256].rearrange("(j p) c -> p j c", p=128)),
              reads=[("zsd", 4 * U + j) for j in range(4)], writes=[zk], dma=True)
        acc, imp = c.acc, c.imp
        gk = [("gsig", 4 * U + j) for j in range(4)]
        for h in range(4):
            QT = QTs[h // 2]
            qkey = "B%d" % (h // 2)
            r0 = (h % 2) * 64
            qreads = [(qkey, 4 * U + j) for j in range(4)]
            qw = QWS[h]
            nsub = 512 // qw
            chunks = [cj for cj in range(NCH) if U - 4 * cj >= 0]

            def extra(cj, U=U):
                e = U - 4 * cj
                if e <= 4:
                    return [(c.ident[:], maskK[:, e, :], ["ident", "maskK"])]
                return []

            def biasf(cj, i, U=U, h=h, nsub=nsub):
                col = boffc[h] + (U - 4 * cj) * nsub + i
                return biasc[:, col:col + 1], "biasc"

            subs = [(5 + j // 2, (j % 2) * 193) for j in range(4)]
            sweep(c, ss, QT[r0:r0 + 64, U * 512:(U + 1) * 512], qreads, chunks,
                  lambda cj: kcmpT[r0:r0 + 64, cj * 128:(cj + 1) * 128], lambda cj: ["kcmpT"], extra, biasf, qw,
                  lambda cj: Vc[:, cj, :], lambda cj: ["Vc"], subs, 193)
            rl = c.rl[h % 2]
            rk = "rl%d" % (h % 2)
            for bi in (5, 6):
                S.add("dve", I("tensor_scalar", out=rl[:, 2 * (bi - 5):2 * (bi - 5) + 2], in0=c.bank[bi][:, 0:386].rearrange("p (j d) -> p j d", d=193)[:, :, 64],
                               scalar1=1e-30, scalar2=None, op0=ALU.max), reads=[("bank", bi)], writes=[rk])
            S.add("dve", I("reciprocal", out=rl[:, 0:4], in_=rl[:, 0:4]), reads=[rk], writes=[rk])
            for j in range(4):
                bi, o0 = subs[j]
                ob = c.bank[bi]
                S.add("dve", I("tensor_scalar", out=acc[:, j, h, :], in0=ob[:, o0:o0 + 64], scalar1=rl[:, j:j + 1], scalar2=gsig[:, 4 * U + j, 3 * h:3 * h + 1],
                               op0=ALU.mult, op1=ALU.mult), reads=[("bank", bi), rk] + gk, writes=["acc"])
                if h == 0:
                    S.add("dve", I("tensor_scalar", out=imp[:, j, :], in0=ob[:, o0 + 65:o0 + 193], scalar1=rl[:, j:j + 1], scalar2=None, op0=ALU.mult),
                          reads=[("bank", bi), rk], writes=["imp"])
                else:
                    S.add("dve", I("scalar_tensor_tensor", out=imp[:, j, :], in0=ob[:, o0 + 65:o0 + 193], scalar=rl[:, j:j + 1], in1=imp[:, j, :],
                                   op0=ALU.mult, op1=ALU.add), reads=[("bank", bi), rk, "imp"], writes=["imp"])
        MBT = c.MBT[U % 2]
        mbk = "MBT%d" % (U % 2)
        tb = c.bank[7].bitcast(BF16)
        for j in range(4):
            tp = 4 * U + j
            S.add("dve", I("tensor_tensor", out=c.impb[:, :], in0=imp[:, j, :], in1=Ttab[:, 127 - 2 * tp:255 - 2 * tp], op=ALU.add), reads=["imp", "Ttab"], writes=["impb"])
            S.add("dve", I("memset", c.impb[:, 0:1], 2e9), reads=["impb"], writes=["impb"])
            S.add("dve", I("max", out=c.mx16[:, 0:8], in_=c.impb[:, :]), reads=["impb"], writes=["mx16"])
            S.add("dve", I("match_replace", out=c.impc[:, :], in_to_replace=c.mx16[:, 0:8], in_values=c.impb[:, :], imm_value=-3e38),
                  reads=["impb", "mx16"], writes=["impc"])
            S.add("dve", I("max", out=c.mx16[:, 8:16], in_=c.impc[:, :]), reads=["impc", "mx16"], writes=["mx16"])
            S.add("dve", I("tensor_scalar", out=c.mb[:, j, :], in0=c.impb[:, :], scalar1=c.mx16[:, 15:16], scalar2=NEG, op0=ALU.is_lt, op1=ALU.mult),
                  reads=["impb", "mx16"], writes=["mb"])
            S.add("pe", I("transpose", out=tb[:, j * 128:(j + 1) * 128], in_=c.mb[:, j, :], identity=c.ident[:]), reads=["mb", "ident"], writes=[("bank", 7)])
        S.add("act", I("copy", out=MBT[:, :], in_=tb[:, 0:512]), reads=[("bank", 7)], writes=[mbk])
        for br in range(2):
            for h in range(4):
                QT = QTs[h // 2]
                qkey = "B%d" % (h // 2)
                r0 = (h % 2) * 64
                qreads = [(qkey, 4 * U + j) for j in range(4)]
                qw = QWS[h]
                nsub = 512 // qw
                oi = 3 + (h % 2)

                def biasf(J, i, U=U, h=h, nsub=nsub):
                    col = boffs[h] + (J - 4 * U + 4 * (NU - 1)) * nsub + i
                    return bias1[:, col:col + 1], "bias1"

                if br == 0:
                    Js = list(range(0, 4 * U + 4))

                    def extra(J, U=U, MBT=MBT, mbk=mbk):
                        ex = [(E64[:, J * 128:(J + 1) * 128], MBT[:, :], ["Ebuf", mbk])]
                        d = J - 4 * U
                        if d >= 0:
                            ex.append((c.ident[:], maskC[:, d, :], ["ident", "maskC"]))
                        return ex
                    KTt, kkey, Vt = KsT, "B2", Vs
                else:
                    Js = list(range(max(0, 4 * U - 4), 4 * U + 4))

                    def extra(J, U=U):
                        return [(c.ident[:], maskW[:, J - (4 * U - 4), :], ["ident", "maskW"])]
                    KTt, kkey, Vt = KwT, "B3", Vw
                sweep(c, ss, QT[r0:r0 + 64, U * 512:(U + 1) * 512], qreads, Js,
                      lambda J, KTt=KTt, r0=r0: KTt[r0:r0 + 64, J * 128:(J + 1) * 128], lambda J, kkey=kkey: [(kkey, J)], extra, biasf, qw,
                      lambda J, Vt=Vt: Vt[:, J, :], lambda J: [("V", J)], [(oi, j * 65) for j in range(4)], 65,
                      skip=lambda J, j, U=U: (J - 4 * U) > j)
                ob = c.bank[oi]
                rl = c.rl[h % 2]
                rk = "rl%d" % (h % 2)
                S.add("dve", I("reciprocal", out=rl[:, 0:4], in_=ob[:, 0:260].rearrange("p (j d) -> p j d", d=65)[:, :, 64]), reads=[("bank", oi)], writes=[rk])
                for j in range(4):
                    S.add("dve", I("tensor_scalar", out=c.otmp[:, :], in0=ob[:, j * 65:j * 65 + 64], scalar1=rl[:, j:j + 1],
                                   scalar2=gsig[:, 4 * U + j, 3 * h + 1 + br:3 * h + 2 + br], op0=ALU.mult, op1=ALU.mult),
                          reads=[("bank", oi), rk] + gk, writes=["otmp"])
                    S.add("dve", I("tensor_tensor", out=acc[:, j, h, :], in0=acc[:, j, h, :], in1=c.otmp[:, :], op=ALU.add), reads=["otmp", "acc"], writes=["acc"])
        S.add("dve", I("tensor_tensor", out=mixt[:, :, :], in0=acc[:, :, :, :].rearrange("p j h d -> p j (h d)"), in1=zt[:, :, :], op=ALU.mult),
              reads=["acc", zk], writes=[mk])
        S.add("sp", I("dma_start", out=out_dram[U * 512:(U + 1) * 512, 256:512].rearrange("(j p) c -> p j c", p=128), in_=mixt[:, :, :]),
              reads=[mk], writes=[("out", 256, U)], dma=True, semkey=("dma", mk))


def build_L1(S_len):
    NT = S_len // 128
    NU = S_len // 512
    NB = S_len // 256
    nc = bass.Bass("TRN2", target_bir_lowering=False)
    st = contextlib.ExitStack()
    with st:
        c = Ctx(nc, st)
        S = c.S
        x = c.din("x", [S_len, D], F32)
        m0T = c.din("m0T", [D, S_len], BF16)
        woe = c.din("woe", [D, D], F32)
        lncol = c.din("lncol", [128, 8], F32)
        wC = [c.din("wC%d" % i, [D, 512], F32) for i in range(2)]
        wD = c.din("wD", [D, 1036], F32)
        gC_d = c.din("gC", [1, 256], F32)
        gD_d = c.din("gD", [1, 512], F32)
        W1_d = c.din("W1cat", [128, 32, 128], F32)
        W2_d = c.din("W2cat", [128, 192], F32)
        pe_d = c.din("peT", [128, 32], F32)
        gk3_d = c.din("gk3col", [128, 1], F32)
        BO_d = c.din("BO", [128, 128], BF16)
        cshape = dict(maskC=[128, 4, 512], maskW=[128, 8, 512], maskK=[128, 5, 512], E32=[128, S_len], E64=[128, S_len], cis=[128, 4, 128])
        cd = {k: c.din(k, v, BF16) for k, v in cshape.items()}
        nb1 = sum((4 * NU) * (512 // q) for q in QWS)
        nbc = sum(NU * (512 // q) for q in QWS)
        bias1_d = c.din("bias1", [128, nb1], F32)
        biasc_d = c.din("biasc", [128, nbc], F32)
        PB_d = c.din("PB", [128, 64], F32)
        T_d = c.din("Ttab", [128, 256], F32)
        x1 = c.dout("x1", [S_len, D], F32)
        out = c.dout("mixed", [S_len, 512], BF16)
        zsd = c.dout("zsd", [S_len, 256], BF16)
        alloc_common(c, qkw=512, bkw=640, zw=256)
        Wb = c.sb("Wb", [128, 8, 1040], BF16)
        Bt = [c.sb("B%d" % i, [128, S_len], BF16) for i in range(5)]
        Vsb = c.sb("Vsb", [128, NT * 130], BF16)
        maskC = c.sb("maskC_s", [128, 4, 512], BF16)
        maskW = c.sb("maskW_s", [128, 8, 512], BF16)
        maskK = c.sb("maskK_s", [128, 5, 512], BF16)
        bias1 = c.sb("bias1_s", [128, nb1], F32)
        biasc = c.sb("biasc_s", [128, nbc], F32)
        PB = c.sb("PB_s", [128, 64], F32)
        Ttab = c.sb("T_s", [128, 256], F32)
        GC = c.sb("GC", [128, 256], F32)
        GD = c.sb("GD", [128, 512], F32)
        BO = c.sb("BO_s", [128, 128], BF16)
        W2f = c.sb("W2f", [128, 192], F32)
        W2b = c.sb("W2b", [128, 192], BF16)
        pef = c.sb("pef", [128, 32], F32)
        peT = c.sb("peTb", [128, 32], BF16)
        gk3col = c.sb("gk3", [128, 1], F32)
        c.b1 = c.sb("b1", [128, 4], F32)
        c.he = c.sb("he", [128, 512], F32)
        c.hs = [c.sb("hs%d" % i, [128, 512], BF16) for i in range(2)]
        c.ksq = c.sb("ksq", [128, 512], BF16)
        kcmpT = c.sb("kcmpT", [128, 512], BF16)
        Vc = c.sb("Vc", [128, 4, 193], BF16)
        gsig = c.sb("gsig", [128, NT, 12], F32)
        kmf = c.sb("kmf", [128, 32], F32)
        kmT = c.sb("kmT", [128, 32], BF16)
        c.pt = [c.sb("pt%d" % i, [128, 512], BF16) for i in range(4)]
        c.mixt = [c.sb("mixt%d" % i, [128, 4, 256], BF16) for i in range(2)]
        c.zu = [c.sb("zu%d" % i, [128, 4, 256], BF16) for i in range(2)]
        c.zt = [c.sb("zt%d" % i, [128, 256], BF16) for i in range(2)]
        c.rl = [c.sb("rl%d" % i, [128, 4], F32) for i in range(2)]
        c.MBT = [c.sb("MBT%d" % i, [128, 512], BF16) for i in range(2)]
        c.gm = c.sb("gm", [128, 4, 32], F32)
        c.mx = c.sb("mx", [128, 4, 8], F32)
        c.mb = c.sb("mb", [128, 4, 128], BF16)
        c.mx16 = c.sb("mx16", [128, 16], F32)
        c.imp = c.sb("imp", [128, 4, 128], F32)
        c.impb = c.sb("impb", [128, 128], F32)
        c.impc = c.sb("impc", [128, 128], F32)
        c.acc = c.sb("acc", [128, 4, 4, 64], F32)
        c.otmp = c.sb("otmp", [128, 64], F32)
        c.st8d = c.sb("st8d", [128, 8], F32)
        m0 = [c.hT[i][:, :].rearrange("p (k t) -> p k t", k=8) for i in range(2)]

        def ld(dst, src, key):
            S.add("sp", I("dma_start", out=dst, in_=src), writes=[key], dma=True)
        ld(maskC[:], cd["maskC"], "maskC"); ld(maskW[:], cd["maskW"], "maskW"); ld(maskK[:], cd["maskK"], "maskK")
        ld(bias1[:], bias1_d, "bias1"); ld(biasc[:], biasc_d, "biasc"); ld(PB[:], PB_d, "PB"); ld(Ttab[:], T_d, "Ttab")
        ld(GC[:], gC_d.partition_broadcast(128), "GC"); ld(GD[:], gD_d.partition_broadcast(128), "GD")
        ld(BO[:], BO_d, "BO"); ld(W2f[:], W2_d, "W2f"); ld(pef[:], pe_d, "pef"); ld(gk3col[:], gk3_d, "gk3col")
        S.add("dve", I("tensor_scalar", out=GC[:, 0:128], in0=GC[:, 0:128], scalar1=0.125, scalar2=None, op0=ALU.mult), reads=["GC"], writes=["GC"])
        S.add("dve", I("tensor_scalar", out=GD[:, 0:256], in0=GD[:, 0:256], scalar1=0.125, scalar2=None, op0=ALU.mult), reads=["GD"], writes=["GD"])
        S.add("dve", I("tensor_copy", out=W2b[:], in_=W2f[:]), reads=["W2f"], writes=["W2"])
        S.add("dve", I("tensor_copy", out=peT[:], in_=pef[:]), reads=["pef"], writes=["peT"])
        S.add("pool", I("memset", Vc[:, :, :], 0.0), writes=["Vc"])
        S.add("pool", I("memset", Vc[:, :, 64:65], 1.0), reads=["Vc"], writes=["Vc"])
        S.add("sp", I("dma_start", out=Vc[:, :, 65:193], in_=cd["cis"]), reads=["Vc"], writes=["Vc"], dma=True)
        for i in range(2):
            S.add("pool", I("memset", c.MBT[i][:, :], 0.0), writes=["MBT%d" % i])

        for kc in range(8):
            i = kc % 2
            S.add("sp", I("dma_start", out=c.xt[i][:, :], in_=woe[kc * 128:(kc + 1) * 128, :]), writes=["xt%d" % i], dma=True)
            S.add("dve", I("tensor_copy", out=Wb[:, kc, 0:1024], in_=c.xt[i][:, :]), reads=["xt%d" % i], writes=[("Wb", kc)])
        for t in range(NT):
            i = t % 2
            xt = c.xt[i]
            S.add("sp", I("dma_start", out=xt[:], in_=x[t * 128:(t + 1) * 128, :]), writes=["xt%d" % i], dma=True)
            S.add("sp", I("dma_start", out=m0[i][:, :, :], in_=m0T[:, t * 128:(t + 1) * 128].rearrange("(kc p) t -> p kc t", p=128)),
                  writes=["hT%d" % i], dma=True)
            for cg in range(2):
                bi = 2 + 2 * i + cg
                for kc in range(8):
                    S.add("pe", I("matmul", c.bank[bi][:, :], lhsT=m0[i][:, kc, :], rhs=Wb[:, kc, cg * 512:(cg + 1) * 512], start=(kc == 0), stop=(kc == 7)),
                          reads=["hT%d" % i, ("Wb", kc)], writes=[("bank", bi)])
                S.add("dve", I("tensor_tensor", out=xt[:, cg * 512:(cg + 1) * 512], in0=xt[:, cg * 512:(cg + 1) * 512], in1=c.bank[bi][:, :], op=ALU.add),
                      reads=["xt%d" % i, ("bank", bi)], writes=["xt%d" % i])
            S.add("sp", I("dma_start", out=x1[t * 128:(t + 1) * 128, :], in_=xt[:]), reads=["xt%d" % i], writes=[("x1d", t)], dma=True,
                  semkey=("dma", "x1st%d" % i))

        bias_l1, boffs, biasc_l1, boffc = None, None, None, None
        boffs, o = [], 0
        for h in range(4):
            boffs.append(o)
            o += (4 * NU) * (512 // QWS[h])
        boffc, o = [], 0
        for h in range(4):
            boffc.append(o)
            o += NU * (512 // QWS[h])

        QT, KT = Bt[0], Bt[1]
        for pi in range(2):
            tag = "C%d" % pi
            load_weights(c, wC[pi], lncol, Wb[:, :, 0:512], tag)
            Vv = Vsb[:, 0:NT * 130].rearrange("p (t h d) -> p t h d", h=2, d=65)
            S.add("pool", I("memset", Vv[:, :, :, 64:65], 1.0), reads=[("V", t) for t in range(NT)], writes=[("V", t) for t in range(NT)])
            ld(Bt[4][:], cd["E32"], "Ebuf")

            def v_evac(t, pb, bk, Vv=Vv):
                S.add("act", I("copy", out=Vv[:, t, :, 0:64], in_=pb[:, 256:384].rearrange("p (h d) -> p h d", h=2)), reads=[bk], writes=[("V", t)])
            emit_hT(c, x1, 0, xreads=[("x1d", 0)])
            for t in range(NT):
                if t + 1 < NT:
                    emit_hT(c, x1, t + 1, xreads=[("x1d", t + 1)])
                proj_tile(c, t, Wb[:, :, 0:512], tag, GC, QT, KT, v_evac, None, z_dram=zsd, kq="B0", kk="B1", gkey="GC")
            S.add("dve", I("tensor_reduce", out=kmf[:, 0:NB], in_=KT[:, :].rearrange("p (n k) -> p n k", k=256), axis=AX.X, op=ALU.add),
                  reads=[("B1", t) for t in range(NT)], writes=["kmf"])
            S.add("dve", I("memset", kmT[:, :], 0.0), writes=["kmT"])
            S.add("dve", I("tensor_scalar", out=kmT[:, 0:NB], in0=kmf[:, 0:NB], scalar1=1.0 / 256, scalar2=None, op0=ALU.mult), reads=["kmf", "kmT"], writes=["kmT"])
            attn_C(c, QT, KT, Vv, zsd, Bt[4], maskC, bias1, boffs[2 * pi:2 * pi + 2], QWS[2 * pi:2 * pi + 2], kmT, PB, out, 128 * pi, S_len)

        load_weights(c, wD, lncol, Wb[:, :, 0:1036], "D")
        Vs = Vsb[:, 0:NT * 65].rearrange("p (t d) -> p t d", d=65)
        Vw = Vsb[:, NT * 65:NT * 130].rearrange("p (t d) -> p t d", d=65)
        S.add("pool", I("memset", Vs[:, :, 64:65], 1.0), reads=[("V", t) for t in range(NT)], writes=[("V", t) for t in range(NT)])
        S.add("pool", I("memset", Vw[:, :, 64:65], 1.0), reads=[("V", t) for t in range(NT)], writes=[("V", t) for t in range(NT)])
        emit_hT(c, x1, 0, xreads=[("x1d", 0)])
        for t in range(NT):
            if t + 1 < NT:
                emit_hT(c, x1, t + 1, xreads=[("x1d", t + 1)])
            proj_tile_D(c, t, Wb, GD, Bt[0], Bt[1], Bt[2], Bt[3], Bt[4], Vs, Vw, zsd, gsig)
        W1b = Wb[:, :, :].rearrange("p a b -> p (a b)")[:, 0:4096].rearrange("p (i h) -> p i h", h=128)
        wk = [("Wb", kc) for kc in range(8)]
        for q4 in range(4):
            i = q4 % 2
            S.add("sp", I("dma_start", out=c.xt[i][:, :].rearrange("p (i h) -> p i h", h=128), in_=W1_d[:, q4 * 8:(q4 + 1) * 8, :]), writes=["xt%d" % i], dma=True)
            S.add("dve", I("tensor_copy", out=W1b[:, q4 * 8:(q4 + 1) * 8, :], in_=c.xt[i][:, :].rearrange("p (i h) -> p i h", h=128)),
                  reads=["xt%d" % i] + wk, writes=wk)
        nsa_compress(c, Bt[4], W1b, W2b[:, 0:128], W2b[:, 128:192], peT, gk3col, BO, kcmpT, Vc, S_len)
        S.add("sp", I("dma_start", out=Bt[4][:], in_=cd["E64"]), reads=[("B4", t) for t in range(NT)] + ["Ebuf"], writes=["Ebuf"] + [("B4", t) for t in range(NT)], dma=True,
              semkey=("dma", "Ebuf"))
        attn_D(c, [Bt[0], Bt[1]], Bt[2], Bt[3], kcmpT, Vs, Vw, Vc, zsd, gsig, Bt[4], maskC, maskW, maskK, bias1, boffs, biasc, boffc, Ttab, out, S_len)
        S.emit()
    return nc


def build_L2(ntok):
    NT = ntok // 128
    nc = bass.Bass("TRN2", target_bir_lowering=False)
    st = contextlib.ExitStack()
    with st:
        c = Ctx(nc, st)
        S = c.S
        x1 = c.din("x1", [ntok, D], F32)
        m1T = c.din("m1T", [D, ntok], BF16)
        woo = c.din("woo", [D, D], F32)
        out = c.dout("out", [ntok, D], F32)
        c.bank = [c.ps("bank%d" % i, [128, 512], F32) for i in range(8)]
        xt = [c.sb("xt%d" % i, [128, D], F32) for i in range(2)]
        m0 = [c.sb("m0_%d" % i, [128, 8, 128], BF16) for i in range(2)]
        Wb = c.sb("Wb", [128, 8, 1024], BF16)
        for kc in range(8):
            i = kc % 2
            S.add("sp", I("dma_start", out=xt[i][:, :], in_=woo[kc * 128:(kc + 1) * 128, :]), writes=["xt%d" % i], dma=True)
            S.add("dve", I("tensor_copy", out=Wb[:, kc, :], in_=xt[i][:, :]), reads=["xt%d" % i], writes=[("Wb", kc)])
        for t in range(NT):
            i = t % 2
            S.add("sp", I("dma_start", out=xt[i][:], in_=x1[t * 128:(t + 1) * 128, :]), writes=["xt%d" % i], dma=True)
            S.add("sp", I("dma_start", out=m0[i][:, :, :], in_=m1T[:, t * 128:(t + 1) * 128].rearrange("(kc p) t -> p kc t", p=128)),
                  writes=["m0_%d" % i], dma=True)
            for cg in range(2):
                bi = 2 * i + cg
                for kc in range(8):
                    S.add("pe", I("matmul", c.bank[bi][:, :], lhsT=m0[i][:, kc, :], rhs=Wb[:, kc, cg * 512:(cg + 1) * 512], start=(kc == 0), stop=(kc == 7)),
                          reads=["m0_%d" % i, ("Wb", kc)], writes=[("bank", bi)])
                S.add("dve", I("tensor_tensor", out=xt[i][:, cg * 512:(cg + 1) * 512], in0=xt[i][:, cg * 512:(cg + 1) * 512], in1=c.bank[bi][:, :], op=ALU.add),
                      reads=["xt%d" % i, ("bank", bi)], writes=["xt%d" % i])
            S.add("sp", I("dma_start", out=out[t * 128:(t + 1) * 128, :], in_=xt[i][:]), reads=["xt%d" % i], writes=[("outd", t)], dma=True,
                  semkey=("dma", "ost%d" % i))
        S.emit()
    return nc


def host_inputs_L0(inp, b, half, S_len):
    w = inp["w_in_e"][0]
    aq, ak, av, az = [w[:, i * 512:(i + 1) * 512] for i in range(4)]
    bq, bk, bv, bz = [w[:, 2048 + i * 512:2048 + (i + 1) * 512] for i in range(4)]
    d = {}
    d["x"] = np.ascontiguousarray(inp["x"][b, :S_len])
    d["lncol"] = np.ascontiguousarray(inp["ln_e"][0].reshape(8, 128).T)
    for pi in range(2):
        c0 = (4 * half + 2 * pi) * 64
        d["wA%d" % pi] = np.ascontiguousarray(np.concatenate([t[:, c0:c0 + 128] for t in (aq, ak, av, az)], axis=1))
        c0 = (2 * half + pi) * 128
        d["wB%d" % pi] = np.ascontiguousarray(np.concatenate([t[:, c0:c0 + 128] for t in (bq, bk, bv, bz)], axis=1))
    q = inp["qkn_e"][0]
    d["gA"] = np.concatenate([q[0], q[0], q[1], q[1]])[None, :].copy()
    d["gB"] = np.concatenate([q[2], q[2], q[3], q[3]])[None, :].copy()
    d["lam"] = inp["lam_e"][0].reshape(1, 256).copy()
    d["subln"] = inp["subln_e"][0].reshape(1, 128).copy()
    cst, _ = consts_L0(half, S_len)
    d.update(cst)
    return d


def host_inputs_L1(inp, xb, mixed0_bf, half, S_len):
    w = inp["w_in_o"][0]
    off = {}
    o = 0
    for name, n in zip(("cq", "ck", "cv", "cz", "dq", "dkc", "dvc", "dks", "dvs", "dkw", "dvw", "dz", "dg"),
                       (512, 512, 512, 512, 512, 128, 128, 128, 128, 128, 128, 512, 24)):
        off[name] = o
        o += n
    col = lambda name, a, n: w[:, off[name] + a:off[name] + a + n]
    d = {}
    d["x"] = np.ascontiguousarray(xb)
    d["m0T"] = np.ascontiguousarray(mixed0_bf.T)
    d["woe"] = np.ascontiguousarray(inp["w_out_e"][0])
    d["lncol"] = np.ascontiguousarray(inp["ln_o"][0].reshape(8, 128).T)
    for pi in range(2):
        c0 = (4 * half + 2 * pi) * 64
        d["wC%d" % pi] = np.ascontiguousarray(np.concatenate([col(nm, c0, 128) for nm in ("cq", "ck", "cv", "cz")], axis=1))
    g = half
    q0 = 4 * half * 64
    ks, kw = col("dks", g * 64, 64), col("dkw", g * 64, 64)
    d["wD"] = np.ascontiguousarray(np.concatenate([col("dq", q0, 256), ks, ks, kw, kw,
                                                   col("dkc", g * 64, 64), col("dvc", g * 64, 64), col("dvs", g * 64, 64), col("dvw", g * 64, 64),
                                                   col("dz", q0, 256), col("dg", 4 * half * 3, 12)], axis=1))
    q = inp["qkn_o"][0]
    d["gC"] = np.concatenate([q[0], q[0], q[1], q[1]])[None, :].copy()
    d["gD"] = np.concatenate([q[2]] * 4 + [q[4]] * 2 + [q[5]] * 2)[None, :].copy()
    w1 = inp["phi_w1"][0]
    d["W1cat"] = np.ascontiguousarray(np.concatenate([w1[s].reshape(32, 64, 128).transpose(1, 0, 2) for s in range(2)], axis=0))
    w2 = inp["phi_w2"][0]
    d["W2cat"] = np.ascontiguousarray(np.concatenate([w2[0], w2[0], w2[1]], axis=1))
    pe = inp["phi_pe"][0]
    d["peT"] = np.ascontiguousarray(np.concatenate([pe[0].T, pe[1].T], axis=0))
    d["gk3col"] = np.concatenate([q[3], q[3]])[:, None].copy()
    a = np.arange(128)
    d["BO"] = (a[:, None] // 64 == a[None, :] // 64).astype(BF)
    cst, _, _ = consts_L1(half, S_len)
    d.update(cst)
    return d


BF = ml_dtypes.bfloat16
SEQ = 8192
NB_ = 4


def kernel(**inputs):
    inp = {k: np.asarray(v) for k, v in inputs.items()}
    S_len = SEQ
    cores = list(range(8))
    nc0 = build_L0(S_len)
    maps = [host_inputs_L0(inp, core // 2, core % 2, S_len) for core in cores]
    res0 = run_bass_kernel_spmd(nc0, maps, core_ids=cores)
    mixed0 = np.empty((NB_, S_len, 1024), BF)
    for core in cores:
        b, half = core // 2, core % 2
        o = res0.results[core]["mixed"]
        mixed0[b][:, 256 * half:256 * half + 256] = o[:, 0:256]
        mixed0[b][:, 512 + 256 * half:512 + 256 * half + 256] = o[:, 256:512]
    del res0, maps
    nc1 = build_L1(S_len)
    maps = [host_inputs_L1(inp, inp["x"][core // 2], mixed0[core // 2], core % 2, S_len) for core in cores]
    res1 = run_bass_kernel_spmd(nc1, maps, core_ids=cores)
    mixed1 = np.empty((NB_, S_len, 1024), BF)
    x1 = []
    for core in cores:
        b, half = core // 2, core % 2
        o = res1.results[core]["mixed"]
        mixed1[b][:, 256 * half:256 * half + 256] = o[:, 0:256]
        mixed1[b][:, 512 + 256 * half:512 + 256 * half + 256] = o[:, 256:512]
        if half == 0:
            x1.append(res1.results[core]["x1"])
    del res1, maps
    ntok = S_len // 2
    nc2 = build_L2(ntok)
    maps = []
    for core in cores:
        b, half = core // 2, core % 2
        sl = slice(half * ntok, (half + 1) * ntok)
        maps.append({"x1": np.ascontiguousarray(x1[b][sl]), "m1T": np.ascontiguousarray(mixed1[b][sl].T),
                     "woo": np.ascontiguousarray(inp["w_out_o"][0])})
    res2 = run_bass_kernel_spmd(nc2, maps, core_ids=cores)
    out = np.empty((NB_, S_len, 1024), np.float32)
    for core in cores:
        b, half = core // 2, core % 2
        out[b, half * ntok:(half + 1) * ntok] = res2.results[core]["out"]
    return out
```

```python
import contextlib
import math
import numpy as np
import ml_dtypes
import concourse.bass as bass
import concourse.mybir as mybir
from concourse.bass_utils import run_bass_kernel_spmd

F32 = mybir.dt.float32
BF16 = mybir.dt.bfloat16
ALU = mybir.AluOpType
AF = mybir.ActivationFunctionType
AX = mybir.AxisListType


import os as _os
SAME_ENGINE_SYNC = _os.environ.get('SAME_SYNC', '1') == '1'


class Op:
    __slots__ = ("eng", "fn", "deps", "signal", "sem", "val", "dma", "semkey", "gi")


class Sched:
    ENG = ["pe", "act", "dve", "pool", "sp"]

    def __init__(self, nc):
        self.nc = nc
        self.ops = {e: [] for e in self.ENG}
        self.lastw = {}
        self.readers = {}
        self.n = 0

    def add(self, eng, fn, reads=(), writes=(), dma=False, semkey=None):
        op = Op()
        op.eng, op.fn, op.dma = eng, fn, dma
        op.signal = dma
        op.sem = None
        op.val = 0
        op.gi = self.n
        self.n += 1
        if dma:
            op.semkey = semkey if semkey is not None else ("dma", writes[0])
        else:
            op.semkey = None
        excl = [k for k in reads if isinstance(k, tuple) and k[0] == "bank"]
        if excl:
            reads = [k for k in reads if k not in excl]
            writes = list(writes) + [k for k in excl if k not in writes]
        deps = set()
        for k in reads:
            w = self.lastw.get(k)
            if w is not None:
                deps.add(w)
        for k in writes:
            w = self.lastw.get(k)
            if w is not None:
                deps.add(w)
            for r in self.readers.get(k, ()):
                deps.add(r)
        deps.discard(op)
        if SAME_ENGINE_SYNC:
            op.deps = [d for d in deps if not (d.eng == "pe" and eng == "pe" and not d.dma)]
        else:
            op.deps = [d for d in deps if d.dma or dma or d.eng != eng]
        for d in op.deps:
            d.signal = True
        for k in reads:
            self.readers.setdefault(k, []).append(op)
        for k in writes:
            self.lastw[k] = op
            self.readers[k] = []
        self.ops[eng].append(op)
        return op

    def emit(self, final_wait_ops=()):
        nc = self.nc
        import contextlib
        with contextlib.ExitStack() as st:
            esem = {e: st.enter_context(nc.semaphore("s_" + e)) for e in self.ENG}
            dsem = {}
            for e in self.ENG:
                if self.ops[e]:
                    self.ops[e][-1].signal = True
            for e in self.ENG:
                cnt = 0
                for op in self.ops[e]:
                    if op.dma:
                        if op.semkey not in dsem:
                            dsem[op.semkey] = [st.enter_context(nc.semaphore("d%d" % len(dsem))), 0]
                        ent = dsem[op.semkey]
                        ent[1] += 16
                        op.sem, op.val = ent[0], ent[1]
                    elif op.signal:
                        cnt += 1
                        op.sem, op.val = esem[e], cnt
            self.nsem = len(dsem) + 5
            block = st.enter_context(nc.Block())
            engobj = {"pe": block.tensor, "act": block.scalar, "dve": block.vector,
                      "pool": block.gpsimd, "sp": block.sync}

            def run_engine(e, extra_wait):
                def body(eng):
                    waited = {}
                    for op in self.ops[e]:
                        need = {}
                        for d in op.deps:
                            k = id(d.sem)
                            if k not in need or need[k][1] < d.val:
                                need[k] = (d.sem, d.val)
                        for k, (sem, val) in need.items():
                            if waited.get(k, 0) < val:
                                eng.wait_ge(sem, val)
                                waited[k] = val
                        ins = op.fn(eng)
                        if op.dma:
                            ins.then_inc(op.sem, 16)
                        elif op.signal:
                            ins.then_inc(op.sem, 1)
                    if e == "sp":
                        for k, ent in dsem.items():
                            eng.wait_ge(ent[0], ent[1])
                        for e2 in self.ENG:
                            if e2 != "sp" and self.ops[e2]:
                                lo = self.ops[e2][-1]
                                if not lo.dma:
                                    eng.wait_ge(lo.sem, lo.val)
                return body

            for e in self.ENG:
                extra = final_wait_ops if e == "sp" else ()
                engobj[e](run_engine(e, extra))


def I(name, *a, **k):
    return lambda e: getattr(e, name)(*a, **k)


NEG = -30000.0
EPS = 1e-6
D = 1024


class Ctx:
    def __init__(self, nc, st):
        self.nc, self.st = nc, st
        self.S = Sched(nc)
        self.uid = 0

    def sb(self, name, shape, dt):
        return self.st.enter_context(self.nc.sbuf_tensor(name, shape, dt))

    def ps(self, name, shape, dt):
        return self.st.enter_context(self.nc.psum_tensor(name, shape, dt))

    def din(self, name, shape, dt):
        return self.nc.dram_tensor(name, list(shape), dt, kind="ExternalInput").ap()

    def dout(self, name, shape, dt):
        return self.nc.dram_tensor(name, list(shape), dt, kind="ExternalOutput").ap()


def alloc_common(c, qkw=256, bkw=256, zw=128):
    c.bank = [c.ps("bank%d" % i, [128, 512], F32) for i in range(8)]
    c.ident = c.sb("ident", [128, 128], BF16)
    S = c.S
    S.add("pool", I("memset", c.ident[:], 1.0), writes=["ident"])
    S.add("pool", I("affine_select", out=c.ident[:], in_=c.ident[:], pattern=[[-1, 128]],
                                            compare_op=ALU.is_equal, fill=0.0, base=0, channel_multiplier=1),
          reads=["ident"], writes=["ident"])
    c.epsb = c.sb("epsb", [128, 1], F32)
    S.add("pool", I("memset", c.epsb[:], EPS), writes=["epsb"])
    c.xt = [c.sb("xt%d" % i, [128, D], F32) for i in range(2)]
    c.xsq_ = [c.sb("xsq%d" % i, [128, D], BF16) for i in range(2)]
    c.xn_ = [c.sb("xn%d" % i, [128, D], BF16) for i in range(2)]
    c.hT = [c.sb("hT%d" % i, [128, D], BF16) for i in range(2)]
    c.st1 = [c.sb("st1_%d" % i, [128, 4], F32) for i in range(2)]
    c.qk32_ = [c.sb("qk32_%d" % i, [128, qkw], F32) for i in range(2)]
    c.qksq_ = [c.sb("qksq_%d" % i, [128, qkw], F32) for i in range(2)]
    c.qkb_ = [c.sb("qkb_%d" % i, [128, bkw], BF16) for i in range(2)]
    c.st8 = [c.sb("st8_%d" % i, [128, 8], F32) for i in range(2)]
    c.ze_ = [c.sb("ze_%d" % i, [128, zw], F32) for i in range(2)]


def load_weights(c, w_dram, lncol_dram, Wb, tag):
    S = c.S
    ncols = w_dram.shape[1]
    lncol = c.sb("lncol" + tag, [128, 8], F32)
    S.add("sp", I("dma_start", out=lncol[:], in_=lncol_dram), writes=["lncol" + tag], dma=True)
    n = 0
    for kc in range(8):
        for c0 in range(0, ncols, 1024):
            c1 = min(ncols, c0 + 1024)
            i = n % 2
            n += 1
            stg = c.xt[i]
            S.add("sp", I("dma_start", out=stg[:, 0:c1 - c0], in_=w_dram[kc * 128:(kc + 1) * 128, c0:c1]),
                  writes=["xt%d" % i], dma=True)
            S.add("dve", I("tensor_scalar", out=Wb[:, kc, c0:c1], in0=stg[:, 0:c1 - c0], scalar1=lncol[:, kc:kc + 1],
                           scalar2=None, op0=ALU.mult),
                  reads=["xt%d" % i, "lncol" + tag], writes=[("Wb", kc)])


def rstd_from_sumsq(c, S, st, n, ncol, key):
    S.add("dve", I("tensor_scalar", out=st[:, 0:ncol], in0=st[:, 0:ncol], scalar1=1.0 / n, scalar2=EPS,
                                           op0=ALU.mult, op1=ALU.add), reads=[key], writes=[key])
    S.add("act", I("activation", out=st[:, 0:ncol], in_=st[:, 0:ncol], func=AF.Ln), reads=[key], writes=[key])
    S.add("act", I("activation", out=st[:, 0:ncol], in_=st[:, 0:ncol], func=AF.Exp, scale=-0.5), reads=[key], writes=[key])


def emit_hT(c, x_dram, t, xreads=()):
    S = c.S
    i = t % 2
    xt, hT, st1 = c.xt[i], c.hT[i], c.st1[i]
    xsq, xn, xnk, xsk = c.xsq_[i], c.xn_[i], "xn%d" % i, "xsq%d" % i
    S.add("sp", I("dma_start", out=xt[:], in_=x_dram[t * 128:(t + 1) * 128, :]), reads=list(xreads), writes=["xt%d" % i], dma=True)
    S.add("act", I("activation", out=xsq[:], in_=xt[:], func=AF.Square, accum_out=st1[:, 0:1]),
          reads=["xt%d" % i], writes=[xsk, "st1_%d" % i])
    rstd_from_sumsq(c, S, st1, D, 1, "st1_%d" % i)
    S.add("dve", I("tensor_scalar", out=xn[:], in0=xt[:], scalar1=st1[:, 0:1], scalar2=None, op0=ALU.mult),
          reads=["xt%d" % i, "st1_%d" % i], writes=[xnk])
    pb = c.bank[i].bitcast(BF16)
    for kc in range(8):
        S.add("pe", I("transpose", out=pb[:, kc * 128:(kc + 1) * 128], in_=xn[:, kc * 128:(kc + 1) * 128],
                                                 identity=c.ident[:]),
              reads=[xnk, "ident"], writes=[("bank", i)])
    S.add("act", I("copy", out=hT[:], in_=pb[:, :]), reads=[("bank", i)], writes=["hT%d" % i])


def proj_tile(c, t, Wb, tag, Gqk, QT, KT, v_evac, zs, z_dram=None, kq="QT", kk="KT", gkey="Gqk", stage=0):
    S = c.S
    i = t % 2
    hT = c.hT[i]
    pb = c.bank[2 + i]
    qk32, qksq, qkb, ze = c.qk32_[i][:, 0:256], c.qksq_[i][:, 0:256], c.qkb_[i][:, 0:256], c.ze_[i][:, 0:128]
    k32, ksq, kkb, kze = "qk32_%d" % i, "qksq_%d" % i, "qkb_%d" % i, "ze_%d" % i
    bk = ("bank", 2 + i)
    st8 = c.st8[i]
    if stage != 2:
        for kc in range(8):
            S.add("pe", I("matmul", pb[:, :], lhsT=hT[:, kc * 128:(kc + 1) * 128], rhs=Wb[:, kc, :],
                                                  start=(kc == 0), stop=(kc == 7)),
                  reads=["hT%d" % i] + [("Wb", kc)], writes=[("bank", 2 + i)])
        S.add("act", I("copy", out=qk32[:], in_=pb[:, 0:256]), reads=[bk], writes=[k32])
        S.add("dve", I("tensor_tensor", out=qksq[:], in0=qk32[:], in1=qk32[:], op=ALU.mult), reads=[k32], writes=[ksq])
        S.add("dve", I("tensor_reduce", out=st8[:, 0:4], in_=qksq[:].rearrange("p (g d) -> p g d", d=64), axis=AX.X, op=ALU.add),
              reads=[ksq], writes=["st8_%d" % i])
        rstd_from_sumsq(c, S, st8, 64, 4, "st8_%d" % i)
    if stage == 1:
        return
    for g in range(4):
        S.add("dve", I("scalar_tensor_tensor", out=qkb[:, g * 64:(g + 1) * 64], in0=qk32[:, g * 64:(g + 1) * 64],
                                                           scalar=st8[:, g:g + 1], in1=Gqk[:, g * 64:(g + 1) * 64],
                                                           op0=ALU.mult, op1=ALU.mult),
              reads=[k32, "st8_%d" % i, gkey], writes=[kkb])
    tb = c.bank[4 + i].bitcast(BF16)
    S.add("pe", I("transpose", out=tb[:, 0:128], in_=qkb[:, 0:128], identity=c.ident[:]), reads=[kkb, "ident"], writes=[("bank", 4 + i)])
    S.add("pe", I("transpose", out=tb[:, 128:256], in_=qkb[:, 128:256], identity=c.ident[:]), reads=[kkb, "ident"], writes=[("bank", 4 + i)])
    S.add("act", I("copy", out=QT[:, t * 128:(t + 1) * 128], in_=tb[:, 0:128]), reads=[("bank", 4 + i)], writes=[(kq, t)])
    S.add("dve", I("tensor_copy", out=KT[:, t * 128:(t + 1) * 128], in_=tb[:, 128:256]), reads=[("bank", 4 + i)], writes=[(kk, t)])
    v_evac(t, pb, bk)
    S.add("act", I("activation", out=ze[:], in_=pb[:, 384:512], func=AF.Exp, scale=-1.0), reads=[bk], writes=[kze])
    S.add("dve", I("tensor_scalar", out=ze[:], in0=ze[:], scalar1=1.0, scalar2=None, op0=ALU.add), reads=[kze], writes=[kze])
    S.add("dve", I("reciprocal", out=ze[:], in_=ze[:]), reads=[kze], writes=[kze])
    if z_dram is None:
        S.add("dve", I("tensor_tensor", out=zs[:, t, :], in0=ze[:], in1=pb[:, 384:512], op=ALU.mult), reads=[kze, bk], writes=[("zs", t)])
    else:
        zt = c.zt[i]
        S.add("dve", I("tensor_tensor", out=zt[:, 0:128], in0=ze[:], in1=pb[:, 384:512], op=ALU.mult), reads=[kze, bk], writes=["zt%d" % i])
        S.add("sp", I("dma_start", out=z_dram[t * 128:(t + 1) * 128, 0:128], in_=zt[:, 0:128]), reads=["zt%d" % i], writes=[("zsd", t)], dma=True,
              semkey=("dma", "zt%d" % i))


def attn_A(c, QT, KT, Vaug, zs, maskA, biasA, boff, qws, out_dram, col0, S_len):
    S = c.S
    NU = S_len // 512
    ss = SweepState()
    ocnt = 0
    for U in range(NU):
        mixt = c.mixt[U % 2]
        mk = "mixt%d" % (U % 2)
        for hl in range(2):
            r0 = hl * 64
            Jlo, Jhi = max(0, 4 * U - 16), 4 * U + 3
            oi = 6 + (ocnt % 2)
            ocnt += 1
            ob = c.bank[oi]
            qw = qws[hl]
            nsub = 512 // qw
            qreads = [("QT", 4 * U + j) for j in range(4)]

            def extra(J, U=U):
                return [(c.ident[:], maskA[:, J - (4 * U - 16), :], ["ident", "maskA"])]

            def biasf(J, i, U=U, hl=hl, nsub=nsub):
                bc = boff[hl] + (J - 4 * U + 16) * nsub + i
                return biasA[:, bc:bc + 1], "biasA"

            sweep(c, ss, QT[r0:r0 + 64, U * 512:(U + 1) * 512], qreads, list(range(Jlo, Jhi + 1)),
                  lambda J, r0=r0: KT[r0:r0 + 64, J * 128:(J + 1) * 128], lambda J: [("KT", J)], extra, biasf, qw,
                  lambda J, hl=hl: Vaug[:, J, hl, :], lambda J: [("V", J)], [(oi, j * 65) for j in range(4)], 65)
            rl = c.rl[ocnt % 2]
            rk = "rl%d" % (ocnt % 2)
            S.add("dve", I("reciprocal", out=rl[:, 0:4], in_=ob[:, 0:260].rearrange("p (j d) -> p j d", d=65)[:, :, 64]),
                  reads=[("bank", oi)], writes=[rk])
            for j in range(4):
                S.add("dve", I("scalar_tensor_tensor", out=mixt[:, j, r0:r0 + 64], in0=ob[:, j * 65:j * 65 + 64], scalar=rl[:, j:j + 1],
                               in1=zs[:, 4 * U + j, r0:r0 + 64], op0=ALU.mult, op1=ALU.mult),
                      reads=[("bank", oi), rk, ("zs", 4 * U + j)], writes=[mk])
        S.add("sp", I("dma_start", out=out_dram[U * 512:(U + 1) * 512, col0:col0 + 128].rearrange("(j p) c -> p j c", p=128), in_=mixt[:, :, :]),
              reads=[mk], writes=[("out", col0, U)], dma=True, semkey=("dma", mk))


def attn_B(c, QT, KT, Vb, zs, maskC, biasB, boff, neglam, subG, out_dram, col0, S_len):
    S = c.S
    NU = S_len // 512
    ss = SweepState()
    for U in range(NU):
        mixt = c.mixt[U % 2]
        mk = "mixt%d" % (U % 2)
        for cc in range(2):
            r0 = cc * 64
            Jhi = 4 * U + 3
            obs = [c.bank[3 + 2 * cc], c.bank[4 + 2 * cc]]
            oks = [("bank", 3 + 2 * cc), ("bank", 4 + 2 * cc)]
            qreads = [("QT", 4 * U + j) for j in range(4)]

            def extra(J, U=U):
                d = J - 4 * U
                if d >= 0:
                    return [(c.ident[:], maskC[:, d, :], ["ident", "maskC"])]
                return []

            def biasf(J, i, U=U):
                bc = boff + (J - 4 * U + 4 * (NU - 1))
                return biasB[:, bc:bc + 1], "biasB"

            sweep(c, ss, QT[r0:r0 + 64, U * 512:(U + 1) * 512], qreads, list(range(0, Jhi + 1)),
                  lambda J, r0=r0: KT[r0:r0 + 64, J * 128:(J + 1) * 128], lambda J: [("KT", J)], extra, biasf, 512,
                  lambda J: Vb[:, J, :], lambda J: [("V", J)], [(3 + 2 * cc + j // 2, (j % 2) * 129) for j in range(4)], 129,
                  skip=lambda J, j, U=U: (J - 4 * U) > j)
            oc = c.oc[cc]
            rl = c.rl[cc]
            for hb in range(2):
                S.add("dve", I("reciprocal", out=rl[:, 2 * hb:2 * hb + 2],
                                                                in_=obs[hb][:, 0:258].rearrange("p (j d) -> p j d", d=129)[:, :, 128]),
                      reads=[oks[hb]], writes=["rl%d" % cc])
            for j in range(4):
                ob = obs[j // 2]
                o0 = (j % 2) * 129
                S.add("dve", I("tensor_scalar", out=oc[:, j, :], in0=ob[:, o0:o0 + 128], scalar1=rl[:, j:j + 1],
                                                                                   scalar2=None, op0=ALU.mult),
                      reads=[oks[j // 2], "rl%d" % cc], writes=["oc%d" % cc])
        df = c.df
        S.add("dve", I("scalar_tensor_tensor", out=df[:, :, :], in0=c.oc[1][:, :, :], scalar=neglam[:, 0:1], in1=c.oc[0][:, :, :],
                                                      op0=ALU.mult, op1=ALU.add), reads=["oc0", "oc1", "neglam"], writes=["df"])
        S.add("pool", I("tensor_tensor", out=c.dsq[:, :, :], in0=df[:, :, :], in1=df[:, :, :], op=ALU.mult), reads=["df"], writes=["dsq"])
        st = c.st4
        S.add("dve", I("tensor_reduce", out=st[:, 0:4], in_=c.dsq[:, :, :], axis=AX.X, op=ALU.add), reads=["dsq"], writes=["st4"])
        rstd_from_sumsq(c, S, st, 128, 4, "st4")
        for j in range(4):
            S.add("dve", I("scalar_tensor_tensor", out=df[:, j, :], in0=df[:, j, :], scalar=st[:, j:j + 1], in1=subG[:, :],
                                                               op0=ALU.mult, op1=ALU.mult), reads=["df", "st4", "subG"], writes=["df"])
        S.add("dve", I("tensor_tensor", out=mixt[:, :, :], in0=df[:, :, :], in1=zs[:, 4 * U:4 * U + 4, :], op=ALU.mult),
              reads=["df"] + [("zs", 4 * U + j) for j in range(4)], writes=[mk])
        S.add("sp", I("dma_start",
            out=out_dram[U * 512:(U + 1) * 512, col0:col0 + 128].rearrange("(j p) c -> p j c", p=128), in_=mixt[:, :, :]),
            reads=[mk], writes=[("out", col0, U)], dma=True, semkey=("dma", mk))


def slopes(n):
    return [2.0 ** (-8.0 * (i + 1) / n) for i in range(n)]


QWS_A = [128, 256, 512, 512]


def consts_L0(half, S_len):
    NU = S_len // 512
    k = np.arange(128)[:, None]
    q = np.arange(512)[None, :]
    maskA = np.zeros((128, 20, 512), np.float32)
    for r in range(20):
        dl = q - k + 2048 - 128 * r
        cnt = ((dl >= 0) & (dl <= 128)).astype(np.float32) + ((dl >= 0) & (dl % 4 == 0) & (dl <= 512)) + ((dl >= 0) & (dl % 16 == 0) & (dl <= 2048))
        maskA[:, r, :] = np.where(cnt > 0, np.log(np.maximum(cnt, 1)), NEG)
    maskC = np.zeros((128, 4, 512), np.float32)
    for d in range(4):
        maskC[:, d, :] = np.where(128 * d + k <= q, 0.0, NEG)
    sA = slopes(8)[4 * half:4 * half + 4]
    cols = []
    boffA = []
    kk = np.arange(128, dtype=np.float64)
    for hl in range(4):
        qw = QWS_A[hl]
        nsub = 512 // qw
        boffA.append(len(cols))
        for dd in range(-16, 4):
            for i in range(nsub):
                cols.append(sA[hl] * (kk + 128 * dd - i * qw - qw / 2))
    biasA = np.stack(cols, axis=1).astype(np.float32)
    sB = slopes(4)[2 * half:2 * half + 2]
    cols = []
    for hb in range(2):
        for dd in range(-4 * (NU - 1), 4):
            cols.append(sB[hb] * (kk + 128 * dd - 256))
    biasB = np.stack(cols, axis=1).astype(np.float32)
    return dict(maskA=maskA.astype(ml_dtypes.bfloat16), maskC=maskC.astype(ml_dtypes.bfloat16), biasA=biasA, biasB=biasB), boffA


def build_L0(S_len, debug=False):
    NT = S_len // 128
    NU = S_len // 512
    nc = bass.Bass("TRN2", target_bir_lowering=False)
    st = contextlib.ExitStack()
    with st:
        c = Ctx(nc, st)
        S = c.S
        x = c.din("x", [S_len, D], F32)
        lncol = c.din("lncol", [128, 8], F32)
        wA = [c.din("wA%d" % i, [D, 512], F32) for i in range(2)]
        wB = [c.din("wB%d" % i, [D, 512], F32) for i in range(2)]
        gA = c.din("gA", [1, 256], F32)
        gB = c.din("gB", [1, 256], F32)
        lam = c.din("lam", [1, 256], F32)
        subln = c.din("subln", [1, 128], F32)
        maskA_d = c.din("maskA", [128, 20, 512], BF16)
        maskC_d = c.din("maskC", [128, 4, 512], BF16)
        nbA = sum(20 * (512 // q) for q in QWS_A)
        biasA_d = c.din("biasA", [128, nbA], F32)
        biasB_d = c.din("biasB", [128, 2 * 4 * NU], F32)
        out = c.dout("mixed", [S_len, 512], BF16)
        alloc_common(c)
        Wb = c.sb("Wb", [128, 8, 512], BF16)
        QT = c.sb("QT", [128, S_len], BF16)
        KT = c.sb("KT", [128, S_len], BF16)
        Vsb = c.sb("Vsb", [128, NT * 130], BF16)
        zs = c.sb("zs", [128, NT, 128], BF16)
        maskA = c.sb("maskA_s", [128, 20, 512], BF16)
        maskC = c.sb("maskC_s", [128, 4, 512], BF16)
        biasA = c.sb("biasA_s", [128, nbA], F32)
        biasB = c.sb("biasB_s", [128, 2 * 4 * NU], F32)
        GA = c.sb("GA", [128, 256], F32)
        GB = c.sb("GB", [128, 256], F32)
        lamt = c.sb("lamt", [128, 256], F32)
        lamp = c.sb("lamp", [128, 128], F32)
        lam2 = c.sb("lam2", [128, 2], F32)
        neglam = c.sb("neglam", [128, 1], F32)
        subG = c.sb("subG", [128, 128], F32)
        c.pt = [c.sb("pt%d" % i, [128, 512], BF16) for i in range(4)]
        c.mixt = [c.sb("mixt%d" % i, [128, 4, 128], BF16) for i in range(2)]
        c.rl = [c.sb("rl%d" % i, [128, 4], F32) for i in range(2)]
        c.oc = [c.sb("oc%d" % i, [128, 4, 128], F32) for i in range(2)]
        c.df = c.sb("df", [128, 4, 128], F32)
        c.dsq = c.sb("dsq", [128, 4, 128], F32)
        c.st4 = c.sb("st4", [128, 4], F32)
        S.add("sp", I("dma_start", out=maskA[:], in_=maskA_d), writes=["maskA"], dma=True)
        S.add("sp", I("dma_start", out=maskC[:], in_=maskC_d), writes=["maskC"], dma=True)
        S.add("sp", I("dma_start", out=biasA[:], in_=biasA_d), writes=["biasA"], dma=True)
        S.add("sp", I("dma_start", out=biasB[:], in_=biasB_d), writes=["biasB"], dma=True)
        S.add("sp", I("dma_start", out=GA[:], in_=gA.partition_broadcast(128)), writes=["GA"], dma=True)
        S.add("sp", I("dma_start", out=GB[:], in_=gB.partition_broadcast(128)), writes=["GB"], dma=True)
        S.add("sp", I("dma_start", out=lamt[:], in_=lam.partition_broadcast(128)), writes=["lamt"], dma=True)
        S.add("sp", I("dma_start", out=subG[:], in_=subln.partition_broadcast(128)), writes=["subG"], dma=True)
        S.add("dve", I("tensor_scalar", out=GA[:, 0:128], in0=GA[:, 0:128], scalar1=0.125, scalar2=None, op0=ALU.mult), reads=["GA"], writes=["GA"])
        S.add("dve", I("tensor_scalar", out=GB[:, 0:128], in0=GB[:, 0:128], scalar1=0.125, scalar2=None, op0=ALU.mult), reads=["GB"], writes=["GB"])
        lam_init = 0.8 - 0.6 * math.exp(-0.3 * 0)
        S.add("dve", I("tensor_scalar", out=subG[:], in0=subG[:], scalar1=1.0 - lam_init, scalar2=None, op0=ALU.mult), reads=["subG"], writes=["subG"])
        lv = lamt[:, :].rearrange("p (a b d) -> p a b d", a=2, b=2)
        S.add("dve", I("tensor_tensor", out=lamp[:, :].rearrange("p (a d) -> p a d", a=2), in0=lv[:, :, 0, :], in1=lv[:, :, 1, :], op=ALU.mult),
              reads=["lamt"], writes=["lamp"])
        S.add("dve", I("tensor_reduce", out=lam2[:, 0:2], in_=lamp[:, :].rearrange("p (a d) -> p a d", a=2), axis=AX.X, op=ALU.add),
              reads=["lamp"], writes=["lam2"])
        S.add("act", I("activation", out=lam2[:, 0:2], in_=lam2[:, 0:2], func=AF.Exp), reads=["lam2"], writes=["lam2"])
        S.add("dve", I("tensor_tensor", out=neglam[:, 0:1], in0=lam2[:, 1:2], in1=lam2[:, 0:1], op=ALU.subtract), reads=["lam2"], writes=["neglam"])
        S.add("dve", I("tensor_scalar", out=neglam[:, 0:1], in0=neglam[:, 0:1], scalar1=-lam_init, scalar2=None, op0=ALU.add), reads=["neglam"], writes=["neglam"])

        boffA = []
        o = 0
        for hl in range(4):
            boffA.append(o)
            o += 20 * (512 // QWS_A[hl])

        passes = [("A", 0), ("A", 1), ("B", 0), ("B", 1)]
        for kind, pi in passes:
            tag = "%s%d" % (kind, pi)
            load_weights(c, (wA if kind == "A" else wB)[pi], lncol, Wb, tag)
            G = GA if kind == "A" else GB
            Gk = "GA" if kind == "A" else "GB"
            if kind == "A":
                Vv = Vsb[:, 0:NT * 130].rearrange("p (t h d) -> p t h d", h=2, d=65)
                S.add("pool", I("memset", Vv[:, :, :, 64:65], 1.0), reads=[("V", t) for t in range(NT)], writes=[("V", t) for t in range(NT)])

                def v_evac(t, pb, bk, Vv=Vv):
                    S.add("act", I("copy", out=Vv[:, t, :, 0:64], in_=pb[:, 256:384].rearrange("p (h d) -> p h d", h=2)),
                          reads=[bk], writes=[("V", t)])
            else:
                Vv = Vsb[:, 0:NT * 129].rearrange("p (t d) -> p t d", d=129)
                S.add("pool", I("memset", Vv[:, :, 128:129], 1.0), reads=[("V", t) for t in range(NT)], writes=[("V", t) for t in range(NT)])

                def v_evac(t, pb, bk, Vv=Vv):
                    S.add("act", I("copy", out=Vv[:, t, 0:128], in_=pb[:, 256:384]), reads=[bk], writes=[("V", t)])
            emit_hT(c, x, 0)
            emit_hT(c, x, 1)
            proj_tile(c, 0, Wb, tag, G, QT, KT, v_evac, zs, gkey=Gk, stage=1)
            for t in range(NT):
                if t + 2 < NT:
                    emit_hT(c, x, t + 2)
                if t + 1 < NT:
                    proj_tile(c, t + 1, Wb, tag, G, QT, KT, v_evac, zs, gkey=Gk, stage=1)
                proj_tile(c, t, Wb, tag, G, QT, KT, v_evac, zs, gkey=Gk, stage=2)
            if kind == "A":
                attn_A(c, QT, KT, Vv, zs, maskA, biasA, boffA[2 * pi:2 * pi + 2], QWS_A[2 * pi:2 * pi + 2], out, 128 * pi, S_len)
            else:
                attn_B(c, QT, KT, Vv, zs, maskC, biasB, pi * 4 * NU, neglam, subG, out, 256 + 128 * pi, S_len)
        S.emit()
    return nc


class SweepState:
    def __init__(self):
        self.cnt = 0


def sweep(c, ss, Qap, qreads, Js, Kap, kreads, extra, biasf, qw, Vap, vreads, subs, dv, skip=None, sbanks=(0, 1, 2)):
    S = c.S
    nsub = 512 // qw
    started = set()
    lastJ = {}
    for J in Js:
        for j in range(4):
            if not (skip and skip(J, j)):
                lastJ[j] = J
    slot = {}

    def qk(J):
        si = sbanks[ss.cnt % len(sbanks)]
        pi = ss.cnt % 4
        ss.cnt += 1
        slot[J] = (si, pi)
        sbk = c.bank[si]
        ex = extra(J)
        S.add("pe", I("matmul", sbk[:, :], lhsT=Kap(J), rhs=Qap, start=True, stop=(len(ex) == 0)),
              reads=list(kreads(J)) + list(qreads), writes=[("bank", si)])
        for n, (l_, r_, rd) in enumerate(ex):
            S.add("pe", I("matmul", sbk[:, :], lhsT=l_, rhs=r_, start=False, stop=(n == len(ex) - 1)), reads=list(rd), writes=[("bank", si)])

    def act(J):
        si, pi = slot[J]
        sbk, pt = c.bank[si], c.pt[pi]
        for i in range(nsub):
            bap, bkey = biasf(J, i)
            S.add("act", I("activation", out=pt[:, i * qw:(i + 1) * qw], in_=sbk[:, i * qw:(i + 1) * qw], func=AF.Exp, bias=bap),
                  reads=[("bank", si), bkey], writes=[("pt", pi)])

    def av(J):
        si, pi = slot[J]
        pt = c.pt[pi]
        for j in range(4):
            if skip and skip(J, j):
                continue
            bi, o0 = subs[j]
            S.add("pe", I("matmul", c.bank[bi][:, o0:o0 + dv], lhsT=pt[:, j * 128:(j + 1) * 128], rhs=Vap(J),
                          start=(bi not in started), stop=(J == lastJ[j]), skip_group_check=True),
                  reads=[("pt", pi)] + list(vreads(J)), writes=[("bank", bi)])
            started.add(bi)

    for J in Js[0:2]:
        qk(J)
    for n, J in enumerate(Js):
        if n + 2 < len(Js):
            qk(Js[n + 2])
        act(J)
        av(J)


QWS = [128, 256, 512, 512]


def bias_tables_L1(half, NU):
    sl = slopes(8)[4 * half:4 * half + 4]
    kk = np.arange(128, dtype=np.float64)
    cols, offs = [], []
    for h in range(4):
        qw = QWS[h]
        offs.append(len(cols))
        for dd in range(-4 * (NU - 1), 4):
            for i in range(512 // qw):
                cols.append(sl[h] * (kk + 128 * dd - i * qw - qw / 2))
    bias = np.stack(cols, 1).astype(np.float32)
    cols, offc = [], []
    for h in range(4):
        qw = QWS[h]
        offc.append(len(cols))
        for e in range(NU):
            for i in range(512 // qw):
                cols.append(sl[h] * (16 * kk + 31 - 512 * e - i * qw - qw / 2))
    biasc = np.stack(cols, 1).astype(np.float32)
    return bias, offs, biasc, offc


def consts_L1(half, S_len):
    NU = S_len // 512
    k = np.arange(128)[:, None]
    q = np.arange(512)[None, :]
    maskC = np.zeros((128, 4, 512), np.float32)
    for d in range(4):
        maskC[:, d, :] = np.where(128 * d + k <= q, 0.0, NEG)
    maskW = np.zeros((128, 8, 512), np.float32)
    for r in range(8):
        dl = q - k + 128 * (4 - r)
        maskW[:, r, :] = np.where((dl >= 0) & (dl <= 511), 0.0, NEG)
    maskK = np.zeros((128, 5, 512), np.float32)
    for e in range(5):
        maskK[:, e, :] = np.where(16 * k + 31 <= 512 * e + q, 0.0, NEG)
    bias, offs, biasc, offc = bias_tables_L1(half, NU)
    cidx = np.arange(S_len)[None, :]
    E32 = np.zeros((128, S_len), np.float32)
    E32[:32] = (cidx // 256 == np.arange(32)[:, None])
    E64 = (cidx // 64 == np.arange(128)[:, None]).astype(np.float32)
    PB = np.zeros((128, 64), np.float32)
    PB[:, 32:] = -1e30
    n = np.arange(512)[:, None]
    j = np.arange(128)[None, :]
    cis = ((16 * n <= 64 * j + 63) & (16 * n + 31 >= 64 * j) & (n < 511)).astype(np.float32)
    cis = cis.reshape(4, 128, 128).transpose(1, 0, 2)
    qh = (np.arange(128) // 64)[:, None]
    r = np.arange(255)[None, :] - 127
    T = np.where(r == qh, 1e9, np.where(r == qh - 1, 3e9, np.where(r > qh, -1e30, 0.0))).astype(np.float32)
    T = np.concatenate([T, np.zeros((128, 1), np.float32)], 1)
    return dict(maskC=maskC.astype(ml_dtypes.bfloat16), maskW=maskW.astype(ml_dtypes.bfloat16), maskK=maskK.astype(ml_dtypes.bfloat16),
                bias1=bias, biasc=biasc, E32=E32.astype(ml_dtypes.bfloat16), E64=E64.astype(ml_dtypes.bfloat16), PB=PB,
                cis=cis.astype(ml_dtypes.bfloat16), Ttab=T), offs, offc


def proj_tile_D(c, t, Wb, GD, QT0, QT1, KsT, KwT, kvcT, Vs, Vw, zs_dram, gsig, stage=0):
    S = c.S
    i = t % 2
    hT = c.hT[i]
    X, Y, Z = c.bank[2 + i], c.bank[4 + i], c.bank[6]
    bx, by, bz = ("bank", 2 + i), ("bank", 4 + i), ("bank", 6)
    st8 = c.st8[i]
    qk32d, qksqd, qkbd, zed = c.qk32_[i], c.qksq_[i], c.qkb_[i], c.ze_[i]
    k32, ksq, kkb, kze = "qk32_%d" % i, "qksq_%d" % i, "qkb_%d" % i, "ze_%d" % i
    if stage != 2:
        for bi, pb, c0, n in ((2 + i, X, 0, 512), (4 + i, Y, 512, 512), (6, Z, 1024, 12)):
            for kc in range(8):
                S.add("pe", I("matmul", pb[:, 0:n], lhsT=hT[:, kc * 128:(kc + 1) * 128], rhs=Wb[:, kc, c0:c0 + n], start=(kc == 0), stop=(kc == 7)),
                      reads=["hT%d" % i, ("Wb", kc)], writes=[("bank", bi)])
        S.add("act", I("copy", out=qk32d[:], in_=X[:, :]), reads=[bx], writes=[k32])
        S.add("pool", I("tensor_tensor", out=qksqd[:], in0=qk32d[:], in1=qk32d[:], op=ALU.mult), reads=[k32], writes=[ksq])
        S.add("dve", I("tensor_reduce", out=st8[:, 0:8], in_=qksqd[:].rearrange("p (g d) -> p g d", d=64), axis=AX.X, op=ALU.add),
              reads=[ksq], writes=["st8_%d" % i])
        rstd_from_sumsq(c, S, st8, 64, 8, "st8_%d" % i)
        S.add("act", I("activation", out=gsig[:, t, :], in_=Z[:, 0:12], func=AF.Exp, scale=-1.0), reads=[bz], writes=[("gsig", t)])
        S.add("dve", I("tensor_scalar", out=gsig[:, t, :], in0=gsig[:, t, :], scalar1=1.0, scalar2=None, op0=ALU.add), reads=[("gsig", t)], writes=[("gsig", t)])
        S.add("dve", I("reciprocal", out=gsig[:, t, :], in_=gsig[:, t, :]), reads=[("gsig", t)], writes=[("gsig", t)])
    if stage == 1:
        return
    for g in range(8):
        S.add("dve", I("scalar_tensor_tensor", out=qkbd[:, g * 64:(g + 1) * 64], in0=qk32d[:, g * 64:(g + 1) * 64],
                       scalar=st8[:, g:g + 1], in1=GD[:, g * 64:(g + 1) * 64], op0=ALU.mult, op1=ALU.mult),
              reads=[k32, "st8_%d" % i, "GD"], writes=[kkb])
    S.add("act", I("copy", out=qkbd[:, 512:640], in_=Y[:, 0:128]), reads=[by], writes=[kkb])
    tb = c.bank[7].bitcast(BF16)
    for n in range(5):
        S.add("pe", I("transpose", out=tb[:, n * 128:(n + 1) * 128], in_=qkbd[:, n * 128:(n + 1) * 128], identity=c.ident[:]),
              reads=[kkb, "ident"], writes=[("bank", 7)])
    sl = slice(t * 128, (t + 1) * 128)
    S.add("act", I("copy", out=QT0[:, sl], in_=tb[:, 0:128]), reads=[("bank", 7)], writes=[("B0", t)])
    S.add("dve", I("tensor_copy", out=QT1[:, sl], in_=tb[:, 128:256]), reads=[("bank", 7)], writes=[("B1", t)])
    S.add("act", I("copy", out=KsT[:, sl], in_=tb[:, 256:384]), reads=[("bank", 7)], writes=[("B2", t)])
    S.add("dve", I("tensor_copy", out=KwT[:, sl], in_=tb[:, 384:512]), reads=[("bank", 7)], writes=[("B3", t)])
    S.add("act", I("copy", out=kvcT[:, sl], in_=tb[:, 512:640]), reads=[("bank", 7)], writes=[("B4", t)])
    S.add("act", I("copy", out=Vs[:, t, 0:64], in_=Y[:, 128:192]), reads=[by], writes=[("V", t)])
    S.add("act", I("copy", out=Vw[:, t, 0:64], in_=Y[:, 192:256]), reads=[by], writes=[("V", t)])
    zt = c.zt[i]
    S.add("act", I("activation", out=zed[:], in_=Y[:, 256:512], func=AF.Exp, scale=-1.0), reads=[by], writes=[kze])
    S.add("dve", I("tensor_scalar", out=zed[:], in0=zed[:], scalar1=1.0, scalar2=None, op0=ALU.add), reads=[kze], writes=[kze])
    S.add("dve", I("reciprocal", out=zed[:], in_=zed[:]), reads=[kze], writes=[kze])
    S.add("dve", I("tensor_tensor", out=zt[:, 0:256], in0=zed[:], in1=Y[:, 256:512], op=ALU.mult), reads=[kze, by], writes=["zt%d" % i])
    S.add("sp", I("dma_start", out=zs_dram[t * 128:(t + 1) * 128, 0:256], in_=zt[:, 0:256]), reads=["zt%d" % i], writes=[("zsd", t)], dma=True,
          semkey=("dma", "zt%d" % i))


def attn_C(c, QT, KT, Vaug, zs_dram, E32, maskC, bias1, boffs, qws, kmT, PB, out_dram, col0, S_len):
    S = c.S
    NU = S_len // 512
    NT = S_len // 128
    ss = SweepState()
    ocnt = 0
    for U in range(NU):
        mixt = c.mixt[U % 2]
        mk = "mixt%d" % (U % 2)
        zt = c.zu[U % 2]
        zk = "zu%d" % (U % 2)
        S.add("sp", I("dma_start", out=zt[:, :, 0:128], in_=zs_dram[U * 512:(U + 1) * 512, 0:128].rearrange("(j p) c -> p j c", p=128)),
              reads=[("zsd", 4 * U + j) for j in range(4)], writes=[zk], dma=True)
        for hl in range(2):
            r0 = hl * 64
            qreads = [("B0", 4 * U + j) for j in range(4)]
            gb = c.bank[7]
            for j in range(4):
                S.add("pe", I("matmul", gb[:, j * 32:(j + 1) * 32], lhsT=QT[r0:r0 + 64, (4 * U + j) * 128:(4 * U + j + 1) * 128],
                              rhs=kmT[r0:r0 + 64, 0:32], start=True, stop=True), reads=qreads + ["kmT"], writes=[("bank", 7)])
            for j in range(4):
                own = (4 * U + j) // 2
                S.add("dve", I("tensor_tensor", out=c.gm[:, j, :], in0=gb[:, j * 32:(j + 1) * 32], in1=PB[:, 32 - own:64 - own], op=ALU.add),
                      reads=[("bank", 7), "PB"], writes=["gm"])
            for j in range(4):
                S.add("dve", I("max", out=c.mx[:, j, :], in_=c.gm[:, j, :]), reads=["gm"], writes=["mx"])
            for j in range(4):
                S.add("dve", I("tensor_scalar", out=c.mb[:, j, 0:32], in0=c.gm[:, j, :], scalar1=c.mx[:, j, 2:3], scalar2=NEG, op0=ALU.is_lt, op1=ALU.mult),
                      reads=["gm", "mx"], writes=["mb"])
            S.add("dve", I("memset", c.mb[:, 0:2, 2 * U:2 * U + 1], 0.0), reads=["mb"], writes=["mb"])
            S.add("dve", I("memset", c.mb[:, 2:4, 2 * U + 1:2 * U + 2], 0.0), reads=["mb"], writes=["mb"])
            tb = c.bank[7].bitcast(BF16)
            for j in range(4):
                S.add("pe", I("transpose", out=tb[0:32, 512 + j * 128:512 + (j + 1) * 128], in_=c.mb[:, j, 0:32], identity=c.ident[:]),
                      reads=["mb", "ident"], writes=[("bank", 7)])
            MBT = c.MBT[ocnt % 2]
            mbk = "MBT%d" % (ocnt % 2)
            S.add("act", I("copy", out=MBT[0:32, :], in_=tb[0:32, 512:1024]), reads=[("bank", 7)], writes=[mbk])
            oi = 3 + (ocnt % 2)
            ocnt += 1
            qw = qws[hl]
            nsub = 512 // qw

            def extra(J, MBT=MBT, mbk=mbk, U=U):
                ex = [(E32[:, J * 128:(J + 1) * 128], MBT[:, :], ["Ebuf", mbk])]
                d = J - 4 * U
                if d >= 0:
                    ex.append((c.ident[:], maskC[:, d, :], ["ident", "maskC"]))
                return ex

            def biasf(J, i, U=U, hl=hl, nsub=nsub):
                col = boffs[hl] + (J - 4 * U + 4 * (NU - 1)) * nsub + i
                return bias1[:, col:col + 1], "bias1"

            sweep(c, ss, QT[r0:r0 + 64, U * 512:(U + 1) * 512], qreads, list(range(0, 4 * U + 4)),
                  lambda J: KT[r0:r0 + 64, J * 128:(J + 1) * 128], lambda J: [("B1", J)], extra, biasf, qw,
                  lambda J: Vaug[:, J, hl, :], lambda J: [("V", J)], [(oi, j * 65) for j in range(4)], 65,
                  skip=lambda J, j, U=U: (J - 4 * U) > j)
            ob = c.bank[oi]
            rl = c.rl[ocnt % 2]
            rk = "rl%d" % (ocnt % 2)
            S.add("dve", I("reciprocal", out=rl[:, 0:4], in_=ob[:, 0:260].rearrange("p (j d) -> p j d", d=65)[:, :, 64]), reads=[("bank", oi)], writes=[rk])
            for j in range(4):
                S.add("dve", I("scalar_tensor_tensor", out=mixt[:, j, r0:r0 + 64], in0=ob[:, j * 65:j * 65 + 64], scalar=rl[:, j:j + 1],
                               in1=zt[:, j, r0:r0 + 64], op0=ALU.mult, op1=ALU.mult), reads=[("bank", oi), rk, zk], writes=[mk])
        S.add("sp", I("dma_start", out=out_dram[U * 512:(U + 1) * 512, col0:col0 + 128].rearrange("(j p) c -> p j c", p=128), in_=mixt[:, :, 0:128]),
              reads=[mk], writes=[("out", col0, U)], dma=True, semkey=("dma", mk))


def nsa_compress(c, kvcT, W1b, W2kk, W2v, peT, gk3col, BO, kcmpT, Vc, S_len):
    S = c.S
    NC = S_len // 16 - 1
    NT = S_len // 128
    allkv = [("B4", t) for t in range(NT)]
    wk = [("Wb", kc) for kc in range(8)]
    b1 = c.b1
    for s, bi in ((0, 3), (1, 4)):
        rows = slice(64 * s, 64 * s + 64)
        for i in range(32):
            S.add("pe", I("matmul", c.bank[bi][:, 0:1], lhsT=W1b[rows, i, :], rhs=peT[rows, i:i + 1], start=(i == 0), stop=(i == 31)),
                  reads=wk + ["peT"], writes=[("bank", bi)])
        S.add("act", I("copy", out=b1[:, s:s + 1], in_=c.bank[bi][:, 0:1]), reads=[("bank", bi)], writes=["b1"])
    S.add("dve", I("tensor_scalar", out=b1[:, 2:4], in0=b1[:, 0:2], scalar1=-1.0, scalar2=None, op0=ALU.mult), reads=["b1"], writes=["b1"])
    hs = c.hs
    for s, bi in ((0, 5), (1, 6)):
        rows = slice(64 * s, 64 * s + 64)
        hb = c.bank[bi]
        for i in range(32):
            S.add("pe", I("matmul", hb[:, 0:NC], lhsT=W1b[rows, i, :], rhs=kvcT[rows, i:i + 16 * (NC - 1) + 1:16], start=(i == 0), stop=(i == 31)),
                  reads=wk + allkv, writes=[("bank", bi)])
        S.add("act", I("activation", out=c.he[:, 0:NC], in_=hb[:, 0:NC], func=AF.Exp, scale=-1.0, bias=b1[:, 2 + s:3 + s]), reads=[("bank", bi), "b1"], writes=["he"])
        S.add("dve", I("tensor_scalar", out=c.he[:, 0:NC], in0=c.he[:, 0:NC], scalar1=1.0, scalar2=None, op0=ALU.add), reads=["he"], writes=["he"])
        S.add("dve", I("reciprocal", out=c.he[:, 0:NC], in_=c.he[:, 0:NC]), reads=["he"], writes=["he"])
        S.add("dve", I("memset", hs[s][:, :], 0.0), writes=["hs%d" % s])
        S.add("dve", I("scalar_tensor_tensor", out=hs[s][:, 0:NC], in0=hb[:, 0:NC], scalar=b1[:, s:s + 1], in1=c.he[:, 0:NC], op0=ALU.add, op1=ALU.mult),
              reads=[("bank", bi), "b1", "he", "hs%d" % s], writes=["hs%d" % s])
    kb = c.bank[3]
    S.add("pe", I("matmul", kb[:, 0:NC], lhsT=W2kk[:, :], rhs=hs[0][:, 0:NC], start=True, stop=True), reads=["W2", "hs0"], writes=[("bank", 3)])
    S.add("act", I("activation", out=c.ksq[:, 0:NC], in_=kb[:, 0:NC], func=AF.Square), reads=[("bank", 3)], writes=["ksq"])
    S.add("pe", I("matmul", c.bank[4][:, 0:NC], lhsT=BO[:, :], rhs=c.ksq[:, 0:NC], start=True, stop=True), reads=["BO", "ksq"], writes=[("bank", 4)])
    S.add("dve", I("tensor_scalar", out=c.he[:, 0:NC], in0=c.bank[4][:, 0:NC], scalar1=1.0 / 64, scalar2=EPS, op0=ALU.mult, op1=ALU.add),
          reads=[("bank", 4)], writes=["he"])
    S.add("act", I("activation", out=c.he[:, 0:NC], in_=c.he[:, 0:NC], func=AF.Ln), reads=["he"], writes=["he"])
    S.add("act", I("activation", out=c.he[:, 0:NC], in_=c.he[:, 0:NC], func=AF.Exp, scale=-0.5), reads=["he"], writes=["he"])
    S.add("dve", I("memset", kcmpT[:, :], 0.0), writes=["kcmpT"])
    S.add("dve", I("scalar_tensor_tensor", out=kcmpT[:, 0:NC], in0=kb[:, 0:NC], scalar=gk3col[:, 0:1], in1=c.he[:, 0:NC], op0=ALU.mult, op1=ALU.mult),
          reads=[("bank", 3), "gk3col", "he", "kcmpT"], writes=["kcmpT"])
    nch = (NC + 127) // 128
    for cj in range(nch):
        S.add("pe", I("matmul", c.bank[5][:, cj * 64:(cj + 1) * 64], lhsT=hs[1][:, cj * 128:(cj + 1) * 128], rhs=W2v[:, :], start=True, stop=True),
              reads=["hs1", "W2"], writes=[("bank", 5)])
    S.add("act", I("copy", out=Vc[:, 0:nch, 0:64], in_=c.bank[5][:, 0:nch * 64].rearrange("p (c d) -> p c d", d=64)), reads=[("bank", 5)], writes=["Vc"])


def attn_D(c, QTs, KsT, KwT, kcmpT, Vs, Vw, Vc, zs_dram, gsig, E64, maskC, maskW, maskK, bias1, boffs, biasc, boffc, Ttab, out_dram, S_len):
    S = c.S
    NU = S_len // 512
    ss = SweepState()
    NCH = (S_len // 16 - 1 + 127) // 128
    for U in range(NU):
        mixt = c.mixt[U % 2]
        mk = "mixt%d" % (U % 2)
        zt = c.zu[U % 2]
        zk = "zu%d" % (U % 2)
        S.add("sp", I("dma_start", out=zt[:, :, :], in_=zs_dram[U * 512:(U + 1) * 512, 0:256].rearrange("(j p) c -> p j c", p=128)),
              reads=[("zsd", 4 * U + j) for j in range(4)], writes=[zk], dma=True)
        acc, imp = c.acc, c.imp
        gk = [("gsig", 4 * U + j) for j in range(4)]
        for h in range(4):
            QT = QTs[h // 2]
            qkey = "B%d" % (h // 2)
            r0 = (h % 2) * 64
            qreads = [(qkey, 4 * U + j) for j in range(4)]
            qw = QWS[h]
            nsub = 512 // qw
            chunks = [cj for cj in range(NCH) if U - 4 * cj >= 0]

            def extra(cj, U=U):
                e = U - 4 * cj
                if e <= 4:
                    return [(c.ident[:], maskK[:, e, :], ["ident", "maskK"])]
                return []

            def biasf(cj, i, U=U, h=h, nsub=nsub):
                col = boffc[h] + (U - 4 * cj) * nsub + i
                return biasc[:, col:col + 1], "biasc"

            subs = [(5 + j // 2, (j % 2) * 193) for j in range(4)]
            sweep(c, ss, QT[r0:r0 + 64, U * 512:(U + 1) * 512], qreads, chunks,
                  lambda cj: kcmpT[r0:r0 + 64, cj * 128:(cj + 1) * 128], lambda cj: ["kcmpT"], extra, biasf, qw,
                  lambda cj: Vc[:, cj, :], lambda cj: ["Vc"], subs, 193)
            rl = c.rl[h % 2]
            rk = "rl%d" % (h % 2)
            for bi in (5, 6):
                S.add("dve", I("tensor_scalar", out=rl[:, 2 * (bi - 5):2 * (bi - 5) + 2], in0=c.bank[bi][:, 0:386].rearrange("p (j d) -> p j d", d=193)[:, :, 64],
                               scalar1=1e-30, scalar2=None, op0=ALU.max), reads=[("bank", bi)], writes=[rk])
            S.add("dve", I("reciprocal", out=rl[:, 0:4], in_=rl[:, 0:4]), reads=[rk], writes=[rk])
            for j in range(4):
                bi, o0 = subs[j]
                ob = c.bank[bi]
                S.add("dve", I("tensor_scalar", out=acc[:, j, h, :], in0=ob[:, o0:o0 + 64], scalar1=rl[:, j:j + 1], scalar2=gsig[:, 4 * U + j, 3 * h:3 * h + 1],
                               op0=ALU.mult, op1=ALU.mult), reads=[("bank", bi), rk] + gk, writes=["acc"])
                if h == 0:
                    S.add("dve", I("tensor_scalar", out=imp[:, j, :], in0=ob[:, o0 + 65:o0 + 193], scalar1=rl[:, j:j + 1], scalar2=None, op0=ALU.mult),
                          reads=[("bank", bi), rk], writes=["imp"])
                else:
                    S.add("dve", I("scalar_tensor_tensor", out=imp[:, j, :], in0=ob[:, o0 + 65:o0 + 193], scalar=rl[:, j:j + 1], in1=imp[:, j, :],
                                   op0=ALU.mult, op1=ALU.add), reads=[("bank", bi), rk, "imp"], writes=["imp"])
        MBT = c.MBT[U % 2]
        mbk = "MBT%d" % (U % 2)
        tb = c.bank[7].bitcast(BF16)
        for j in range(4):
            tp = 4 * U + j
            S.add("dve", I("tensor_tensor", out=c.impb[:, :], in0=imp[:, j, :], in1=Ttab[:, 127 - 2 * tp:255 - 2 * tp], op=ALU.add), reads=["imp", "Ttab"], writes=["impb"])
            S.add("dve", I("memset", c.impb[:, 0:1], 2e9), reads=["impb"], writes=["impb"])
            S.add("dve", I("max", out=c.mx16[:, 0:8], in_=c.impb[:, :]), reads=["impb"], writes=["mx16"])
            S.add("dve", I("match_replace", out=c.impc[:, :], in_to_replace=c.mx16[:, 0:8], in_values=c.impb[:, :], imm_value=-3e38),
                  reads=["impb", "mx16"], writes=["impc"])
            S.add("dve", I("max", out=c.mx16[:, 8:16], in_=c.impc[:, :]), reads=["impc", "mx16"], writes=["mx16"])
            S.add("dve", I("tensor_scalar", out=c.mb[:, j, :], in0=c.impb[:, :], scalar1=c.mx16[:, 15:16], scalar2=NEG, op0=ALU.is_lt, op1=ALU.mult),
                  reads=["impb", "mx16"], writes=["mb"])
            S.add("pe", I("transpose", out=tb[:, j * 128:(j + 1) * 128], in_=c.mb[:, j, :], identity=c.ident[:]), reads=["mb", "ident"], writes=[("bank", 7)])
        S.add("act", I("copy", out=MBT[:, :], in_=tb[:, 0:512]), reads=[("bank", 7)], writes=[mbk])
        for br in range(2):
            for h in range(4):
                QT = QTs[h // 2]
                qkey = "B%d" % (h // 2)
                r0 = (h % 2) * 64
                qreads = [(qkey, 4 * U + j) for j in range(4)]
                qw = QWS[h]
                nsub = 512 // qw
                oi = 3 + (h % 2)

                def biasf(J, i, U=U, h=h, nsub=nsub):
                    col = boffs[h] + (J - 4 * U + 4 * (NU - 1)) * nsub + i
                    return bias1[:, col:col + 1], "bias1"

                if br == 0:
                    Js = list(range(0, 4 * U + 4))

                    def extra(J, U=U, MBT=MBT, mbk=mbk):
                        ex = [(E64[:, J * 128:(J + 1) * 128], MBT[:, :], ["Ebuf", mbk])]
                        d = J - 4 * U
                        if d >= 0:
                            ex.append((c.ident[:], maskC[:, d, :], ["ident", "maskC"]))
                        return ex
                    KTt, kkey, Vt = KsT, "B2", Vs
                else:
                    Js = list(range(max(0, 4 * U - 4), 4 * U + 4))

                    def extra(J, U=U):
                        return [(c.ident[:], maskW[:, J - (4 * U - 4), :], ["ident", "maskW"])]
                    KTt, kkey, Vt = KwT, "B3", Vw
                sweep(c, ss, QT[r0:r0 + 64, U * 512:(U + 1) * 512], qreads, Js,
                      lambda J, KTt=KTt, r0=r0: KTt[r0:r0 + 64, J * 128:(J + 1) * 128], lambda J, kkey=kkey: [(kkey, J)], extra, biasf, qw,
                      lambda J, Vt=Vt: Vt[:, J, :], lambda J: [("V", J)], [(oi, j * 65) for j in range(4)], 65,
                      skip=lambda J, j, U=U: (J - 4 * U) > j)
                ob = c.bank[oi]
                rl = c.rl[h % 2]
                rk = "rl%d" % (h % 2)
                S.add("dve", I("reciprocal", out=rl[:, 0:4], in_=ob[:, 0:260].rearrange("p (j d) -> p j d", d=65)[:, :, 64]), reads=[("bank", oi)], writes=[rk])
                for j in range(4):
                    S.add("dve", I("tensor_scalar", out=c.otmp[:, :], in0=ob[:, j * 65:j * 65 + 64], scalar1=rl[:, j:j + 1],
                                   scalar2=gsig[:, 4 * U + j, 3 * h + 1 + br:3 * h + 2 + br], op0=ALU.mult, op1=ALU.mult),
                          reads=[("bank", oi), rk] + gk, writes=["otmp"])
                    S.add("dve", I("tensor_tensor", out=acc[:, j, h, :], in0=acc[:, j, h, :], in1=c.otmp[:, :], op=ALU.add), reads=["otmp", "acc"], writes=["acc"])
        S.add("dve", I("tensor_tensor", out=mixt[:, :, :], in0=acc[:, :, :, :].rearrange("p j h d -> p j (h d)"), in1=zt[:, :, :], op=ALU.mult),
              reads=["acc", zk], writes=[mk])
        S.add("sp", I("dma_start", out=out_dram[U * 512:(U + 1) * 512, 256:512].rearrange("(j p) c -> p j c", p=128), in_=mixt[:, :, :]),
              reads=[mk], writes=[("out", 256, U)], dma=True, semkey=("dma", mk))


def build_L1(S_len):
    NT = S_len // 128
    NU = S_len // 512
    NB = S_len // 256
    nc = bass.Bass("TRN2", target_bir_lowering=False)
    st = contextlib.ExitStack()
    with st:
        c = Ctx(nc, st)
        S = c.S
        x = c.din("x", [S_len, D], F32)
        m0T = c.din("m0T", [D, S_len], BF16)
        woe = c.din("woe", [D, D], F32)
        lncol = c.din("lncol", [128, 8], F32)
        wC = [c.din("wC%d" % i, [D, 512], F32) for i in range(2)]
        wD = c.din("wD", [D, 1036], F32)
        gC_d = c.din("gC", [1, 256], F32)
        gD_d = c.din("gD", [1, 512], F32)
        W1_d = c.din("W1cat", [128, 32, 128], F32)
        W2_d = c.din("W2cat", [128, 192], F32)
        pe_d = c.din("peT", [128, 32], F32)
        gk3_d = c.din("gk3col", [128, 1], F32)
        BO_d = c.din("BO", [128, 128], BF16)
        cshape = dict(maskC=[128, 4, 512], maskW=[128, 8, 512], maskK=[128, 5, 512], E32=[128, S_len], E64=[128, S_len], cis=[128, 4, 128])
        cd = {k: c.din(k, v, BF16) for k, v in cshape.items()}
        nb1 = sum((4 * NU) * (512 // q) for q in QWS)
        nbc = sum(NU * (512 // q) for q in QWS)
        bias1_d = c.din("bias1", [128, nb1], F32)
        biasc_d = c.din("biasc", [128, nbc], F32)
        PB_d = c.din("PB", [128, 64], F32)
        T_d = c.din("Ttab", [128, 256], F32)
        x1 = c.dout("x1", [S_len, D], F32)
        out = c.dout("mixed", [S_len, 512], BF16)
        zsd = c.dout("zsd", [S_len, 256], BF16)
        alloc_common(c, qkw=512, bkw=640, zw=256)
        Wb = c.sb("Wb", [128, 8, 1040], BF16)
        Bt = [c.sb("B%d" % i, [128, S_len], BF16) for i in range(5)]
        Vsb = c.sb("Vsb", [128, NT * 130], BF16)
        maskC = c.sb("maskC_s", [128, 4, 512], BF16)
        maskW = c.sb("maskW_s", [128, 8, 512], BF16)
        maskK = c.sb("maskK_s", [128, 5, 512], BF16)
        bias1 = c.sb("bias1_s", [128, nb1], F32)
        biasc = c.sb("biasc_s", [128, nbc], F32)
        PB = c.sb("PB_s", [128, 64], F32)
        Ttab = c.sb("T_s", [128, 256], F32)
        GC = c.sb("GC", [128, 256], F32)
        GD = c.sb("GD", [128, 512], F32)
        BO = c.sb("BO_s", [128, 128], BF16)
        W2f = c.sb("W2f", [128, 192], F32)
        W2b = c.sb("W2b", [128, 192], BF16)
        pef = c.sb("pef", [128, 32], F32)
        peT = c.sb("peTb", [128, 32], BF16)
        gk3col = c.sb("gk3", [128, 1], F32)
        c.b1 = c.sb("b1", [128, 4], F32)
        c.he = c.sb("he", [128, 512], F32)
        c.hs = [c.sb("hs%d" % i, [128, 512], BF16) for i in range(2)]
        c.ksq = c.sb("ksq", [128, 512], BF16)
        kcmpT = c.sb("kcmpT", [128, 512], BF16)
        Vc = c.sb("Vc", [128, 4, 193], BF16)
        gsig = c.sb("gsig", [128, NT, 12], F32)
        kmf = c.sb("kmf", [128, 32], F32)
        kmT = c.sb("kmT", [128, 32], BF16)
        c.pt = [c.sb("pt%d" % i, [128, 512], BF16) for i in range(4)]
        c.mixt = [c.sb("mixt%d" % i, [128, 4, 256], BF16) for i in range(2)]
        c.zu = [c.sb("zu%d" % i, [128, 4, 256], BF16) for i in range(2)]
        c.zt = [c.sb("zt%d" % i, [128, 256], BF16) for i in range(2)]
        c.rl = [c.sb("rl%d" % i, [128, 4], F32) for i in range(2)]
        c.MBT = [c.sb("MBT%d" % i, [128, 512], BF16) for i in range(2)]
        c.gm = c.sb("gm", [128, 4, 32], F32)
        c.mx = c.sb("mx", [128, 4, 8], F32)
        c.mb = c.sb("mb", [128, 4, 128], BF16)
        c.mx16 = c.sb("mx16", [128, 16], F32)
        c.imp = c.sb("imp", [128, 4, 128], F32)
        c.impb = c.sb("impb", [128, 128], F32)
        c.impc = c.sb("impc", [128, 128], F32)
        c.acc = c.sb("acc", [128, 4, 4, 64], F32)
        c.otmp = c.sb("otmp", [128, 64], F32)
        m0 = [c.hT[i][:, :].rearrange("p (k t) -> p k t", k=8) for i in range(2)]

        def ld(dst, src, key):
            S.add("sp", I("dma_start", out=dst, in_=src), writes=[key], dma=True)
        ld(maskC[:], cd["maskC"], "maskC"); ld(maskW[:], cd["maskW"], "maskW"); ld(maskK[:], cd["maskK"], "maskK")
        ld(bias1[:], bias1_d, "bias1"); ld(biasc[:], biasc_d, "biasc"); ld(PB[:], PB_d, "PB"); ld(Ttab[:], T_d, "Ttab")
        ld(GC[:], gC_d.partition_broadcast(128), "GC"); ld(GD[:], gD_d.partition_broadcast(128), "GD")
        ld(BO[:], BO_d, "BO"); ld(W2f[:], W2_d, "W2f"); ld(pef[:], pe_d, "pef"); ld(gk3col[:], gk3_d, "gk3col")
        S.add("dve", I("tensor_scalar", out=GC[:, 0:128], in0=GC[:, 0:128], scalar1=0.125, scalar2=None, op0=ALU.mult), reads=["GC"], writes=["GC"])
        S.add("dve", I("tensor_scalar", out=GD[:, 0:256], in0=GD[:, 0:256], scalar1=0.125, scalar2=None, op0=ALU.mult), reads=["GD"], writes=["GD"])
        S.add("dve", I("tensor_copy", out=W2b[:], in_=W2f[:]), reads=["W2f"], writes=["W2"])
        S.add("dve", I("tensor_copy", out=peT[:], in_=pef[:]), reads=["pef"], writes=["peT"])
        S.add("pool", I("memset", Vc[:, :, :], 0.0), writes=["Vc"])
        S.add("pool", I("memset", Vc[:, :, 64:65], 1.0), reads=["Vc"], writes=["Vc"])
        S.add("sp", I("dma_start", out=Vc[:, :, 65:193], in_=cd["cis"]), reads=["Vc"], writes=["Vc"], dma=True)
        for i in range(2):
            S.add("pool", I("memset", c.MBT[i][:, :], 0.0), writes=["MBT%d" % i])

        for kc in range(8):
            i = kc % 2
            S.add("sp", I("dma_start", out=c.xt[i][:, :], in_=woe[kc * 128:(kc + 1) * 128, :]), writes=["xt%d" % i], dma=True)
            S.add("dve", I("tensor_copy", out=Wb[:, kc, 0:1024], in_=c.xt[i][:, :]), reads=["xt%d" % i], writes=[("Wb", kc)])
        for t in range(NT):
            i = t % 2
            xt = c.xt[i]
            S.add("sp", I("dma_start", out=xt[:], in_=x[t * 128:(t + 1) * 128, :]), writes=["xt%d" % i], dma=True)
            S.add("sp", I("dma_start", out=m0[i][:, :, :], in_=m0T[:, t * 128:(t + 1) * 128].rearrange("(kc p) t -> p kc t", p=128)),
                  writes=["hT%d" % i], dma=True)
            for cg in range(2):
                bi = 2 + 2 * i + cg
                for kc in range(8):
                    S.add("pe", I("matmul", c.bank[bi][:, :], lhsT=m0[i][:, kc, :], rhs=Wb[:, kc, cg * 512:(cg + 1) * 512], start=(kc == 0), stop=(kc == 7)),
                          reads=["hT%d" % i, ("Wb", kc)], writes=[("bank", bi)])
                S.add("dve", I("tensor_tensor", out=xt[:, cg * 512:(cg + 1) * 512], in0=xt[:, cg * 512:(cg + 1) * 512], in1=c.bank[bi][:, :], op=ALU.add),
                      reads=["xt%d" % i, ("bank", bi)], writes=["xt%d" % i])
            S.add("sp", I("dma_start", out=x1[t * 128:(t + 1) * 128, :], in_=xt[:]), reads=["xt%d" % i], writes=[("x1d", t)], dma=True,
                  semkey=("dma", "x1st%d" % i))

        bias_l1, boffs, biasc_l1, boffc = None, None, None, None
        boffs, o = [], 0
        for h in range(4):
            boffs.append(o)
            o += (4 * NU) * (512 // QWS[h])
        boffc, o = [], 0
        for h in range(4):
            boffc.append(o)
            o += NU * (512 // QWS[h])

        QT, KT = Bt[0], Bt[1]
        for pi in range(2):
            tag = "C%d" % pi
            load_weights(c, wC[pi], lncol, Wb[:, :, 0:512], tag)
            Vv = Vsb[:, 0:NT * 130].rearrange("p (t h d) -> p t h d", h=2, d=65)
            S.add("pool", I("memset", Vv[:, :, :, 64:65], 1.0), reads=[("V", t) for t in range(NT)], writes=[("V", t) for t in range(NT)])
            ld(Bt[4][:], cd["E32"], "Ebuf")

            def v_evac(t, pb, bk, Vv=Vv):
                S.add("act", I("copy", out=Vv[:, t, :, 0:64], in_=pb[:, 256:384].rearrange("p (h d) -> p h d", h=2)), reads=[bk], writes=[("V", t)])
            pk = dict(z_dram=zsd, kq="B0", kk="B1", gkey="GC")
            emit_hT(c, x1, 0, xreads=[("x1d", 0)])
            emit_hT(c, x1, 1, xreads=[("x1d", 1)])
            proj_tile(c, 0, Wb[:, :, 0:512], tag, GC, QT, KT, v_evac, None, stage=1, **pk)
            for t in range(NT):
                if t + 2 < NT:
                    emit_hT(c, x1, t + 2, xreads=[("x1d", t + 2)])
                if t + 1 < NT:
                    proj_tile(c, t + 1, Wb[:, :, 0:512], tag, GC, QT, KT, v_evac, None, stage=1, **pk)
                proj_tile(c, t, Wb[:, :, 0:512], tag, GC, QT, KT, v_evac, None, stage=2, **pk)
            S.add("dve", I("tensor_reduce", out=kmf[:, 0:NB], in_=KT[:, :].rearrange("p (n k) -> p n k", k=256), axis=AX.X, op=ALU.add),
                  reads=[("B1", t) for t in range(NT)], writes=["kmf"])
            S.add("dve", I("memset", kmT[:, :], 0.0), writes=["kmT"])
            S.add("dve", I("tensor_scalar", out=kmT[:, 0:NB], in0=kmf[:, 0:NB], scalar1=1.0 / 256, scalar2=None, op0=ALU.mult), reads=["kmf", "kmT"], writes=["kmT"])
            attn_C(c, QT, KT, Vv, zsd, Bt[4], maskC, bias1, boffs[2 * pi:2 * pi + 2], QWS[2 * pi:2 * pi + 2], kmT, PB, out, 128 * pi, S_len)

        load_weights(c, wD, lncol, Wb[:, :, 0:1036], "D")
        Vs = Vsb[:, 0:NT * 65].rearrange("p (t d) -> p t d", d=65)
        Vw = Vsb[:, NT * 65:NT * 130].rearrange("p (t d) -> p t d", d=65)
        S.add("pool", I("memset", Vs[:, :, 64:65], 1.0), reads=[("V", t) for t in range(NT)], writes=[("V", t) for t in range(NT)])
        S.add("pool", I("memset", Vw[:, :, 64:65], 1.0), reads=[("V", t) for t in range(NT)], writes=[("V", t) for t in range(NT)])
        dargs = (Wb, GD, Bt[0], Bt[1], Bt[2], Bt[3], Bt[4], Vs, Vw, zsd, gsig)
        emit_hT(c, x1, 0, xreads=[("x1d", 0)])
        emit_hT(c, x1, 1, xreads=[("x1d", 1)])
        proj_tile_D(c, 0, *dargs, stage=1)
        for t in range(NT):
            if t + 2 < NT:
                emit_hT(c, x1, t + 2, xreads=[("x1d", t + 2)])
            if t + 1 < NT:
                proj_tile_D(c, t + 1, *dargs, stage=1)
            proj_tile_D(c, t, *dargs, stage=2)
        W1b = Wb[:, :, :].rearrange("p a b -> p (a b)")[:, 0:4096].rearrange("p (i h) -> p i h", h=128)
        wk = [("Wb", kc) for kc in range(8)]
        for q4 in range(4):
            i = q4 % 2
            S.add("sp", I("dma_start", out=c.xt[i][:, :].rearrange("p (i h) -> p i h", h=128), in_=W1_d[:, q4 * 8:(q4 + 1) * 8, :]), writes=["xt%d" % i], dma=True)
            S.add("dve", I("tensor_copy", out=W1b[:, q4 * 8:(q4 + 1) * 8, :], in_=c.xt[i][:, :].rearrange("p (i h) -> p i h", h=128)),
                  reads=["xt%d" % i] + wk, writes=wk)
        nsa_compress(c, Bt[4], W1b, W2b[:, 0:128], W2b[:, 128:192], peT, gk3col, BO, kcmpT, Vc, S_len)
        S.add("sp", I("dma_start", out=Bt[4][:], in_=cd["E64"]), reads=[("B4", t) for t in range(NT)] + ["Ebuf"], writes=["Ebuf"] + [("B4", t) for t in range(NT)], dma=True,
              semkey=("dma", "Ebuf"))
        attn_D(c, [Bt[0], Bt[1]], Bt[2], Bt[3], kcmpT, Vs, Vw, Vc, zsd, gsig, Bt[4], maskC, maskW, maskK, bias1, boffs, biasc, boffc, Ttab, out, S_len)
        S.emit()
    return nc


def build_L2(ntok):
    NT = ntok // 128
    nc = bass.Bass("TRN2", target_bir_lowering=False)
    st = contextlib.ExitStack()
    with st:
        c = Ctx(nc, st)
        S = c.S
        x1 = c.din("x1", [ntok, D], F32)
        m1T = c.din("m1T", [D, ntok], BF16)
        woo = c.din("woo", [D, D], F32)
        out = c.dout("out", [ntok, D], F32)
        c.bank = [c.ps("bank%d" % i, [128, 512], F32) for i in range(8)]
        xt = [c.sb("xt%d" % i, [128, D], F32) for i in range(2)]
        m0 = [c.sb("m0_%d" % i, [128, 8, 128], BF16) for i in range(2)]
        Wb = c.sb("Wb", [128, 8, 1024], BF16)
        for kc in range(8):
            i = kc % 2
            S.add("sp", I("dma_start", out=xt[i][:, :], in_=woo[kc * 128:(kc + 1) * 128, :]), writes=["xt%d" % i], dma=True)
            S.add("dve", I("tensor_copy", out=Wb[:, kc, :], in_=xt[i][:, :]), reads=["xt%d" % i], writes=[("Wb", kc)])
        for t in range(NT):
            i = t % 2
            S.add("sp", I("dma_start", out=xt[i][:], in_=x1[t * 128:(t + 1) * 128, :]), writes=["xt%d" % i], dma=True)
            S.add("sp", I("dma_start", out=m0[i][:, :, :], in_=m1T[:, t * 128:(t + 1) * 128].rearrange("(kc p) t -> p kc t", p=128)),
                  writes=["m0_%d" % i], dma=True)
            for cg in range(2):
                bi = 2 * i + cg
                for kc in range(8):
                    S.add("pe", I("matmul", c.bank[bi][:, :], lhsT=m0[i][:, kc, :], rhs=Wb[:, kc, cg * 512:(cg + 1) * 512], start=(kc == 0), stop=(kc == 7)),
                          reads=["m0_%d" % i, ("Wb", kc)], writes=[("bank", bi)])
                S.add("dve", I("tensor_tensor", out=xt[i][:, cg * 512:(cg + 1) * 512], in0=xt[i][:, cg * 512:(cg + 1) * 512], in1=c.bank[bi][:, :], op=ALU.add),
                      reads=["xt%d" % i, ("bank", bi)], writes=["xt%d" % i])
            S.add("sp", I("dma_start", out=out[t * 128:(t + 1) * 128, :], in_=xt[i][:]), reads=["xt%d" % i], writes=[("outd", t)], dma=True,
                  semkey=("dma", "ost%d" % i))
        S.emit()
    return nc


def host_inputs_L0(inp, b, half, S_len):
    w = inp["w_in_e"][0]
    aq, ak, av, az = [w[:, i * 512:(i + 1) * 512] for i in range(4)]
    bq, bk, bv, bz = [w[:, 2048 + i * 512:2048 + (i + 1) * 512] for i in range(4)]
    d = {}
    d["x"] = np.ascontiguousarray(inp["x"][b, :S_len])
    d["lncol"] = np.ascontiguousarray(inp["ln_e"][0].reshape(8, 128).T)
    for pi in range(2):
        c0 = (4 * half + 2 * pi) * 64
        d["wA%d" % pi] = np.ascontiguousarray(np.concatenate([t[:, c0:c0 + 128] for t in (aq, ak, av, az)], axis=1))
        c0 = (2 * half + pi) * 128
        d["wB%d" % pi] = np.ascontiguousarray(np.concatenate([t[:, c0:c0 + 128] for t in (bq, bk, bv, bz)], axis=1))
    q = inp["qkn_e"][0]
    d["gA"] = np.concatenate([q[0], q[0], q[1], q[1]])[None, :].copy()
    d["gB"] = np.concatenate([q[2], q[2], q[3], q[3]])[None, :].copy()
    d["lam"] = inp["lam_e"][0].reshape(1, 256).copy()
    d["subln"] = inp["subln_e"][0].reshape(1, 128).copy()
    cst, _ = consts_L0(half, S_len)
    d.update(cst)
    return d


def host_inputs_L1(inp, xb, mixed0_bf, half, S_len):
    w = inp["w_in_o"][0]
    off = {}
    o = 0
    for name, n in zip(("cq", "ck", "cv", "cz", "dq", "dkc", "dvc", "dks", "dvs", "dkw", "dvw", "dz", "dg"),
                       (512, 512, 512, 512, 512, 128, 128, 128, 128, 128, 128, 512, 24)):
        off[name] = o
        o += n
    col = lambda name, a, n: w[:, off[name] + a:off[name] + a + n]
    d = {}
    d["x"] = np.ascontiguousarray(xb)
    d["m0T"] = np.ascontiguousarray(mixed0_bf.T)
    d["woe"] = np.ascontiguousarray(inp["w_out_e"][0])
    d["lncol"] = np.ascontiguousarray(inp["ln_o"][0].reshape(8, 128).T)
    for pi in range(2):
        c0 = (4 * half + 2 * pi) * 64
        d["wC%d" % pi] = np.ascontiguousarray(np.concatenate([col(nm, c0, 128) for nm in ("cq", "ck", "cv", "cz")], axis=1))
    g = half
    q0 = 4 * half * 64
    ks, kw = col("dks", g * 64, 64), col("dkw", g * 64, 64)
    d["wD"] = np.ascontiguousarray(np.concatenate([col("dq", q0, 256), ks, ks, kw, kw,
                                                   col("dkc", g * 64, 64), col("dvc", g * 64, 64), col("dvs", g * 64, 64), col("dvw", g * 64, 64),
                                                   col("dz", q0, 256), col("dg", 4 * half * 3, 12)], axis=1))
    q = inp["qkn_o"][0]
    d["gC"] = np.concatenate([q[0], q[0], q[1], q[1]])[None, :].copy()
    d["gD"] = np.concatenate([q[2]] * 4 + [q[4]] * 2 + [q[5]] * 2)[None, :].copy()
    w1 = inp["phi_w1"][0]
    d["W1cat"] = np.ascontiguousarray(np.concatenate([w1[s].reshape(32, 64, 128).transpose(1, 0, 2) for s in range(2)], axis=0))
    w2 = inp["phi_w2"][0]
    d["W2cat"] = np.ascontiguousarray(np.concatenate([w2[0], w2[0], w2[1]], axis=1))
    pe = inp["phi_pe"][0]
    d["peT"] = np.ascontiguousarray(np.concatenate([pe[0].T, pe[1].T], axis=0))
    d["gk3col"] = np.concatenate([q[3], q[3]])[:, None].copy()
    a = np.arange(128)
    d["BO"] = (a[:, None] // 64 == a[None, :] // 64).astype(BF)
    cst, _, _ = consts_L1(half, S_len)
    d.update(cst)
    return d


BF = ml_dtypes.bfloat16
SEQ = 8192
NB_ = 4


def kernel(**inputs):
    inp = {k: np.asarray(v) for k, v in inputs.items()}
    S_len = SEQ
    cores = list(range(8))
    nc0 = build_L0(S_len)
    maps = [host_inputs_L0(inp, core // 2, core % 2, S_len) for core in cores]
    res0 = run_bass_kernel_spmd(nc0, maps, core_ids=cores)
    mixed0 = np.empty((NB_, S_len, 1024), BF)
    for core in cores:
        b, half = core // 2, core % 2
        o = res0.results[core]["mixed"]
        mixed0[b][:, 256 * half:256 * half + 256] = o[:, 0:256]
        mixed0[b][:, 512 + 256 * half:512 + 256 * half + 256] = o[:, 256:512]
    del res0, maps
    nc1 = build_L1(S_len)
    maps = [host_inputs_L1(inp, inp["x"][core // 2], mixed0[core // 2], core % 2, S_len) for core in cores]
    res1 = run_bass_kernel_spmd(nc1, maps, core_ids=cores)
    mixed1 = np.empty((NB_, S_len, 1024), BF)
    x1 = []
    for core in cores:
        b, half = core // 2, core % 2
        o = res1.results[core]["mixed"]
        mixed1[b][:, 256 * half:256 * half + 256] = o[:, 0:256]
        mixed1[b][:, 512 + 256 * half:512 + 256 * half + 256] = o[:, 256:512]
        if half == 0:
            x1.append(res1.results[core]["x1"])
    del res1, maps
    ntok = S_len // 2
    nc2 = build_L2(ntok)
    maps = []
    for core in cores:
        b, half = core // 2, core % 2
        sl = slice(half * ntok, (half + 1) * ntok)
        maps.append({"x1": np.ascontiguousarray(x1[b][sl]), "m1T": np.ascontiguousarray(mixed1[b][sl].T),
                     "woo": np.ascontiguousarray(inp["w_out_o"][0])})
    res2 = run_bass_kernel_spmd(nc2, maps, core_ids=cores)
    out = np.empty((NB_, S_len, 1024), np.float32)
    for core in cores:
        b, half = core // 2, core % 2
        out[b, half * ntok:(half + 1) * ntok] = res2.results[core]["out"]
    return out
```

```python
import contextlib
import math
import numpy as np
import ml_dtypes
import concourse.bass as bass
import concourse.mybir as mybir
from concourse.bass_utils import run_bass_kernel_spmd

F32 = mybir.dt.float32
BF16 = mybir.dt.bfloat16
ALU = mybir.AluOpType
AF = mybir.ActivationFunctionType
AX = mybir.AxisListType


import os as _os
SAME_ENGINE_SYNC = _os.environ.get('SAME_SYNC', '1') == '1'


class Op:
    __slots__ = ("eng", "fn", "deps", "signal", "sem", "val", "dma", "semkey", "gi")


class Sched:
    ENG = ["pe", "act", "dve", "pool", "sp"]

    def __init__(self, nc):
        self.nc = nc
        self.ops = {e: [] for e in self.ENG}
        self.lastw = {}
        self.readers = {}
        self.n = 0

    def add(self, eng, fn, reads=(), writes=(), dma=False, semkey=None):
        op = Op()
        op.eng, op.fn, op.dma = eng, fn, dma
        op.signal = dma
        op.sem = None
        op.val = 0
        op.gi = self.n
        self.n += 1
        if dma:
            op.semkey = semkey if semkey is not None else ("dma", writes[0])
        else:
            op.semkey = None
        excl = [k for k in reads if isinstance(k, tuple) and k[0] == "bank"]
        if excl:
            reads = [k for k in reads if k not in excl]
            writes = list(writes) + [k for k in excl if k not in writes]
        deps = set()
        for k in reads:
            w = self.lastw.get(k)
            if w is not None:
                deps.add(w)
        for k in writes:
            w = self.lastw.get(k)
            if w is not None:
                deps.add(w)
            for r in self.readers.get(k, ()):
                deps.add(r)
        deps.discard(op)
        if SAME_ENGINE_SYNC:
            op.deps = [d for d in deps if not (d.eng == "pe" and eng == "pe" and not d.dma)]
        else:
            op.deps = [d for d in deps if d.dma or dma or d.eng != eng]
        for d in op.deps:
            d.signal = True
        for k in reads:
            self.readers.setdefault(k, []).append(op)
        for k in writes:
            self.lastw[k] = op
            self.readers[k] = []
        self.ops[eng].append(op)
        return op

    def emit(self, final_wait_ops=()):
        nc = self.nc
        import contextlib
        with contextlib.ExitStack() as st:
            esem = {e: st.enter_context(nc.semaphore("s_" + e)) for e in self.ENG}
            dsem = {}
            for e in self.ENG:
                if self.ops[e]:
                    self.ops[e][-1].signal = True
            for e in self.ENG:
                cnt = 0
                for op in self.ops[e]:
                    if op.dma:
                        if op.semkey not in dsem:
                            dsem[op.semkey] = [st.enter_context(nc.semaphore("d%d" % len(dsem))), 0]
                        ent = dsem[op.semkey]
                        ent[1] += 16
                        op.sem, op.val = ent[0], ent[1]
                    elif op.signal:
                        cnt += 1
                        op.sem, op.val = esem[e], cnt
            self.nsem = len(dsem) + 5
            block = st.enter_context(nc.Block())
            engobj = {"pe": block.tensor, "act": block.scalar, "dve": block.vector,
                      "pool": block.gpsimd, "sp": block.sync}

            def run_engine(e, extra_wait):
                def body(eng):
                    waited = {}
                    for op in self.ops[e]:
                        need = {}
                        for d in op.deps:
                            k = id(d.sem)
                            if k not in need or need[k][1] < d.val:
                                need[k] = (d.sem, d.val)
                        for k, (sem, val) in need.items():
                            if waited.get(k, 0) < val:
                                eng.wait_ge(sem, val)
                                waited[k] = val
                        ins = op.fn(eng)
                        if op.dma:
                            ins.then_inc(op.sem, 16)
                        elif op.signal:
                            ins.then_inc(op.sem, 1)
                    if e == "sp":
                        for k, ent in dsem.items():
                            eng.wait_ge(ent[0], ent[1])
                        for e2 in self.ENG:
                            if e2 != "sp" and self.ops[e2]:
                                lo = self.ops[e2][-1]
                                if not lo.dma:
                                    eng.wait_ge(lo.sem, lo.val)
                return body

            for e in self.ENG:
                extra = final_wait_ops if e == "sp" else ()
                engobj[e](run_engine(e, extra))


def I(name, *a, **k):
    return lambda e: getattr(e, name)(*a, **k)


NEG = -30000.0
EPS = 1e-6
D = 1024


class Ctx:
    def __init__(self, nc, st):
        self.nc, self.st = nc, st
        self.S = Sched(nc)
        self.uid = 0

    def sb(self, name, shape, dt):
        return self.st.enter_context(self.nc.sbuf_tensor(name, shape, dt))

    def ps(self, name, shape, dt):
        return self.st.enter_context(self.nc.psum_tensor(name, shape, dt))

    def din(self, name, shape, dt):
        return self.nc.dram_tensor(name, list(shape), dt, kind="ExternalInput").ap()

    def dout(self, name, shape, dt):
        return self.nc.dram_tensor(name, list(shape), dt, kind="ExternalOutput").ap()


def alloc_common(c, qkw=256, bkw=256, zw=128):
    c.bank = [c.ps("bank%d" % i, [128, 512], F32) for i in range(8)]
    c.ident = c.sb("ident", [128, 128], BF16)
    S = c.S
    S.add("pool", I("memset", c.ident[:], 1.0), writes=["ident"])
    S.add("pool", I("affine_select", out=c.ident[:], in_=c.ident[:], pattern=[[-1, 128]],
                                            compare_op=ALU.is_equal, fill=0.0, base=0, channel_multiplier=1),
          reads=["ident"], writes=["ident"])
    c.epsb = c.sb("epsb", [128, 1], F32)
    S.add("pool", I("memset", c.epsb[:], EPS), writes=["epsb"])
    c.xt = [c.sb("xt%d" % i, [128, D], F32) for i in range(2)]
    c.xsq_ = [c.sb("xsq%d" % i, [128, D], BF16) for i in range(2)]
    c.xn_ = [c.sb("xn%d" % i, [128, D], BF16) for i in range(2)]
    c.hT = [c.sb("hT%d" % i, [128, D], BF16) for i in range(2)]
    c.st1 = [c.sb("st1_%d" % i, [128, 4], F32) for i in range(2)]
    c.qk32_ = [c.sb("qk32_%d" % i, [128, qkw], F32) for i in range(2)]
    c.qksq_ = [c.sb("qksq_%d" % i, [128, qkw], F32) for i in range(2)]
    c.qkb_ = [c.sb("qkb_%d" % i, [128, bkw], BF16) for i in range(2)]
    c.st8 = [c.sb("st8_%d" % i, [128, 8], F32) for i in range(2)]
    c.ze_ = [c.sb("ze_%d" % i, [128, zw], F32) for i in range(2)]


def load_weights(c, w_dram, lncol_dram, Wb, tag):
    S = c.S
    ncols = w_dram.shape[1]
    lncol = c.sb("lncol" + tag, [128, 8], F32)
    S.add("sp", I("dma_start", out=lncol[:], in_=lncol_dram), writes=["lncol" + tag], dma=True)
    n = 0
    for kc in range(8):
        for c0 in range(0, ncols, 1024):
            c1 = min(ncols, c0 + 1024)
            i = n % 2
            n += 1
            stg = c.xt[i]
            S.add("sp", I("dma_start", out=stg[:, 0:c1 - c0], in_=w_dram[kc * 128:(kc + 1) * 128, c0:c1]),
                  writes=["xt%d" % i], dma=True)
            S.add("dve", I("tensor_scalar", out=Wb[:, kc, c0:c1], in0=stg[:, 0:c1 - c0], scalar1=lncol[:, kc:kc + 1],
                           scalar2=None, op0=ALU.mult),
                  reads=["xt%d" % i, "lncol" + tag], writes=[("Wb", kc)])


def rstd_from_sumsq(c, S, st, n, ncol, key):
    S.add("dve", I("tensor_scalar", out=st[:, 0:ncol], in0=st[:, 0:ncol], scalar1=1.0 / n, scalar2=EPS,
                                           op0=ALU.mult, op1=ALU.add), reads=[key], writes=[key])
    S.add("act", I("activation", out=st[:, 0:ncol], in_=st[:, 0:ncol], func=AF.Ln), reads=[key], writes=[key])
    S.add("act", I("activation", out=st[:, 0:ncol], in_=st[:, 0:ncol], func=AF.Exp, scale=-0.5), reads=[key], writes=[key])


def emit_hT(c, x_dram, t, xreads=()):
    S = c.S
    i = t % 2
    xt, hT, st1 = c.xt[i], c.hT[i], c.st1[i]
    xsq, xn, xnk, xsk = c.xsq_[i], c.xn_[i], "xn%d" % i, "xsq%d" % i
    S.add("sp", I("dma_start", out=xt[:], in_=x_dram[t * 128:(t + 1) * 128, :]), reads=list(xreads), writes=["xt%d" % i], dma=True)
    S.add("act", I("activation", out=xsq[:], in_=xt[:], func=AF.Square, accum_out=st1[:, 0:1]),
          reads=["xt%d" % i], writes=[xsk, "st1_%d" % i])
    rstd_from_sumsq(c, S, st1, D, 1, "st1_%d" % i)
    S.add("dve", I("tensor_scalar", out=xn[:], in0=xt[:], scalar1=st1[:, 0:1], scalar2=None, op0=ALU.mult),
          reads=["xt%d" % i, "st1_%d" % i], writes=[xnk])
    pb = c.bank[i].bitcast(BF16)
    for kc in range(8):
        S.add("pe", I("transpose", out=pb[:, kc * 128:(kc + 1) * 128], in_=xn[:, kc * 128:(kc + 1) * 128],
                                                 identity=c.ident[:]),
              reads=[xnk, "ident"], writes=[("bank", i)])
    S.add("act", I("copy", out=hT[:], in_=pb[:, :]), reads=[("bank", i)], writes=["hT%d" % i])


def proj_tile(c, t, Wb, tag, Gqk, QT, KT, v_evac, zs, z_dram=None, kq="QT", kk="KT", gkey="Gqk", stage=0):
    S = c.S
    i = t % 2
    hT = c.hT[i]
    pb = c.bank[2 + i]
    qk32, qksq, qkb, ze = c.qk32_[i][:, 0:256], c.qksq_[i][:, 0:256], c.qkb_[i][:, 0:256], c.ze_[i][:, 0:128]
    k32, ksq, kkb, kze = "qk32_%d" % i, "qksq_%d" % i, "qkb_%d" % i, "ze_%d" % i
    bk = ("bank", 2 + i)
    st8 = c.st8[i]
    if stage != 2:
        for kc in range(8):
            S.add("pe", I("matmul", pb[:, :], lhsT=hT[:, kc * 128:(kc + 1) * 128], rhs=Wb[:, kc, :],
                                                  start=(kc == 0), stop=(kc == 7)),
                  reads=["hT%d" % i] + [("Wb", kc)], writes=[("bank", 2 + i)])
        S.add("act", I("copy", out=qk32[:], in_=pb[:, 0:256]), reads=[bk], writes=[k32])
        S.add("dve", I("tensor_tensor", out=qksq[:], in0=qk32[:], in1=qk32[:], op=ALU.mult), reads=[k32], writes=[ksq])
        S.add("dve", I("tensor_reduce", out=st8[:, 0:4], in_=qksq[:].rearrange("p (g d) -> p g d", d=64), axis=AX.X, op=ALU.add),
              reads=[ksq], writes=["st8_%d" % i])
        rstd_from_sumsq(c, S, st8, 64, 4, "st8_%d" % i)
    if stage == 1:
        return
    for g in range(4):
        S.add("dve", I("scalar_tensor_tensor", out=qkb[:, g * 64:(g + 1) * 64], in0=qk32[:, g * 64:(g + 1) * 64],
                                                           scalar=st8[:, g:g + 1], in1=Gqk[:, g * 64:(g + 1) * 64],
                                                           op0=ALU.mult, op1=ALU.mult),
              reads=[k32, "st8_%d" % i, gkey], writes=[kkb])
    tb = c.bank[4 + i].bitcast(BF16)
    S.add("pe", I("transpose", out=tb[:, 0:128], in_=qkb[:, 0:128], identity=c.ident[:]), reads=[kkb, "ident"], writes=[("bank", 4 + i)])
    S.add("pe", I("transpose", out=tb[:, 128:256], in_=qkb[:, 128:256], identity=c.ident[:]), reads=[kkb, "ident"], writes=[("bank", 4 + i)])
    S.add("act", I("copy", out=QT[:, t * 128:(t + 1) * 128], in_=tb[:, 0:128]), reads=[("bank", 4 + i)], writes=[(kq, t)])
    S.add("dve", I("tensor_copy", out=KT[:, t * 128:(t + 1) * 128], in_=tb[:, 128:256]), reads=[("bank", 4 + i)], writes=[(kk, t)])
    v_evac(t, pb, bk)
    S.add("act", I("activation", out=ze[:], in_=pb[:, 384:512], func=AF.Exp, scale=-1.0), reads=[bk], writes=[kze])
    S.add("dve", I("tensor_scalar", out=ze[:], in0=ze[:], scalar1=1.0, scalar2=None, op0=ALU.add), reads=[kze], writes=[kze])
    S.add("dve", I("reciprocal", out=ze[:], in_=ze[:]), reads=[kze], writes=[kze])
    if z_dram is None:
        S.add("dve", I("tensor_tensor", out=zs[:, t, :], in0=ze[:], in1=pb[:, 384:512], op=ALU.mult), reads=[kze, bk], writes=[("zs", t)])
    else:
        zt = c.zt[i]
        S.add("dve", I("tensor_tensor", out=zt[:, 0:128], in0=ze[:], in1=pb[:, 384:512], op=ALU.mult), reads=[kze, bk], writes=["zt%d" % i])
        S.add("sp", I("dma_start", out=z_dram[t * 128:(t + 1) * 128, 0:128], in_=zt[:, 0:128]), reads=["zt%d" % i], writes=[("zsd", t)], dma=True,
              semkey=("dma", "zt%d" % i))


def attn_A(c, QT, KT, Vaug, zs, maskA, biasA, boff, qws, out_dram, col0, S_len):
    S = c.S
    NU = S_len // 512
    ss = SweepState()
    ocnt = 0
    for U in range(NU):
        mixt = c.mixt[U % 2]
        mk = "mixt%d" % (U % 2)
        for hl in range(2):
            r0 = hl * 64
            Jlo, Jhi = max(0, 4 * U - 16), 4 * U + 3
            oi = 6 + (ocnt % 2)
            ocnt += 1
            ob = c.bank[oi]
            qw = qws[hl]
            nsub = 512 // qw
            qreads = [("QT", 4 * U + j) for j in range(4)]

            def extra(J, U=U):
                return [(c.ident[:], maskA[:, J - (4 * U - 16), :], ["ident", "maskA"])]

            def biasf(J, i, U=U, hl=hl, nsub=nsub):
                bc = boff[hl] + (J - 4 * U + 16) * nsub + i
                return biasA[:, bc:bc + 1], "biasA"

            sweep(c, ss, QT[r0:r0 + 64, U * 512:(U + 1) * 512], qreads, list(range(Jlo, Jhi + 1)),
                  lambda J, r0=r0: KT[r0:r0 + 64, J * 128:(J + 1) * 128], lambda J: [("KT", J)], extra, biasf, qw,
                  lambda J, hl=hl: Vaug[:, J, hl, :], lambda J: [("V", J)], [(oi, j * 65) for j in range(4)], 65)
            rl = c.rl[ocnt % 2]
            rk = "rl%d" % (ocnt % 2)
            S.add("dve", I("reciprocal", out=rl[:, 0:4], in_=ob[:, 0:260].rearrange("p (j d) -> p j d", d=65)[:, :, 64]),
                  reads=[("bank", oi)], writes=[rk])
            for j in range(4):
                S.add("dve", I("scalar_tensor_tensor", out=mixt[:, j, r0:r0 + 64], in0=ob[:, j * 65:j * 65 + 64], scalar=rl[:, j:j + 1],
                               in1=zs[:, 4 * U + j, r0:r0 + 64], op0=ALU.mult, op1=ALU.mult),
                      reads=[("bank", oi), rk, ("zs", 4 * U + j)], writes=[mk])
        S.add("sp", I("dma_start", out=out_dram[U * 512:(U + 1) * 512, col0:col0 + 128].rearrange("(j p) c -> p j c", p=128), in_=mixt[:, :, :]),
              reads=[mk], writes=[("out", col0, U)], dma=True, semkey=("dma", mk))


def attn_B(c, QT, KT, Vb, zs, maskC, biasB, boff, neglam, subG, out_dram, col0, S_len):
    S = c.S
    NU = S_len // 512
    ss = SweepState()
    for U in range(NU):
        mixt = c.mixt[U % 2]
        mk = "mixt%d" % (U % 2)
        for cc in range(2):
            r0 = cc * 64
            Jhi = 4 * U + 3
            obs = [c.bank[3 + 2 * cc], c.bank[4 + 2 * cc]]
            oks = [("bank", 3 + 2 * cc), ("bank", 4 + 2 * cc)]
            qreads = [("QT", 4 * U + j) for j in range(4)]

            def extra(J, U=U):
                d = J - 4 * U
                if d >= 0:
                    return [(c.ident[:], maskC[:, d, :], ["ident", "maskC"])]
                return []

            def biasf(J, i, U=U):
                bc = boff + (J - 4 * U + 4 * (NU - 1))
                return biasB[:, bc:bc + 1], "biasB"

            sweep(c, ss, QT[r0:r0 + 64, U * 512:(U + 1) * 512], qreads, list(range(0, Jhi + 1)),
                  lambda J, r0=r0: KT[r0:r0 + 64, J * 128:(J + 1) * 128], lambda J: [("KT", J)], extra, biasf, 512,
                  lambda J: Vb[:, J, :], lambda J: [("V", J)], [(3 + 2 * cc + j // 2, (j % 2) * 129) for j in range(4)], 129,
                  skip=lambda J, j, U=U: (J - 4 * U) > j)
            oc = c.oc[cc]
            rl = c.rl[cc]
            for hb in range(2):
                S.add("dve", I("reciprocal", out=rl[:, 2 * hb:2 * hb + 2],
                                                                in_=obs[hb][:, 0:258].rearrange("p (j d) -> p j d", d=129)[:, :, 128]),
                      reads=[oks[hb]], writes=["rl%d" % cc])
            for j in range(4):
                ob = obs[j // 2]
                o0 = (j % 2) * 129
                S.add("dve", I("tensor_scalar", out=oc[:, j, :], in0=ob[:, o0:o0 + 128], scalar1=rl[:, j:j + 1],
                                                                                   scalar2=None, op0=ALU.mult),
                      reads=[oks[j // 2], "rl%d" % cc], writes=["oc%d" % cc])
        df = c.df
        S.add("dve", I("scalar_tensor_tensor", out=df[:, :, :], in0=c.oc[1][:, :, :], scalar=neglam[:, 0:1], in1=c.oc[0][:, :, :],
                                                      op0=ALU.mult, op1=ALU.add), reads=["oc0", "oc1", "neglam"], writes=["df"])
        S.add("pool", I("tensor_tensor", out=c.dsq[:, :, :], in0=df[:, :, :], in1=df[:, :, :], op=ALU.mult), reads=["df"], writes=["dsq"])
        st = c.st4
        S.add("dve", I("tensor_reduce", out=st[:, 0:4], in_=c.dsq[:, :, :], axis=AX.X, op=ALU.add), reads=["dsq"], writes=["st4"])
        rstd_from_sumsq(c, S, st, 128, 4, "st4")
        for j in range(4):
            S.add("dve", I("scalar_tensor_tensor", out=df[:, j, :], in0=df[:, j, :], scalar=st[:, j:j + 1], in1=subG[:, :],
                                                               op0=ALU.mult, op1=ALU.mult), reads=["df", "st4", "subG"], writes=["df"])
        S.add("dve", I("tensor_tensor", out=mixt[:, :, :], in0=df[:, :, :], in1=zs[:, 4 * U:4 * U + 4, :], op=ALU.mult),
              reads=["df"] + [("zs", 4 * U + j) for j in range(4)], writes=[mk])
        S.add("sp", I("dma_start",
            out=out_dram[U * 512:(U + 1) * 512, col0:col0 + 128].rearrange("(j p) c -> p j c", p=128), in_=mixt[:, :, :]),
            reads=[mk], writes=[("out", col0, U)], dma=True, semkey=("dma", mk))


def slopes(n):
    return [2.0 ** (-8.0 * (i + 1) / n) for i in range(n)]


QWS_A = [256, 512, 512, 512]


def consts_L0(half, S_len):
    NU = S_len // 512
    k = np.arange(128)[:, None]
    q = np.arange(512)[None, :]
    maskA = np.zeros((128, 20, 512), np.float32)
    for r in range(20):
        dl = q - k + 2048 - 128 * r
        cnt = ((dl >= 0) & (dl <= 128)).astype(np.float32) + ((dl >= 0) & (dl % 4 == 0) & (dl <= 512)) + ((dl >= 0) & (dl % 16 == 0) & (dl <= 2048))
        maskA[:, r, :] = np.where(cnt > 0, np.log(np.maximum(cnt, 1)), NEG)
    maskC = np.zeros((128, 4, 512), np.float32)
    for d in range(4):
        maskC[:, d, :] = np.where(128 * d + k <= q, 0.0, NEG)
    sA = slopes(8)[4 * half:4 * half + 4]
    cols = []
    boffA = []
    kk = np.arange(128, dtype=np.float64)
    for hl in range(4):
        qw = QWS_A[hl]
        nsub = 512 // qw
        boffA.append(len(cols))
        for dd in range(-16, 4):
            for i in range(nsub):
                cols.append(sA[hl] * (kk + 128 * dd - i * qw - qw / 2))
    biasA = np.stack(cols, axis=1).astype(np.float32)
    sB = slopes(4)[2 * half:2 * half + 2]
    cols = []
    for hb in range(2):
        for dd in range(-4 * (NU - 1), 4):
            cols.append(sB[hb] * (kk + 128 * dd - 256))
    biasB = np.stack(cols, axis=1).astype(np.float32)
    return dict(maskA=maskA.astype(ml_dtypes.bfloat16), maskC=maskC.astype(ml_dtypes.bfloat16), biasA=biasA, biasB=biasB), boffA


def build_L0(S_len, debug=False):
    NT = S_len // 128
    NU = S_len // 512
    nc = bass.Bass("TRN2", target_bir_lowering=False)
    st = contextlib.ExitStack()
    with st:
        c = Ctx(nc, st)
        S = c.S
        x = c.din("x", [S_len, D], F32)
        lncol = c.din("lncol", [128, 8], F32)
        wA = [c.din("wA%d" % i, [D, 512], F32) for i in range(2)]
        wB = [c.din("wB%d" % i, [D, 512], F32) for i in range(2)]
        gA = c.din("gA", [1, 256], F32)
        gB = c.din("gB", [1, 256], F32)
        lam = c.din("lam", [1, 256], F32)
        subln = c.din("subln", [1, 128], F32)
        maskA_d = c.din("maskA", [128, 20, 512], BF16)
        maskC_d = c.din("maskC", [128, 4, 512], BF16)
        nbA = sum(20 * (512 // q) for q in QWS_A)
        biasA_d = c.din("biasA", [128, nbA], F32)
        biasB_d = c.din("biasB", [128, 2 * 4 * NU], F32)
        out = c.dout("mixed", [S_len, 512], BF16)
        alloc_common(c)
        Wb = c.sb("Wb", [128, 8, 512], BF16)
        QT = c.sb("QT", [128, S_len], BF16)
        KT = c.sb("KT", [128, S_len], BF16)
        Vsb = c.sb("Vsb", [128, NT * 130], BF16)
        zs = c.sb("zs", [128, NT, 128], BF16)
        maskA = c.sb("maskA_s", [128, 20, 512], BF16)
        maskC = c.sb("maskC_s", [128, 4, 512], BF16)
        biasA = c.sb("biasA_s", [128, nbA], F32)
        biasB = c.sb("biasB_s", [128, 2 * 4 * NU], F32)
        GA = c.sb("GA", [128, 256], F32)
        GB = c.sb("GB", [128, 256], F32)
        lamt = c.sb("lamt", [128, 256], F32)
        lamp = c.sb("lamp", [128, 128], F32)
        lam2 = c.sb("lam2", [128, 2], F32)
        neglam = c.sb("neglam", [128, 1], F32)
        subG = c.sb("subG", [128, 128], F32)
        c.pt = [c.sb("pt%d" % i, [128, 512], BF16) for i in range(4)]
        c.mixt = [c.sb("mixt%d" % i, [128, 4, 128], BF16) for i in range(2)]
        c.rl = [c.sb("rl%d" % i, [128, 4], F32) for i in range(2)]
        c.oc = [c.sb("oc%d" % i, [128, 4, 128], F32) for i in range(2)]
        c.df = c.sb("df", [128, 4, 128], F32)
        c.dsq = c.sb("dsq", [128, 4, 128], F32)
        c.st4 = c.sb("st4", [128, 4], F32)
        S.add("sp", I("dma_start", out=maskA[:], in_=maskA_d), writes=["maskA"], dma=True)
        S.add("sp", I("dma_start", out=maskC[:], in_=maskC_d), writes=["maskC"], dma=True)
        S.add("sp", I("dma_start", out=biasA[:], in_=biasA_d), writes=["biasA"], dma=True)
        S.add("sp", I("dma_start", out=biasB[:], in_=biasB_d), writes=["biasB"], dma=True)
        S.add("sp", I("dma_start", out=GA[:], in_=gA.partition_broadcast(128)), writes=["GA"], dma=True)
        S.add("sp", I("dma_start", out=GB[:], in_=gB.partition_broadcast(128)), writes=["GB"], dma=True)
        S.add("sp", I("dma_start", out=lamt[:], in_=lam.partition_broadcast(128)), writes=["lamt"], dma=True)
        S.add("sp", I("dma_start", out=subG[:], in_=subln.partition_broadcast(128)), writes=["subG"], dma=True)
        S.add("dve", I("tensor_scalar", out=GA[:, 0:128], in0=GA[:, 0:128], scalar1=0.125, scalar2=None, op0=ALU.mult), reads=["GA"], writes=["GA"])
        S.add("dve", I("tensor_scalar", out=GB[:, 0:128], in0=GB[:, 0:128], scalar1=0.125, scalar2=None, op0=ALU.mult), reads=["GB"], writes=["GB"])
        lam_init = 0.8 - 0.6 * math.exp(-0.3 * 0)
        S.add("dve", I("tensor_scalar", out=subG[:], in0=subG[:], scalar1=1.0 - lam_init, scalar2=None, op0=ALU.mult), reads=["subG"], writes=["subG"])
        lv = lamt[:, :].rearrange("p (a b d) -> p a b d", a=2, b=2)
        S.add("dve", I("tensor_tensor", out=lamp[:, :].rearrange("p (a d) -> p a d", a=2), in0=lv[:, :, 0, :], in1=lv[:, :, 1, :], op=ALU.mult),
              reads=["lamt"], writes=["lamp"])
        S.add("dve", I("tensor_reduce", out=lam2[:, 0:2], in_=lamp[:, :].rearrange("p (a d) -> p a d", a=2), axis=AX.X, op=ALU.add),
              reads=["lamp"], writes=["lam2"])
        S.add("act", I("activation", out=lam2[:, 0:2], in_=lam2[:, 0:2], func=AF.Exp), reads=["lam2"], writes=["lam2"])
        S.add("dve", I("tensor_tensor", out=neglam[:, 0:1], in0=lam2[:, 1:2], in1=lam2[:, 0:1], op=ALU.subtract), reads=["lam2"], writes=["neglam"])
        S.add("dve", I("tensor_scalar", out=neglam[:, 0:1], in0=neglam[:, 0:1], scalar1=-lam_init, scalar2=None, op0=ALU.add), reads=["neglam"], writes=["neglam"])

        boffA = []
        o = 0
        for hl in range(4):
            boffA.append(o)
            o += 20 * (512 // QWS_A[hl])

        passes = [("A", 0), ("A", 1), ("B", 0), ("B", 1)]
        for kind, pi in passes:
            tag = "%s%d" % (kind, pi)
            load_weights(c, (wA if kind == "A" else wB)[pi], lncol, Wb, tag)
            G = GA if kind == "A" else GB
            Gk = "GA" if kind == "A" else "GB"
            if kind == "A":
                Vv = Vsb[:, 0:NT * 130].rearrange("p (t h d) -> p t h d", h=2, d=65)
                S.add("pool", I("memset", Vv[:, :, :, 64:65], 1.0), reads=[("V", t) for t in range(NT)], writes=[("V", t) for t in range(NT)])

                def v_evac(t, pb, bk, Vv=Vv):
                    S.add("act", I("copy", out=Vv[:, t, :, 0:64], in_=pb[:, 256:384].rearrange("p (h d) -> p h d", h=2)),
                          reads=[bk], writes=[("V", t)])
            else:
                Vv = Vsb[:, 0:NT * 129].rearrange("p (t d) -> p t d", d=129)
                S.add("pool", I("memset", Vv[:, :, 128:129], 1.0), reads=[("V", t) for t in range(NT)], writes=[("V", t) for t in range(NT)])

                def v_evac(t, pb, bk, Vv=Vv):
                    S.add("act", I("copy", out=Vv[:, t, 0:128], in_=pb[:, 256:384]), reads=[bk], writes=[("V", t)])
            emit_hT(c, x, 0)
            emit_hT(c, x, 1)
            proj_tile(c, 0, Wb, tag, G, QT, KT, v_evac, zs, gkey=Gk, stage=1)
            for t in range(NT):
                if t + 2 < NT:
                    emit_hT(c, x, t + 2)
                if t + 1 < NT:
                    proj_tile(c, t + 1, Wb, tag, G, QT, KT, v_evac, zs, gkey=Gk, stage=1)
                proj_tile(c, t, Wb, tag, G, QT, KT, v_evac, zs, gkey=Gk, stage=2)
            if kind == "A":
                attn_A(c, QT, KT, Vv, zs, maskA, biasA, boffA[2 * pi:2 * pi + 2], QWS_A[2 * pi:2 * pi + 2], out, 128 * pi, S_len)
            else:
                attn_B(c, QT, KT, Vv, zs, maskC, biasB, pi * 4 * NU, neglam, subG, out, 256 + 128 * pi, S_len)
        S.emit()
    return nc


class SweepState:
    def __init__(self):
        self.cnt = 0


def sweep(c, ss, Qap, qreads, Js, Kap, kreads, extra, biasf, qw, Vap, vreads, subs, dv, skip=None, sbanks=(0, 1, 2)):
    S = c.S
    nsub = 512 // qw
    started = set()
    lastJ = {}
    for J in Js:
        for j in range(4):
            if not (skip and skip(J, j)):
                lastJ[j] = J
    slot = {}

    def qk(J):
        si = sbanks[ss.cnt % len(sbanks)]
        pi = ss.cnt % 4
        ss.cnt += 1
        slot[J] = (si, pi)
        sbk = c.bank[si]
        ex = extra(J)
        S.add("pe", I("matmul", sbk[:, :], lhsT=Kap(J), rhs=Qap, start=True, stop=(len(ex) == 0)),
              reads=list(kreads(J)) + list(qreads), writes=[("bank", si)])
        for n, (l_, r_, rd) in enumerate(ex):
            S.add("pe", I("matmul", sbk[:, :], lhsT=l_, rhs=r_, start=False, stop=(n == len(ex) - 1)), reads=list(rd), writes=[("bank", si)])

    def act(J):
        si, pi = slot[J]
        sbk, pt = c.bank[si], c.pt[pi]
        for i in range(nsub):
            bap, bkey = biasf(J, i)
            S.add("act", I("activation", out=pt[:, i * qw:(i + 1) * qw], in_=sbk[:, i * qw:(i + 1) * qw], func=AF.Exp, bias=bap),
                  reads=[("bank", si), bkey], writes=[("pt", pi)])

    def av(J):
        si, pi = slot[J]
        pt = c.pt[pi]
        for j in range(4):
            if skip and skip(J, j):
                continue
            bi, o0 = subs[j]
            S.add("pe", I("matmul", c.bank[bi][:, o0:o0 + dv], lhsT=pt[:, j * 128:(j + 1) * 128], rhs=Vap(J),
                          start=(bi not in started), stop=(J == lastJ[j]), skip_group_check=True),
                  reads=[("pt", pi)] + list(vreads(J)), writes=[("bank", bi)])
            started.add(bi)

    for J in Js[0:2]:
        qk(J)
    for n, J in enumerate(Js):
        if n + 2 < len(Js):
            qk(Js[n + 2])
        act(J)
        av(J)


QWS = [256, 512, 512, 512]


def bias_tables_L1(half, NU):
    sl = slopes(8)[4 * half:4 * half + 4]
    kk = np.arange(128, dtype=np.float64)
    cols, offs = [], []
    for h in range(4):
        qw = QWS[h]
        offs.append(len(cols))
        for dd in range(-4 * (NU - 1), 4):
            for i in range(512 // qw):
                cols.append(sl[h] * (kk + 128 * dd - i * qw - qw / 2))
    bias = np.stack(cols, 1).astype(np.float32)
    cols, offc = [], []
    for h in range(4):
        qw = QWS[h]
        offc.append(len(cols))
        for e in range(NU):
            for i in range(512 // qw):
                cols.append(sl[h] * (16 * kk + 31 - 512 * e - i * qw - qw / 2))
    biasc = np.stack(cols, 1).astype(np.float32)
    return bias, offs, biasc, offc


def consts_L1(half, S_len):
    NU = S_len // 512
    k = np.arange(128)[:, None]
    q = np.arange(512)[None, :]
    maskC = np.zeros((128, 4, 512), np.float32)
    for d in range(4):
        maskC[:, d, :] = np.where(128 * d + k <= q, 0.0, NEG)
    maskW = np.zeros((128, 8, 512), np.float32)
    for r in range(8):
        dl = q - k + 128 * (4 - r)
        maskW[:, r, :] = np.where((dl >= 0) & (dl <= 511), 0.0, NEG)
    maskK = np.zeros((128, 5, 512), np.float32)
    for e in range(5):
        maskK[:, e, :] = np.where(16 * k + 31 <= 512 * e + q, 0.0, NEG)
    bias, offs, biasc, offc = bias_tables_L1(half, NU)
    cidx = np.arange(S_len)[None, :]
    E32 = np.zeros((128, S_len), np.float32)
    E32[:32] = (cidx // 256 == np.arange(32)[:, None])
    E64 = (cidx // 64 == np.arange(128)[:, None]).astype(np.float32)
    PB = np.zeros((128, 64), np.float32)
    PB[:, 32:] = -1e30
    n = np.arange(512)[:, None]
    j = np.arange(128)[None, :]
    cis = ((16 * n <= 64 * j + 63) & (16 * n + 31 >= 64 * j) & (n < 511)).astype(np.float32)
    cis = cis.reshape(4, 128, 128).transpose(1, 0, 2)
    qh = (np.arange(128) // 64)[:, None]
    r = np.arange(255)[None, :] - 127
    T = np.where(r == qh, 1e9, np.where(r == qh - 1, 3e9, np.where(r > qh, -1e30, 0.0))).astype(np.float32)
    T = np.concatenate([T, np.zeros((128, 1), np.float32)], 1)
    return dict(maskC=maskC.astype(ml_dtypes.bfloat16), maskW=maskW.astype(ml_dtypes.bfloat16), maskK=maskK.astype(ml_dtypes.bfloat16),
                bias1=bias, biasc=biasc, E32=E32.astype(ml_dtypes.bfloat16), E64=E64.astype(ml_dtypes.bfloat16), PB=PB,
                cis=cis.astype(ml_dtypes.bfloat16), Ttab=T), offs, offc


def proj_tile_D(c, t, Wb, GD, QT0, QT1, KsT, KwT, kvcT, Vs, Vw, zs_dram, gsig, stage=0):
    S = c.S
    i = t % 2
    hT = c.hT[i]
    X, Y, Z = c.bank[2 + i], c.bank[4 + i], c.bank[6]
    bx, by, bz = ("bank", 2 + i), ("bank", 4 + i), ("bank", 6)
    st8 = c.st8[i]
    qk32d, qksqd, qkbd, zed = c.qk32_[i], c.qksq_[i], c.qkb_[i], c.ze_[i]
    k32, ksq, kkb, kze = "qk32_%d" % i, "qksq_%d" % i, "qkb_%d" % i, "ze_%d" % i
    if stage != 2:
        for bi, pb, c0, n in ((2 + i, X, 0, 512), (4 + i, Y, 512, 512), (6, Z, 1024, 12)):
            for kc in range(8):
                S.add("pe", I("matmul", pb[:, 0:n], lhsT=hT[:, kc * 128:(kc + 1) * 128], rhs=Wb[:, kc, c0:c0 + n], start=(kc == 0), stop=(kc == 7)),
                      reads=["hT%d" % i, ("Wb", kc)], writes=[("bank", bi)])
        S.add("act", I("copy", out=qk32d[:], in_=X[:, :]), reads=[bx], writes=[k32])
        S.add("pool", I("tensor_tensor", out=qksqd[:], in0=qk32d[:], in1=qk32d[:], op=ALU.mult), reads=[k32], writes=[ksq])
        S.add("dve", I("tensor_reduce", out=st8[:, 0:8], in_=qksqd[:].rearrange("p (g d) -> p g d", d=64), axis=AX.X, op=ALU.add),
              reads=[ksq], writes=["st8_%d" % i])
        rstd_from_sumsq(c, S, st8, 64, 8, "st8_%d" % i)
        S.add("act", I("activation", out=gsig[:, t, :], in_=Z[:, 0:12], func=AF.Exp, scale=-1.0), reads=[bz], writes=[("gsig", t)])
        S.add("dve", I("tensor_scalar", out=gsig[:, t, :], in0=gsig[:, t, :], scalar1=1.0, scalar2=None, op0=ALU.add), reads=[("gsig", t)], writes=[("gsig", t)])
        S.add("dve", I("reciprocal", out=gsig[:, t, :], in_=gsig[:, t, :]), reads=[("gsig", t)], writes=[("gsig", t)])
    if stage == 1:
        return
    for g in range(8):
        S.add("dve", I("scalar_tensor_tensor", out=qkbd[:, g * 64:(g + 1) * 64], in0=qk32d[:, g * 64:(g + 1) * 64],
                       scalar=st8[:, g:g + 1], in1=GD[:, g * 64:(g + 1) * 64], op0=ALU.mult, op1=ALU.mult),
              reads=[k32, "st8_%d" % i, "GD"], writes=[kkb])
    S.add("act", I("copy", out=qkbd[:, 512:640], in_=Y[:, 0:128]), reads=[by], writes=[kkb])
    tb = c.bank[7].bitcast(BF16)
    for n in range(5):
        S.add("pe", I("transpose", out=tb[:, n * 128:(n + 1) * 128], in_=qkbd[:, n * 128:(n + 1) * 128], identity=c.ident[:]),
              reads=[kkb, "ident"], writes=[("bank", 7)])
    sl = slice(t * 128, (t + 1) * 128)
    S.add("act", I("copy", out=QT0[:, sl], in_=tb[:, 0:128]), reads=[("bank", 7)], writes=[("B0", t)])
    S.add("dve", I("tensor_copy", out=QT1[:, sl], in_=tb[:, 128:256]), reads=[("bank", 7)], writes=[("B1", t)])
    S.add("act", I("copy", out=KsT[:, sl], in_=tb[:, 256:384]), reads=[("bank", 7)], writes=[("B2", t)])
    S.add("dve", I("tensor_copy", out=KwT[:, sl], in_=tb[:, 384:512]), reads=[("bank", 7)], writes=[("B3", t)])
    S.add("act", I("copy", out=kvcT[:, sl], in_=tb[:, 512:640]), reads=[("bank", 7)], writes=[("B4", t)])
    S.add("act", I("copy", out=Vs[:, t, 0:64], in_=Y[:, 128:192]), reads=[by], writes=[("V", t)])
    S.add("act", I("copy", out=Vw[:, t, 0:64], in_=Y[:, 192:256]), reads=[by], writes=[("V", t)])
    zt = c.zt[i]
    S.add("act", I("activation", out=zed[:], in_=Y[:, 256:512], func=AF.Exp, scale=-1.0), reads=[by], writes=[kze])
    S.add("dve", I("tensor_scalar", out=zed[:], in0=zed[:], scalar1=1.0, scalar2=None, op0=ALU.add), reads=[kze], writes=[kze])
    S.add("dve", I("reciprocal", out=zed[:], in_=zed[:]), reads=[kze], writes=[kze])
    S.add("dve", I("tensor_tensor", out=zt[:, 0:256], in0=zed[:], in1=Y[:, 256:512], op=ALU.mult), reads=[kze, by], writes=["zt%d" % i])
    S.add("sp", I("dma_start", out=zs_dram[t * 128:(t + 1) * 128, 0:256], in_=zt[:, 0:256]), reads=["zt%d" % i], writes=[("zsd", t)], dma=True,
          semkey=("dma", "zt%d" % i))


def attn_C(c, QT, KT, Vaug, zs_dram, E32, maskC, bias1, boffs, qws, kmT, PB, out_dram, col0, S_len):
    S = c.S
    NU = S_len // 512
    NT = S_len // 128
    ss = SweepState()
    ocnt = 0
    for U in range(NU):
        mixt = c.mixt[U % 2]
        mk = "mixt%d" % (U % 2)
        zt = c.zu[U % 2]
        zk = "zu%d" % (U % 2)
        S.add("sp", I("dma_start", out=zt[:, :, 0:128], in_=zs_dram[U * 512:(U + 1) * 512, 0:128].rearrange("(j p) c -> p j c", p=128)),
              reads=[("zsd", 4 * U + j) for j in range(4)], writes=[zk], dma=True)
        for hl in range(2):
            r0 = hl * 64
            qreads = [("B0", 4 * U + j) for j in range(4)]
            gb = c.bank[7]
            for j in range(4):
                S.add("pe", I("matmul", gb[:, j * 32:(j + 1) * 32], lhsT=QT[r0:r0 + 64, (4 * U + j) * 128:(4 * U + j + 1) * 128],
                              rhs=kmT[r0:r0 + 64, 0:32], start=True, stop=True), reads=qreads + ["kmT"], writes=[("bank", 7)])
            for j in range(4):
                own = (4 * U + j) // 2
                S.add("dve", I("tensor_tensor", out=c.gm[:, j, :], in0=gb[:, j * 32:(j + 1) * 32], in1=PB[:, 32 - own:64 - own], op=ALU.add),
                      reads=[("bank", 7), "PB"], writes=["gm"])
            for j in range(4):
                S.add("dve", I("max", out=c.mx[:, j, :], in_=c.gm[:, j, :]), reads=["gm"], writes=["mx"])
            for j in range(4):
                S.add("dve", I("tensor_scalar", out=c.mb[:, j, 0:32], in0=c.gm[:, j, :], scalar1=c.mx[:, j, 2:3], scalar2=NEG, op0=ALU.is_lt, op1=ALU.mult),
                      reads=["gm", "mx"], writes=["mb"])
            S.add("dve", I("memset", c.mb[:, 0:2, 2 * U:2 * U + 1], 0.0), reads=["mb"], writes=["mb"])
            S.add("dve", I("memset", c.mb[:, 2:4, 2 * U + 1:2 * U + 2], 0.0), reads=["mb"], writes=["mb"])
            tb = c.bank[7].bitcast(BF16)
            for j in range(4):
                S.add("pe", I("transpose", out=tb[0:32, 512 + j * 128:512 + (j + 1) * 128], in_=c.mb[:, j, 0:32], identity=c.ident[:]),
                      reads=["mb", "ident"], writes=[("bank", 7)])
            MBT = c.MBT[ocnt % 2]
            mbk = "MBT%d" % (ocnt % 2)
            S.add("act", I("copy", out=MBT[0:32, :], in_=tb[0:32, 512:1024]), reads=[("bank", 7)], writes=[mbk])
            oi = 3 + (ocnt % 2)
            ocnt += 1
            qw = qws[hl]
            nsub = 512 // qw

            def extra(J, MBT=MBT, mbk=mbk, U=U):
                ex = [(E32[:, J * 128:(J + 1) * 128], MBT[:, :], ["Ebuf", mbk])]
                d = J - 4 * U
                if d >= 0:
                    ex.append((c.ident[:], maskC[:, d, :], ["ident", "maskC"]))
                return ex

            def biasf(J, i, U=U, hl=hl, nsub=nsub):
                col = boffs[hl] + (J - 4 * U + 4 * (NU - 1)) * nsub + i
                return bias1[:, col:col + 1], "bias1"

            sweep(c, ss, QT[r0:r0 + 64, U * 512:(U + 1) * 512], qreads, list(range(0, 4 * U + 4)),
                  lambda J: KT[r0:r0 + 64, J * 128:(J + 1) * 128], lambda J: [("B1", J)], extra, biasf, qw,
                  lambda J: Vaug[:, J, hl, :], lambda J: [("V", J)], [(oi, j * 65) for j in range(4)], 65,
                  skip=lambda J, j, U=U: (J - 4 * U) > j)
            ob = c.bank[oi]
            rl = c.rl[ocnt % 2]
            rk = "rl%d" % (ocnt % 2)
            S.add("dve", I("reciprocal", out=rl[:, 0:4], in_=ob[:, 0:260].rearrange("p (j d) -> p j d", d=65)[:, :, 64]), reads=[("bank", oi)], writes=[rk])
            for j in range(4):
                S.add("dve", I("scalar_tensor_tensor", out=mixt[:, j, r0:r0 + 64], in0=ob[:, j * 65:j * 65 + 64], scalar=rl[:, j:j + 1],
                               in1=zt[:, j, r0:r0 + 64], op0=ALU.mult, op1=ALU.mult), reads=[("bank", oi), rk, zk], writes=[mk])
        S.add("sp", I("dma_start", out=out_dram[U * 512:(U + 1) * 512, col0:col0 + 128].rearrange("(j p) c -> p j c", p=128), in_=mixt[:, :, 0:128]),
              reads=[mk], writes=[("out", col0, U)], dma=True, semkey=("dma", mk))


def nsa_compress(c, kvcT, W1b, W2kk, W2v, peT, gk3col, BO, kcmpT, Vc, S_len):
    S = c.S
    NC = S_len // 16 - 1
    NT = S_len // 128
    allkv = [("B4", t) for t in range(NT)]
    wk = [("Wb", kc) for kc in range(8)]
    b1 = c.b1
    for s, bi in ((0, 3), (1, 4)):
        rows = slice(64 * s, 64 * s + 64)
        for i in range(32):
            S.add("pe", I("matmul", c.bank[bi][:, 0:1], lhsT=W1b[rows, i, :], rhs=peT[rows, i:i + 1], start=(i == 0), stop=(i == 31)),
                  reads=wk + ["peT"], writes=[("bank", bi)])
        S.add("act", I("copy", out=b1[:, s:s + 1], in_=c.bank[bi][:, 0:1]), reads=[("bank", bi)], writes=["b1"])
    S.add("dve", I("tensor_scalar", out=b1[:, 2:4], in0=b1[:, 0:2], scalar1=-1.0, scalar2=None, op0=ALU.mult), reads=["b1"], writes=["b1"])
    hs = c.hs
    for s, bi in ((0, 5), (1, 6)):
        rows = slice(64 * s, 64 * s + 64)
        hb = c.bank[bi]
        for i in range(32):
            S.add("pe", I("matmul", hb[:, 0:NC], lhsT=W1b[rows, i, :], rhs=kvcT[rows, i:i + 16 * (NC - 1) + 1:16], start=(i == 0), stop=(i == 31)),
                  reads=wk + allkv, writes=[("bank", bi)])
        S.add("act", I("activation", out=c.he[:, 0:NC], in_=hb[:, 0:NC], func=AF.Exp, scale=-1.0, bias=b1[:, 2 + s:3 + s]), reads=[("bank", bi), "b1"], writes=["he"])
        S.add("dve", I("tensor_scalar", out=c.he[:, 0:NC], in0=c.he[:, 0:NC], scalar1=1.0, scalar2=None, op0=ALU.add), reads=["he"], writes=["he"])
        S.add("dve", I("reciprocal", out=c.he[:, 0:NC], in_=c.he[:, 0:NC]), reads=["he"], writes=["he"])
        S.add("dve", I("memset", hs[s][:, :], 0.0), writes=["hs%d" % s])
        S.add("dve", I("scalar_tensor_tensor", out=hs[s][:, 0:NC], in0=hb[:, 0:NC], scalar=b1[:, s:s + 1], in1=c.he[:, 0:NC], op0=ALU.add, op1=ALU.mult),
              reads=[("bank", bi), "b1", "he", "hs%d" % s], writes=["hs%d" % s])
    kb = c.bank[3]
    S.add("pe", I("matmul", kb[:, 0:NC], lhsT=W2kk[:, :], rhs=hs[0][:, 0:NC], start=True, stop=True), reads=["W2", "hs0"], writes=[("bank", 3)])
    S.add("act", I("activation", out=c.ksq[:, 0:NC], in_=kb[:, 0:NC], func=AF.Square), reads=[("bank", 3)], writes=["ksq"])
    S.add("pe", I("matmul", c.bank[4][:, 0:NC], lhsT=BO[:, :], rhs=c.ksq[:, 0:NC], start=True, stop=True), reads=["BO", "ksq"], writes=[("bank", 4)])
    S.add("dve", I("tensor_scalar", out=c.he[:, 0:NC], in0=c.bank[4][:, 0:NC], scalar1=1.0 / 64, scalar2=EPS, op0=ALU.mult, op1=ALU.add),
          reads=[("bank", 4)], writes=["he"])
    S.add("act", I("activation", out=c.he[:, 0:NC], in_=c.he[:, 0:NC], func=AF.Ln), reads=["he"], writes=["he"])
    S.add("act", I("activation", out=c.he[:, 0:NC], in_=c.he[:, 0:NC], func=AF.Exp, scale=-0.5), reads=["he"], writes=["he"])
    S.add("dve", I("memset", kcmpT[:, :], 0.0), writes=["kcmpT"])
    S.add("dve", I("scalar_tensor_tensor", out=kcmpT[:, 0:NC], in0=kb[:, 0:NC], scalar=gk3col[:, 0:1], in1=c.he[:, 0:NC], op0=ALU.mult, op1=ALU.mult),
          reads=[("bank", 3), "gk3col", "he", "kcmpT"], writes=["kcmpT"])
    nch = (NC + 127) // 128
    for cj in range(nch):
        S.add("pe", I("matmul", c.bank[5][:, cj * 64:(cj + 1) * 64], lhsT=hs[1][:, cj * 128:(cj + 1) * 128], rhs=W2v[:, :], start=True, stop=True),
              reads=["hs1", "W2"], writes=[("bank", 5)])
    S.add("act", I("copy", out=Vc[:, 0:nch, 0:64], in_=c.bank[5][:, 0:nch * 64].rearrange("p (c d) -> p c d", d=64)), reads=[("bank", 5)], writes=["Vc"])


def attn_D(c, QTs, KsT, KwT, kcmpT, Vs, Vw, Vc, zs_dram, gsig, E64, maskC, maskW, maskK, bias1, boffs, biasc, boffc, Ttab, out_dram, S_len):
    S = c.S
    NU = S_len // 512
    ss = SweepState()
    NCH = (S_len // 16 - 1 + 127) // 128
    for U in range(NU):
        mixt = c.mixt[U % 2]
        mk = "mixt%d" % (U % 2)
        zt = c.zu[U % 2]
        zk = "zu%d" % (U % 2)
        S.add("sp", I("dma_start", out=zt[:, :, :], in_=zs_dram[U * 512:(U + 1) * 512, 0:256].rearrange("(j p) c -> p j c", p=128)),
              reads=[("zsd", 4 * U + j) for j in range(4)], writes=[zk], dma=True)
        acc, imp = c.acc, c.imp
        gk = [("gsig", 4 * U + j) for j in range(4)]
        for h in range(4):
            QT = QTs[h // 2]
            qkey = "B%d" % (h // 2)
            r0 = (h % 2) * 64
            qreads = [(qkey, 4 * U + j) for j in range(4)]
            qw = QWS[h]
            nsub = 512 // qw
            chunks = [cj for cj in range(NCH) if U - 4 * cj >= 0]

            def extra(cj, U=U):
                e = U - 4 * cj
                if e <= 4:
                    return [(c.ident[:], maskK[:, e, :], ["ident", "maskK"])]
                return []

            def biasf(cj, i, U=U, h=h, nsub=nsub):
                col = boffc[h] + (U - 4 * cj) * nsub + i
                return biasc[:, col:col + 1], "biasc"

            subs = [(5 + j // 2, (j % 2) * 193) for j in range(4)]
            sweep(c, ss, QT[r0:r0 + 64, U * 512:(U + 1) * 512], qreads, chunks,
                  lambda cj: kcmpT[r0:r0 + 64, cj * 128:(cj + 1) * 128], lambda cj: ["kcmpT"], extra, biasf, qw,
                  lambda cj: Vc[:, cj, :], lambda cj: ["Vc"], subs, 193)
            rl = c.rl[h % 2]
            rk = "rl%d" % (h % 2)
            for bi in (5, 6):
                S.add("dve", I("tensor_scalar", out=rl[:, 2 * (bi - 5):2 * (bi - 5) + 2], in0=c.bank[bi][:, 0:386].rearrange("p (j d) -> p j d", d=193)[:, :, 64],
                               scalar1=1e-30, scalar2=None, op0=ALU.max), reads=[("bank", bi)], writes=[rk])
            S.add("dve", I("reciprocal", out=rl[:, 0:4], in_=rl[:, 0:4]), reads=[rk], writes=[rk])
            for j in range(4):
                bi, o0 = subs[j]
                ob = c.bank[bi]
                S.add("dve", I("tensor_scalar", out=acc[:, j, h, :], in0=ob[:, o0:o0 + 64], scalar1=rl[:, j:j + 1], scalar2=gsig[:, 4 * U + j, 3 * h:3 * h + 1],
                               op0=ALU.mult, op1=ALU.mult), reads=[("bank", bi), rk] + gk, writes=["acc"])
                if h == 0:
                    S.add("dve", I("tensor_scalar", out=imp[:, j, :], in0=ob[:, o0 + 65:o0 + 193], scalar1=rl[:, j:j + 1], scalar2=None, op0=ALU.mult),
                          reads=[("bank", bi), rk], writes=["imp"])
                else:
                    S.add("dve", I("scalar_tensor_tensor", out=imp[:, j, :], in0=ob[:, o0 + 65:o0 + 193], scalar=rl[:, j:j + 1], in1=imp[:, j, :],
                                   op0=ALU.mult, op1=ALU.add), reads=[("bank", bi), rk, "imp"], writes=["imp"])
        MBT = c.MBT[U % 2]
        mbk = "MBT%d" % (U % 2)
        tb = c.bank[7].bitcast(BF16)
        for j in range(4):
            tp = 4 * U + j
            S.add("dve", I("tensor_tensor", out=c.impb[:, :], in0=imp[:, j, :], in1=Ttab[:, 127 - 2 * tp:255 - 2 * tp], op=ALU.add), reads=["imp", "Ttab"], writes=["impb"])
            S.add("dve", I("memset", c.impb[:, 0:1], 2e9), reads=["impb"], writes=["impb"])
            S.add("dve", I("max", out=c.mx16[:, 0:8], in_=c.impb[:, :]), reads=["impb"], writes=["mx16"])
            S.add("dve", I("match_replace", out=c.impc[:, :], in_to_replace=c.mx16[:, 0:8], in_values=c.impb[:, :], imm_value=-3e38),
                  reads=["impb", "mx16"], writes=["impc"])
            S.add("dve", I("max", out=c.mx16[:, 8:16], in_=c.impc[:, :]), reads=["impc", "mx16"], writes=["mx16"])
            S.add("dve", I("tensor_scalar", out=c.mb[:, j, :], in0=c.impb[:, :], scalar1=c.mx16[:, 15:16], scalar2=NEG, op0=ALU.is_lt, op1=ALU.mult),
                  reads=["impb", "mx16"], writes=["mb"])
            S.add("pe", I("transpose", out=tb[:, j * 128:(j + 1) * 128], in_=c.mb[:, j, :], identity=c.ident[:]), reads=["mb", "ident"], writes=[("bank", 7)])
        S.add("act", I("copy", out=MBT[:, :], in_=tb[:, 0:512]), reads=[("bank", 7)], writes=[mbk])
        for br in range(2):
            for h in range(4):
                QT = QTs[h // 2]
                qkey = "B%d" % (h // 2)
                r0 = (h % 2) * 64
                qreads = [(qkey, 4 * U + j) for j in range(4)]
                qw = QWS[h]
                nsub = 512 // qw
                oi = 3 + (h % 2)

                def biasf(J, i, U=U, h=h, nsub=nsub):
                    col = boffs[h] + (J - 4 * U + 4 * (NU - 1)) * nsub + i
                    return bias1[:, col:col + 1], "bias1"

                if br == 0:
                    Js = list(range(0, 4 * U + 4))

                    def extra(J, U=U, MBT=MBT, mbk=mbk):
                        ex = [(E64[:, J * 128:(J + 1) * 128], MBT[:, :], ["Ebuf", mbk])]
                        d = J - 4 * U
                        if d >= 0:
                            ex.append((c.ident[:], maskC[:, d, :], ["ident", "maskC"]))
                        return ex
                    KTt, kkey, Vt = KsT, "B2", Vs
                else:
                    Js = list(range(max(0, 4 * U - 4), 4 * U + 4))

                    def extra(J, U=U):
                        return [(c.ident[:], maskW[:, J - (4 * U - 4), :], ["ident", "maskW"])]
                    KTt, kkey, Vt = KwT, "B3", Vw
                sweep(c, ss, QT[r0:r0 + 64, U * 512:(U + 1) * 512], qreads, Js,
                      lambda J, KTt=KTt, r0=r0: KTt[r0:r0 + 64, J * 128:(J + 1) * 128], lambda J, kkey=kkey: [(kkey, J)], extra, biasf, qw,
                      lambda J, Vt=Vt: Vt[:, J, :], lambda J: [("V", J)], [(oi, j * 65) for j in range(4)], 65,
                      skip=lambda J, j, U=U: (J - 4 * U) > j)
                ob = c.bank[oi]
                rl = c.rl[h % 2]
                rk = "rl%d" % (h % 2)
                S.add("dve", I("reciprocal", out=rl[:, 0:4], in_=ob[:, 0:260].rearrange("p (j d) -> p j d", d=65)[:, :, 64]), reads=[("bank", oi)], writes=[rk])
                for j in range(4):
                    S.add("dve", I("tensor_scalar", out=c.otmp[:, :], in0=ob[:, j * 65:j * 65 + 64], scalar1=rl[:, j:j + 1],
                                   scalar2=gsig[:, 4 * U + j, 3 * h + 1 + br:3 * h + 2 + br], op0=ALU.mult, op1=ALU.mult),
                          reads=[("bank", oi), rk] + gk, writes=["otmp"])
                    S.add("dve", I("tensor_tensor", out=acc[:, j, h, :], in0=acc[:, j, h, :], in1=c.otmp[:, :], op=ALU.add), reads=["otmp", "acc"], writes=["acc"])
        S.add("dve", I("tensor_tensor", out=mixt[:, :, :], in0=acc[:, :, :, :].rearrange("p j h d -> p j (h d)"), in1=zt[:, :, :], op=ALU.mult),
              reads=["acc", zk], writes=[mk])
        S.add("sp", I("dma_start", out=out_dram[U * 512:(U + 1) * 512, 256:512].rearrange("(j p) c -> p j c", p=128), in_=mixt[:, :, :]),
              reads=[mk], writes=[("out", 256, U)], dma=True, semkey=("dma", mk))


def build_L1(S_len):
    NT = S_len // 128
    NU = S_len // 512
    NB = S_len // 256
    nc = bass.Bass("TRN2", target_bir_lowering=False)
    st = contextlib.ExitStack()
    with st:
        c = Ctx(nc, st)
        S = c.S
        x = c.din("x", [S_len, D], F32)
        m0T = c.din("m0T", [D, S_len], BF16)
        woe = c.din("woe", [D, D], F32)
        lncol = c.din("lncol", [128, 8], F32)
        wC = [c.din("wC%d" % i, [D, 512], F32) for i in range(2)]
        wD = c.din("wD", [D, 1036], F32)
        gC_d = c.din("gC", [1, 256], F32)
        gD_d = c.din("gD", [1, 512], F32)
        W1_d = c.din("W1cat", [128, 32, 128], F32)
        W2_d = c.din("W2cat", [128, 192], F32)
        pe_d = c.din("peT", [128, 32], F32)
        gk3_d = c.din("gk3col", [128, 1], F32)
        BO_d = c.din("BO", [128, 128], BF16)
        cshape = dict(maskC=[128, 4, 512], maskW=[128, 8, 512], maskK=[128, 5, 512], E32=[128, S_len], E64=[128, S_len], cis=[128, 4, 128])
        cd = {k: c.din(k, v, BF16) for k, v in cshape.items()}
        nb1 = sum((4 * NU) * (512 // q) for q in QWS)
        nbc = sum(NU * (512 // q) for q in QWS)
        bias1_d = c.din("bias1", [128, nb1], F32)
        biasc_d = c.din("biasc", [128, nbc], F32)
        PB_d = c.din("PB", [128, 64], F32)
        T_d = c.din("Ttab", [128, 256], F32)
        x1 = c.dout("x1", [S_len, D], F32)
        out = c.dout("mixed", [S_len, 512], BF16)
        zsd = c.dout("zsd", [S_len, 256], BF16)
        alloc_common(c, qkw=512, bkw=640, zw=256)
        Wb = c.sb("Wb", [128, 8, 1040], BF16)
        Bt = [c.sb("B%d" % i, [128, S_len], BF16) for i in range(5)]
        Vsb = c.sb("Vsb", [128, NT * 130], BF16)
        maskC = c.sb("maskC_s", [128, 4, 512], BF16)
        maskW = c.sb("maskW_s", [128, 8, 512], BF16)
        maskK = c.sb("maskK_s", [128, 5, 512], BF16)
        bias1 = c.sb("bias1_s", [128, nb1], F32)
        biasc = c.sb("biasc_s", [128, nbc], F32)
        PB = c.sb("PB_s", [128, 64], F32)
        Ttab = c.sb("T_s", [128, 256], F32)
        GC = c.sb("GC", [128, 256], F32)
        GD = c.sb("GD", [128, 512], F32)
        BO = c.sb("BO_s", [128, 128], BF16)
        W2f = c.sb("W2f", [128, 192], F32)
        W2b = c.sb("W2b", [128, 192], BF16)
        pef = c.sb("pef", [128, 32], F32)
        peT = c.sb("peTb", [128, 32], BF16)
        gk3col = c.sb("gk3", [128, 1], F32)
        c.b1 = c.sb("b1", [128, 4], F32)
        c.he = c.sb("he", [128, 512], F32)
        c.hs = [c.sb("hs%d" % i, [128, 512], BF16) for i in range(2)]
        c.ksq = c.sb("ksq", [128, 512], BF16)
        kcmpT = c.sb("kcmpT", [128, 512], BF16)
        Vc = c.sb("Vc", [128, 4, 193], BF16)
        gsig = c.sb("gsig", [128, NT, 12], F32)
        kmf = c.sb("kmf", [128, 32], F32)
        kmT = c.sb("kmT", [128, 32], BF16)
        c.pt = [c.sb("pt%d" % i, [128, 512], BF16) for i in range(4)]
        c.mixt = [c.sb("mixt%d" % i, [128, 4, 256], BF16) for i in range(2)]
        c.zu = [c.sb("zu%d" % i, [128, 4, 256], BF16) for i in range(2)]
        c.zt = [c.sb("zt%d" % i, [128, 256], BF16) for i in range(2)]
        c.rl = [c.sb("rl%d" % i, [128, 4], F32) for i in range(2)]
        c.MBT = [c.sb("MBT%d" % i, [128, 512], BF16) for i in range(2)]
        c.gm = c.sb("gm", [128, 4, 32], F32)
        c.mx = c.sb("mx", [128, 4, 8], F32)
        c.mb = c.sb("mb", [128, 4, 128], BF16)
        c.mx16 = c.sb("mx16", [128, 16], F32)
        c.imp = c.sb("imp", [128, 4, 128], F32)
        c.impb = c.sb("impb", [128, 128], F32)
        c.impc = c.sb("impc", [128, 128], F32)
        c.acc = c.sb("acc", [128, 4, 4, 64], F32)
        c.otmp = c.sb("otmp", [128, 64], F32)
        m0 = [c.hT[i][:, :].rearrange("p (k t) -> p k t", k=8) for i in range(2)]

        def ld(dst, src, key):
            S.add("sp", I("dma_start", out=dst, in_=src), writes=[key], dma=True)
        ld(maskC[:], cd["maskC"], "maskC"); ld(maskW[:], cd["maskW"], "maskW"); ld(maskK[:], cd["maskK"], "maskK")
        ld(bias1[:], bias1_d, "bias1"); ld(biasc[:], biasc_d, "biasc"); ld(PB[:], PB_d, "PB"); ld(Ttab[:], T_d, "Ttab")
        ld(GC[:], gC_d.partition_broadcast(128), "GC"); ld(GD[:], gD_d.partition_broadcast(128), "GD")
        ld(BO[:], BO_d, "BO"); ld(W2f[:], W2_d, "W2f"); ld(pef[:], pe_d, "pef"); ld(gk3col[:], gk3_d, "gk3col")
        S.add("dve", I("tensor_scalar", out=GC[:, 0:128], in0=GC[:, 0:128], scalar1=0.125, scalar2=None, op0=ALU.mult), reads=["GC"], writes=["GC"])
        S.add("dve", I("tensor_scalar", out=GD[:, 0:256], in0=GD[:, 0:256], scalar1=0.125, scalar2=None, op0=ALU.mult), reads=["GD"], writes=["GD"])
        S.add("dve", I("tensor_copy", out=W2b[:], in_=W2f[:]), reads=["W2f"], writes=["W2"])
        S.add("dve", I("tensor_copy", out=peT[:], in_=pef[:]), reads=["pef"], writes=["peT"])
        S.add("pool", I("memset", Vc[:, :, :], 0.0), writes=["Vc"])
        S.add("pool", I("memset", Vc[:, :, 64:65], 1.0), reads=["Vc"], writes=["Vc"])
        S.add("sp", I("dma_start", out=Vc[:, :, 65:193], in_=cd["cis"]), reads=["Vc"], writes=["Vc"], dma=True)
        for i in range(2):
            S.add("pool", I("memset", c.MBT[i][:, :], 0.0), writes=["MBT%d" % i])

        for kc in range(8):
            i = kc % 2
            S.add("sp", I("dma_start", out=c.xt[i][:, :], in_=woe[kc * 128:(kc + 1) * 128, :]), writes=["xt%d" % i], dma=True)
            S.add("dve", I("tensor_copy", out=Wb[:, kc, 0:1024], in_=c.xt[i][:, :]), reads=["xt%d" % i], writes=[("Wb", kc)])
        for t in range(NT):
            i = t % 2
            xt = c.xt[i]
            S.add("sp", I("dma_start", out=xt[:], in_=x[t * 128:(t + 1) * 128, :]), writes=["xt%d" % i], dma=True)
            S.add("sp", I("dma_start", out=m0[i][:, :, :], in_=m0T[:, t * 128:(t + 1) * 128].rearrange("(kc p) t -> p kc t", p=128)),
                  writes=["hT%d" % i], dma=True)
            for cg in range(2):
                bi = 2 + 2 * i + cg
                for kc in range(8):
                    S.add("pe", I("matmul", c.bank[bi][:, :], lhsT=m0[i][:, kc, :], rhs=Wb[:, kc, cg * 512:(cg + 1) * 512], start=(kc == 0), stop=(kc == 7)),
                          reads=["hT%d" % i, ("Wb", kc)], writes=[("bank", bi)])
                S.add("dve", I("tensor_tensor", out=xt[:, cg * 512:(cg + 1) * 512], in0=xt[:, cg * 512:(cg + 1) * 512], in1=c.bank[bi][:, :], op=ALU.add),
                      reads=["xt%d" % i, ("bank", bi)], writes=["xt%d" % i])
            S.add("sp", I("dma_start", out=x1[t * 128:(t + 1) * 128, :], in_=xt[:]), reads=["xt%d" % i], writes=[("x1d", t)], dma=True,
                  semkey=("dma", "x1st%d" % i))

        bias_l1, boffs, biasc_l1, boffc = None, None, None, None
        boffs, o = [], 0
        for h in range(4):
            boffs.append(o)
            o += (4 * NU) * (512 // QWS[h])
        boffc, o = [], 0
        for h in range(4):
            boffc.append(o)
            o += NU * (512 // QWS[h])

        QT, KT = Bt[0], Bt[1]
        for pi in range(2):
            tag = "C%d" % pi
            load_weights(c, wC[pi], lncol, Wb[:, :, 0:512], tag)
            Vv = Vsb[:, 0:NT * 130].rearrange("p (t h d) -> p t h d", h=2, d=65)
            S.add("pool", I("memset", Vv[:, :, :, 64:65], 1.0), reads=[("V", t) for t in range(NT)], writes=[("V", t) for t in range(NT)])
            ld(Bt[4][:], cd["E32"], "Ebuf")

            def v_evac(t, pb, bk, Vv=Vv):
                S.add("act", I("copy", out=Vv[:, t, :, 0:64], in_=pb[:, 256:384].rearrange("p (h d) -> p h d", h=2)), reads=[bk], writes=[("V", t)])
            pk = dict(z_dram=zsd, kq="B0", kk="B1", gkey="GC")
            emit_hT(c, x1, 0, xreads=[("x1d", 0)])
            emit_hT(c, x1, 1, xreads=[("x1d", 1)])
            proj_tile(c, 0, Wb[:, :, 0:512], tag, GC, QT, KT, v_evac, None, stage=1, **pk)
            for t in range(NT):
                if t + 2 < NT:
                    emit_hT(c, x1, t + 2, xreads=[("x1d", t + 2)])
                if t + 1 < NT:
                    proj_tile(c, t + 1, Wb[:, :, 0:512], tag, GC, QT, KT, v_evac, None, stage=1, **pk)
                proj_tile(c, t, Wb[:, :, 0:512], tag, GC, QT, KT, v_evac, None, stage=2, **pk)
            S.add("dve", I("tensor_reduce", out=kmf[:, 0:NB], in_=KT[:, :].rearrange("p (n k) -> p n k", k=256), axis=AX.X, op=ALU.add),
                  reads=[("B1", t) for t in range(NT)], writes=["kmf"])
            S.add("dve", I("memset", kmT[:, :], 0.0), writes=["kmT"])
            S.add("dve", I("tensor_scalar", out=kmT[:, 0:NB], in0=kmf[:, 0:NB], scalar1=1.0 / 256, scalar2=None, op0=ALU.mult), reads=["kmf", "kmT"], writes=["kmT"])
            attn_C(c, QT, KT, Vv, zsd, Bt[4], maskC, bias1, boffs[2 * pi:2 * pi + 2], QWS[2 * pi:2 * pi + 2], kmT, PB, out, 128 * pi, S_len)

        load_weights(c, wD, lncol, Wb[:, :, 0:1036], "D")
        Vs = Vsb[:, 0:NT * 65].rearrange("p (t d) -> p t d", d=65)
        Vw = Vsb[:, NT * 65:NT * 130].rearrange("p (t d) -> p t d", d=65)
        S.add("pool", I("memset", Vs[:, :, 64:65], 1.0), reads=[("V", t) for t in range(NT)], writes=[("V", t) for t in range(NT)])
        S.add("pool", I("memset", Vw[:, :, 64:65], 1.0), reads=[("V", t) for t in range(NT)], writes=[("V", t) for t in range(NT)])
        dargs = (Wb, GD, Bt[0], Bt[1], Bt[2], Bt[3], Bt[4], Vs, Vw, zsd, gsig)
        emit_hT(c, x1, 0, xreads=[("x1d", 0)])
        emit_hT(c, x1, 1, xreads=[("x1d", 1)])
        proj_tile_D(c, 0, *dargs, stage=1)
        for t in range(NT):
            if t + 2 < NT:
                emit_hT(c, x1, t + 2, xreads=[("x1d", t + 2)])
            if t + 1 < NT:
                proj_tile_D(c, t + 1, *dargs, stage=1)
            proj_tile_D(c, t, *dargs, stage=2)
        W1b = Wb[:, :, :].rearrange("p a b -> p (a b)")[:, 0:4096].rearrange("p (i h) -> p i h", h=128)
        wk = [("Wb", kc) for kc in range(8)]
        for q4 in range(4):
            i = q4 % 2
            S.add("sp", I("dma_start", out=c.xt[i][:, :].rearrange("p (i h) -> p i h", h=128), in_=W1_d[:, q4 * 8:(q4 + 1) * 8, :]), writes=["xt%d" % i], dma=True)
            S.add("dve", I("tensor_copy", out=W1b[:, q4 * 8:(q4 + 1) * 8, :], in_=c.xt[i][:, :].rearrange("p (i h) -> p i h", h=128)),
                  reads=["xt%d" % i] + wk, writes=wk)
        nsa_compress(c, Bt[4], W1b, W2b[:, 0:128], W2b[:, 128:192], peT, gk3col, BO, kcmpT, Vc, S_len)
        S.add("sp", I("dma_start", out=Bt[4][:], in_=cd["E64"]), reads=[("B4", t) for t in range(NT)] + ["Ebuf"], writes=["Ebuf"] + [("B4", t) for t in range(NT)], dma=True,
              semkey=("dma", "Ebuf"))
        attn_D(c, [Bt[0], Bt[1]], Bt[2], Bt[3], kcmpT, Vs, Vw, Vc, zsd, gsig, Bt[4], maskC, maskW, maskK, bias1, boffs, biasc, boffc, Ttab, out, S_len)
        S.emit()
    return nc


def build_L2(ntok):
    NT = ntok // 128
    nc = bass.Bass("TRN2", target_bir_lowering=False)
    st = contextlib.ExitStack()
    with st:
        c = Ctx(nc, st)
        S = c.S
        x1 = c.din("x1", [ntok, D], F32)
        m1T = c.din("m1T", [D, ntok], BF16)
        woo = c.din("woo", [D, D], F32)
        out = c.dout("out", [ntok, D], F32)
        c.bank = [c.ps("bank%d" % i, [128, 512], F32) for i in range(8)]
        xt = [c.sb("xt%d" % i, [128, D], F32) for i in range(2)]
        m0 = [c.sb("m0_%d" % i, [128, 8, 128], BF16) for i in range(2)]
        Wb = c.sb("Wb", [128, 8, 1024], BF16)
        for kc in range(8):
            i = kc % 2
            S.add("sp", I("dma_start", out=xt[i][:, :], in_=woo[kc * 128:(kc + 1) * 128, :]), writes=["xt%d" % i], dma=True)
            S.add("dve", I("tensor_copy", out=Wb[:, kc, :], in_=xt[i][:, :]), reads=["xt%d" % i], writes=[("Wb", kc)])
        for t in range(NT):
            i = t % 2
            S.add("sp", I("dma_start", out=xt[i][:], in_=x1[t * 128:(t + 1) * 128, :]), writes=["xt%d" % i], dma=True)
            S.add("sp", I("dma_start", out=m0[i][:, :, :], in_=m1T[:, t * 128:(t + 1) * 128].rearrange("(kc p) t -> p kc t", p=128)),
                  writes=["m0_%d" % i], dma=True)
            for cg in range(2):
                bi = 2 * i + cg
                for kc in range(8):
                    S.add("pe", I("matmul", c.bank[bi][:, :], lhsT=m0[i][:, kc, :], rhs=Wb[:, kc, cg * 512:(cg + 1) * 512], start=(kc == 0), stop=(kc == 7)),
                          reads=["m0_%d" % i, ("Wb", kc)], writes=[("bank", bi)])
                S.add("dve", I("tensor_tensor", out=xt[i][:, cg * 512:(cg + 1) * 512], in0=xt[i][:, cg * 512:(cg + 1) * 512], in1=c.bank[bi][:, :], op=ALU.add),
                      reads=["xt%d" % i, ("bank", bi)], writes=["xt%d" % i])
            S.add("sp", I("dma_start", out=out[t * 128:(t + 1) * 128, :], in_=xt[i][:]), reads=["xt%d" % i], writes=[("outd", t)], dma=True,
                  semkey=("dma", "ost%d" % i))
        S.emit()
    return nc


def host_inputs_L0(inp, b, half, S_len):
    w = inp["w_in_e"][0]
    aq, ak, av, az = [w[:, i * 512:(i + 1) * 512] for i in range(4)]
    bq, bk, bv, bz = [w[:, 2048 + i * 512:2048 + (i + 1) * 512] for i in range(4)]
    d = {}
    d["x"] = np.ascontiguousarray(inp["x"][b, :S_len])
    d["lncol"] = np.ascontiguousarray(inp["ln_e"][0].reshape(8, 128).T)
    for pi in range(2):
        c0 = (4 * half + 2 * pi) * 64
        d["wA%d" % pi] = np.ascontiguousarray(np.concatenate([t[:, c0:c0 + 128] for t in (aq, ak, av, az)], axis=1))
        c0 = (2 * half + pi) * 128
        d["wB%d" % pi] = np.ascontiguousarray(np.concatenate([t[:, c0:c0 + 128] for t in (bq, bk, bv, bz)], axis=1))
    q = inp["qkn_e"][0]
    d["gA"] = np.concatenate([q[0], q[0], q[1], q[1]])[None, :].copy()
    d["gB"] = np.concatenate([q[2], q[2], q[3], q[3]])[None, :].copy()
    d["lam"] = inp["lam_e"][0].reshape(1, 256).copy()
    d["subln"] = inp["subln_e"][0].reshape(1, 128).copy()
    cst, _ = consts_L0(half, S_len)
    d.update(cst)
    return d


def host_inputs_L1(inp, xb, mixed0_bf, half, S_len):
    w = inp["w_in_o"][0]
    off = {}
    o = 0
    for name, n in zip(("cq", "ck", "cv", "cz", "dq", "dkc", "dvc", "dks", "dvs", "dkw", "dvw", "dz", "dg"),
                       (512, 512, 512, 512, 512, 128, 128, 128, 128, 128, 128, 512, 24)):
        off[name] = o
        o += n
    col = lambda name, a, n: w[:, off[name] + a:off[name] + a + n]
    d = {}
    d["x"] = np.ascontiguousarray(xb)
    d["m0T"] = np.ascontiguousarray(mixed0_bf.T)
    d["woe"] = np.ascontiguousarray(inp["w_out_e"][0])
    d["lncol"] = np.ascontiguousarray(inp["ln_o"][0].reshape(8, 128).T)
    for pi in range(2):
        c0 = (4 * half + 2 * pi) * 64
        d["wC%d" % pi] = np.ascontiguousarray(np.concatenate([col(nm, c0, 128) for nm in ("cq", "ck", "cv", "cz")], axis=1))
    g = half
    q0 = 4 * half * 64
    ks, kw = col("dks", g * 64, 64), col("dkw", g * 64, 64)
    d["wD"] = np.ascontiguousarray(np.concatenate([col("dq", q0, 256), ks, ks, kw, kw,
                                                   col("dkc", g * 64, 64), col("dvc", g * 64, 64), col("dvs", g * 64, 64), col("dvw", g * 64, 64),
                                                   col("dz", q0, 256), col("dg", 4 * half * 3, 12)], axis=1))
    q = inp["qkn_o"][0]
    d["gC"] = np.concatenate([q[0], q[0], q[1], q[1]])[None, :].copy()
    d["gD"] = np.concatenate([q[2]] * 4 + [q[4]] * 2 + [q[5]] * 2)[None, :].copy()
    w1 = inp["phi_w1"][0]
    d["W1cat"] = np.ascontiguousarray(np.concatenate([w1[s].reshape(32, 64, 128).transpose(1, 0, 2) for s in range(2)], axis=0))
    w2 = inp["phi_w2"][0]
    d["W2cat"] = np.ascontiguousarray(np.concatenate([w2[0], w2[0], w2[1]], axis=1))
    pe = inp["phi_pe"][0]
    d["peT"] = np.ascontiguousarray(np.concatenate([pe[0].T, pe[1].T], axis=0))
    d["gk3col"] = np.concatenate([q[3], q[3]])[:, None].copy()
    a = np.arange(128)
    d["BO"] = (a[:, None] // 64 == a[None, :] // 64).astype(BF)
    cst, _, _ = consts_L1(half, S_len)
    d.update(cst)
    return d


BF = ml_dtypes.bfloat16
SEQ = 8192
NB_ = 4


def kernel(**inputs):
    inp = {k: np.asarray(v) for k, v in inputs.items()}
    S_len = SEQ
    cores = list(range(8))
    nc0 = build_L0(S_len)
    maps = [host_inputs_L0(inp, core // 2, core % 2, S_len) for core in cores]
    res0 = run_bass_kernel_spmd(nc0, maps, core_ids=cores)
    mixed0 = np.empty((NB_, S_len, 1024), BF)
    for core in cores:
        b, half = core // 2, core % 2
        o = res0.results[core]["mixed"]
        mixed0[b][:, 256 * half:256 * half + 256] = o[:, 0:256]
        mixed0[b][:, 512 + 256 * half:512 + 256 * half + 256] = o[:, 256:512]
    del res0, maps
    nc1 = build_L1(S_len)
    maps = [host_inputs_L1(inp, inp["x"][core // 2], mixed0[core // 2], core % 2, S_len) for core in cores]
    res1 = run_bass_kernel_spmd(nc1, maps, core_ids=cores)
    mixed1 = np.empty((NB_, S_len, 1024), BF)
    x1 = []
    for core in cores:
        b, half = core // 2, core % 2
        o = res1.results[core]["mixed"]
        mixed1[b][:, 256 * half:256 * half + 256] = o[:, 0:256]
        mixed1[b][:, 512 + 256 * half:512 + 256 * half + 256] = o[:, 256:512]
        if half == 0:
            x1.append(res1.results[core]["x1"])
    del res1, maps
    ntok = S_len // 2
    nc2 = build_L2(ntok)
    maps = []
    for core in cores:
        b, half = core // 2, core % 2
        sl = slice(half * ntok, (half + 1) * ntok)
        maps.append({"x1": np.ascontiguousarray(x1[b][sl]), "m1T": np.ascontiguousarray(mixed1[b][sl].T),
                     "woo": np.ascontiguousarray(inp["w_out_o"][0])})
    res2 = run_bass_kernel_spmd(nc2, maps, core_ids=cores)
    out = np.empty((NB_, S_len, 1024), np.float32)
    for core in cores:
        b, half = core // 2, core % 2
        out[b, half * ntok:(half + 1) * ntok] = res2.results[core]["out"]
    return out
```
